# Optimizing a Trainium2 kernel written in Bass

```python
import jax, jax.numpy as jnp
from jax import lax
import numpy as np

D_MODEL = 1024
BATCH = 4
SEQ = 8192
DEPTH = 1

SSD_D_INNER = 2 * D_MODEL
SSD_HEAD_DIM = 64
SSD_N_HEADS = SSD_D_INNER // SSD_HEAD_DIM
SSD_N_GROUPS = 8
SSD_HPG = SSD_N_HEADS // SSD_N_GROUPS
SSD_D_STATE = 128
SSD_CONV = 4
SSD_CHUNK = 128
SSD_XBC = SSD_D_INNER + 2 * SSD_N_GROUPS * SSD_D_STATE
GMLP_D = D_MODEL
GMLP_GROUPS = 8
GMLP_GROUP_DIM = GMLP_D // GMLP_GROUPS
GMLP_CHUNK = 128
IN_SPLITS = (SSD_D_INNER,
             SSD_D_INNER + SSD_XBC,
             SSD_D_INNER + SSD_XBC + SSD_N_HEADS,
             SSD_D_INNER + SSD_XBC + SSD_N_HEADS + 2 * GMLP_D,
             SSD_D_INNER + SSD_XBC + SSD_N_HEADS + 2 * GMLP_D + D_MODEL)
IN_COLS = IN_SPLITS[-1] + D_MODEL
N_EXPERTS = 256
TOP_K = 8
N_EXPERT_GROUPS = 8
TOPK_GROUPS = 4
EXPERT_FF = 256
SHARED_FF = 256
ROUTED_SCALE = 2.5
MOE_BLOCK = 128
DEEPNORM_ALPHA = (2 * DEPTH) ** 0.25
DEEPNORM_BETA = (8 * DEPTH) ** -0.25
LN_EPS = 1e-5
RMS_EPS = 1e-5

kernel_name = 'hybrid_ssd_gmlp_moe_deepnorm_adaln'


def layer_norm(x):
    xf = x.astype(jnp.float32)
    xc = xf - jnp.mean(xf, -1, keepdims=True)
    var = jnp.mean(xc * xc, -1, keepdims=True)
    return (xc * lax.rsqrt(var + LN_EPS)).astype(x.dtype)


def modulate(h, shift, scale):
    return layer_norm(h) * (1.0 + scale[:, None, :]) + shift[:, None, :]


def causal_dwconv(x, w, b):
    out = lax.conv_general_dilated(x, w[:, None, :], window_strides=(1,),
                                   padding=[(SSD_CONV - 1, 0)],
                                   dimension_numbers=('NWC', 'WIO', 'NWC'),
                                   feature_group_count=x.shape[-1])
    return out + b


def ssd_chunked(xh, dt, a, bm, cm):
    bsz, s = xh.shape[:2]
    nc = s // SSD_CHUNK
    xh = xh.reshape(bsz, nc, SSD_CHUNK, SSD_N_GROUPS, SSD_HPG, SSD_HEAD_DIM)
    dt = dt.reshape(bsz, nc, SSD_CHUNK, SSD_N_GROUPS, SSD_HPG)
    bm = bm.reshape(bsz, nc, SSD_CHUNK, SSD_N_GROUPS, SSD_D_STATE)
    cm = cm.reshape(bsz, nc, SSD_CHUNK, SSD_N_GROUPS, SSD_D_STATE)
    a_cs = jnp.cumsum(dt * a, axis=2)
    causal = jnp.tril(jnp.ones((SSD_CHUNK, SSD_CHUNK), bool))
    seg = a_cs[:, :, :, None] - a_cs[:, :, None, :]
    decay = jnp.exp(jnp.where(causal[:, :, None, None], seg, -jnp.inf))
    cb = jnp.einsum('bclgn,bcsgn->bclsg', cm, bm)
    w = cb[..., None] * decay * dt[:, :, None]
    y_diag = jnp.einsum('bclsgr,bcsgrp->bclgrp', w, xh)
    to_end = jnp.exp(a_cs[:, :, -1:] - a_cs) * dt
    states = jnp.einsum('bclgn,bclgrp->bcgrpn', bm, xh * to_end[..., None])
    chunk_decay = jnp.exp(a_cs[:, :, -1])

    def step(h, inp):
        st, dec = inp
        return dec[..., None, None] * h + st, h

    h0 = jnp.zeros((bsz, SSD_N_GROUPS, SSD_HPG, SSD_HEAD_DIM, SSD_D_STATE), jnp.float32)
    _, prev = lax.scan(step, h0, (jnp.moveaxis(states, 1, 0), jnp.moveaxis(chunk_decay, 1, 0)))
    prev = jnp.moveaxis(prev, 0, 1)
    y_off = jnp.einsum('bclgn,bcgrpn->bclgrp', cm, prev) * jnp.exp(a_cs)[..., None]
    return (y_diag + y_off).reshape(bsz, s, SSD_N_GROUPS, SSD_HPG, SSD_HEAD_DIM)


def ssd_branch(z, xbc, dt_raw, conv_w, conv_b, dt_bias, a_log, d_skip, norm_w):
    bsz, s, _ = z.shape
    gn = SSD_N_GROUPS * SSD_D_STATE
    xbc = jax.nn.silu(causal_dwconv(xbc, conv_w, conv_b)).astype(jnp.float32)
    xs = xbc[..., :SSD_D_INNER].reshape(bsz, s, SSD_N_GROUPS, SSD_HPG, SSD_HEAD_DIM)
    bm = xbc[..., SSD_D_INNER:SSD_D_INNER + gn].reshape(bsz, s, SSD_N_GROUPS, SSD_D_STATE)
    cm = xbc[..., SSD_D_INNER + gn:].reshape(bsz, s, SSD_N_GROUPS, SSD_D_STATE)
    dt = jax.nn.softplus((dt_raw + dt_bias).astype(jnp.float32)).reshape(bsz, s, SSD_N_GROUPS, SSD_HPG)
    a = -jnp.exp(a_log.astype(jnp.float32)).reshape(SSD_N_GROUPS, SSD_HPG)
    y = ssd_chunked(xs, dt, a, bm, cm)
    y = y + d_skip.astype(jnp.float32).reshape(SSD_N_GROUPS, SSD_HPG)[:, :, None] * xs
    y = y.reshape(bsz, s, SSD_D_INNER) * jax.nn.silu(z.astype(jnp.float32))
    y = y * lax.rsqrt(jnp.mean(y * y, -1, keepdims=True) + RMS_EPS)
    return (y * norm_w).astype(z.dtype)


def gmlp_branch(uv, ln_g, ln_b, ws, bs):
    bsz, s, _ = uv.shape
    uv = jax.nn.gelu(uv)
    u, v = uv[..., :GMLP_D], uv[..., GMLP_D:]
    v = layer_norm(v) * ln_g + ln_b
    v = v.reshape(bsz, s // GMLP_CHUNK, GMLP_CHUNK, GMLP_GROUPS, GMLP_GROUP_DIM)
    mask = jnp.tril(jnp.ones((GMLP_CHUNK, GMLP_CHUNK), ws.dtype))
    v = jnp.einsum('gts,bcsgd->bctgd', ws * mask, v) + bs.T[None, None, :, :, None]
    return u * v.reshape(bsz, s, GMLP_D)


def token_mixer(m, w_in, conv_w, conv_b, dt_bias, a_log, d_skip, ssd_norm_w,
                gmlp_ln_g, gmlp_ln_b, gmlp_ws, gmlp_bs, w_proj_ssd, w_proj_gmlp, w_out):
    proj = m @ w_in
    z, xbc, dt_raw, uv, gate_a, gate_b = jnp.split(proj, IN_SPLITS, axis=-1)
    y_a = ssd_branch(z, xbc, dt_raw, conv_w, conv_b, dt_bias, a_log, d_skip, ssd_norm_w) @ w_proj_ssd
    y_b = gmlp_branch(uv, gmlp_ln_g, gmlp_ln_b, gmlp_ws, gmlp_bs) @ w_proj_gmlp
    merged = jax.nn.sigmoid(gate_a) * y_a + jax.nn.sigmoid(gate_b) * y_b
    return merged @ w_out


def swiglu(x, wg, wu, wd):
    return (jax.nn.silu(x @ wg) * (x @ wu)) @ wd


def routed_experts(xs, top_idx, top_w, w_gate, w_up, w_down):
    t, d = xs.shape
    tk = t * TOP_K
    nb = -(-tk // MOE_BLOCK) + N_EXPERTS
    flat_e = top_idx.reshape(tk)
    flat_tok = jnp.repeat(jnp.arange(t, dtype=jnp.int32), TOP_K)
    flat_w = top_w.reshape(tk)
    order = jnp.argsort(flat_e)
    e_sorted = flat_e[order]
    counts = jnp.bincount(flat_e, length=N_EXPERTS)
    start = jnp.cumsum(counts) - counts
    padded = (counts + MOE_BLOCK - 1) // MOE_BLOCK * MOE_BLOCK
    pad_end = jnp.cumsum(padded)
    pad_start = pad_end - padded
    dest = pad_start[e_sorted] + jnp.arange(tk, dtype=jnp.int32) - start[e_sorted]
    slot_tok = jnp.full((nb * MOE_BLOCK,), t, jnp.int32).at[dest].set(flat_tok[order])
    slot_w = jnp.zeros((nb * MOE_BLOCK,), xs.dtype).at[dest].set(flat_w[order].astype(xs.dtype))
    block_e = jnp.searchsorted(pad_end, jnp.arange(nb, dtype=jnp.int32) * MOE_BLOCK, side='right')
    block_e = jnp.minimum(block_e, N_EXPERTS - 1)
    xs_pad = jnp.concatenate([xs, jnp.zeros((1, d), xs.dtype)], axis=0)

    def body(acc, blk):
        tok, wt, e = blk
        xb = xs_pad[tok]
        yb = swiglu(xb, w_gate[e], w_up[e], w_down[e]) * wt[:, None]
        return acc.at[tok].add(yb), None

    acc, _ = lax.scan(body, jnp.zeros((t + 1, d), xs.dtype),
                      (slot_tok.reshape(nb, MOE_BLOCK), slot_w.reshape(nb, MOE_BLOCK), block_e))
    return acc[:t]


def moe(h, w_router, router_bias, w_e_gate, w_e_up, w_e_down, w_sh_gate, w_sh_up, w_sh_down):
    bsz, s, d = h.shape
    t = bsz * s
    xs = h.reshape(t, d)
    scores = jax.nn.sigmoid((xs @ w_router).astype(jnp.float32))
    choice = scores + router_bias.astype(jnp.float32)
    grp = choice.reshape(t, N_EXPERT_GROUPS, N_EXPERTS // N_EXPERT_GROUPS)
    grp_score = lax.top_k(grp, 2)[0].sum(-1)
    _, grp_idx = lax.top_k(grp_score, TOPK_GROUPS)
    grp_mask = jnp.any(jax.nn.one_hot(grp_idx, N_EXPERT_GROUPS) > 0, axis=1)
    exp_mask = jnp.repeat(grp_mask, N_EXPERTS // N_EXPERT_GROUPS, axis=1)
    _, top_idx = lax.top_k(jnp.where(exp_mask, choice, -jnp.inf), TOP_K)
    top_w = jnp.take_along_axis(scores, top_idx, axis=1)
    top_w = top_w / (top_w.sum(-1, keepdims=True) + 1e-20) * ROUTED_SCALE
    routed = routed_experts(xs, top_idx, top_w, w_e_gate, w_e_up, w_e_down)
    shared = swiglu(xs, w_sh_gate, w_sh_up, w_sh_down)
    return (routed + shared).reshape(bsz, s, d)


def setup_inputs(seed: int = 0) -> dict:
    key = jax.random.key(seed)
    ks = iter(jax.random.split(key, 40))
    L, D = DEPTH, D_MODEL

    def nrm(shape, scale):
        return scale * jax.random.normal(next(ks), shape, jnp.float32)

    dt0 = jnp.exp(jax.random.uniform(next(ks), (L, SSD_N_HEADS), jnp.float32,
                                     minval=np.log(1e-3), maxval=np.log(1e-1)))
    return {
        'x': nrm((BATCH, SEQ, D), 1.0),
        'c': nrm((BATCH, D), 1.0),
        'w_ada': nrm((L, D, 6 * D), 0.5 * D ** -0.5),
        'b_ada': nrm((L, 6 * D), 0.02),
        'w_in': nrm((L, D, IN_COLS), D ** -0.5),
        'conv_w': nrm((L, SSD_CONV, SSD_XBC), SSD_CONV ** -0.5),
        'conv_b': nrm((L, SSD_XBC), 0.02),
        'dt_bias': dt0 + jnp.log(-jnp.expm1(-dt0)),
        'a_log': jnp.log(jax.random.uniform(next(ks), (L, SSD_N_HEADS), jnp.float32, minval=1.0, maxval=16.0)),
        'd_skip': 1.0 + nrm((L, SSD_N_HEADS), 0.1),
        'ssd_norm_w': 1.0 + nrm((L, SSD_D_INNER), 0.05),
        'gmlp_ln_g': 1.0 + nrm((L, GMLP_D), 0.05),
        'gmlp_ln_b': nrm((L, GMLP_D), 0.02),
        'gmlp_ws': nrm((L, GMLP_GROUPS, GMLP_CHUNK, GMLP_CHUNK), 0.5 * GMLP_CHUNK ** -0.5),
        'gmlp_bs': 1.0 + nrm((L, GMLP_GROUPS, GMLP_CHUNK), 0.1),
        'w_proj_ssd': nrm((L, SSD_D_INNER, D), DEEPNORM_BETA * SSD_D_INNER ** -0.5),
        'w_proj_gmlp': nrm((L, GMLP_D, D), DEEPNORM_BETA * GMLP_D ** -0.5),
        'w_out': nrm((L, D, D), DEEPNORM_BETA * D ** -0.5),
        'ln1_g': 1.0 + nrm((L, D), 0.05),
        'ln1_b': nrm((L, D), 0.02),
        'w_router': nrm((L, D, N_EXPERTS), D ** -0.5),
        'router_bias': nrm((L, N_EXPERTS), 0.01),
        'w_e_gate': nrm((L, N_EXPERTS, D, EXPERT_FF), D ** -0.5),
        'w_e_up': nrm((L, N_EXPERTS, D, EXPERT_FF), D ** -0.5),
        'w_e_down': nrm((L, N_EXPERTS, EXPERT_FF, D), DEEPNORM_BETA * EXPERT_FF ** -0.5),
        'w_sh_gate': nrm((L, D, SHARED_FF), D ** -0.5),
        'w_sh_up': nrm((L, D, SHARED_FF), D ** -0.5),
        'w_sh_down': nrm((L, SHARED_FF, D), DEEPNORM_BETA * SHARED_FF ** -0.5),
        'ln2_g': 1.0 + nrm((L, D), 0.05),
        'ln2_b': nrm((L, D), 0.02),
    }


def reference(x, c, w_ada, b_ada, w_in, conv_w, conv_b, dt_bias, a_log, d_skip, ssd_norm_w,
              gmlp_ln_g, gmlp_ln_b, gmlp_ws, gmlp_bs, w_proj_ssd, w_proj_gmlp, w_out,
              ln1_g, ln1_b, w_router, router_bias, w_e_gate, w_e_up, w_e_down,
              w_sh_gate, w_sh_up, w_sh_down, ln2_g, ln2_b):
    h = x
    for i in range(DEPTH):
        ada = jax.nn.silu(c) @ w_ada[i] + b_ada[i]
        sh1, sc1, g1, sh2, sc2, g2 = jnp.split(ada, 6, axis=-1)
        m = modulate(h, sh1, sc1)
        mix = token_mixer(m, w_in[i], conv_w[i], conv_b[i], dt_bias[i], a_log[i], d_skip[i],
                          ssd_norm_w[i], gmlp_ln_g[i], gmlp_ln_b[i], gmlp_ws[i], gmlp_bs[i],
                          w_proj_ssd[i], w_proj_gmlp[i], w_out[i])
        h = layer_norm(DEEPNORM_ALPHA * h + g1[:, None, :] * mix) * ln1_g[i] + ln1_b[i]
        m = modulate(h, sh2, sc2)
        f = moe(m, w_router[i], router_bias[i], w_e_gate[i], w_e_up[i], w_e_down[i],
                w_sh_gate[i], w_sh_up[i], w_sh_down[i])
        h = layer_norm(DEEPNORM_ALPHA * h + g2[:, None, :] * f) * ln2_g[i] + ln2_b[i]
    return h
```

```python
import os
import numpy as np
from contextlib import ExitStack
import concourse.bass as bass
import concourse.mybir as mybir
from concourse.bass_utils import run_bass_kernel_spmd

F32 = mybir.dt.float32
BF16 = mybir.dt.bfloat16
U32 = mybir.dt.uint32
AF = mybir.ActivationFunctionType
ALU = mybir.AluOpType
AX = mybir.AxisListType

D = 1024
NIN = 10272
ALPHA = float(2.0 ** 0.25)
LN_EPS = 1e-5
RMS_EPS = 1e-5
GC = 0.7978845608028654
BIG = 1.0e4


class Buf:
    __slots__ = ("name", "w", "r")

    def __init__(self, name=""):
        self.name = name
        self.w = None
        self.r = {}


class T:
    def __init__(self, h, nsub=0, name=""):
        self.h = h
        self.b = Buf(name)
        self.subs = [Buf("%s.%d" % (name, i)) for i in range(nsub)]

    def __getitem__(self, k):
        return self.h[k]

    def s(self, i):
        return self.subs[i]


def _b(t):
    return t.b if isinstance(t, T) else t


class Sched:
    NDMA = 40

    def __init__(self, nc):
        self.nc = nc
        self.eng = {"pe": nc.tensor, "act": nc.scalar, "dve": nc.vector,
                    "pool": nc.gpsimd, "sp": nc.sync}
        self.sem = {k: nc.alloc_semaphore("q_" + k) for k in self.eng}
        self.cnt = {k: 0 for k in self.eng}
        self.waited = {k: {} for k in self.eng}
        self.dsem = [nc.alloc_semaphore("d%d" % i) for i in range(self.NDMA)]
        self.duse = [0] * self.NDMA
        self.dnext = 0
        self.nins = 0

    def _wait(self, e, ev):
        sem, val = ev
        if e == "pe" and sem is self.sem["pe"]:
            return
        w = self.waited[e]
        if w.get(sem.num, 0) >= val:
            return
        w[sem.num] = val
        self.eng[e].wait_ge(sem, val)
        self.nins += 1

    def _deps(self, e, reads, writes):
        for t in reads:
            b = _b(t)
            if b.w is not None:
                self._wait(e, b.w)
        for t in writes:
            b = _b(t)
            if b.w is not None:
                self._wait(e, b.w)
            for ev in b.r.values():
                self._wait(e, ev)

    def _commit(self, ev, reads, writes):
        sem, val = ev
        for t in reads:
            b = _b(t)
            old = b.r.get(sem.num)
            if old is None or old[1] < val:
                b.r[sem.num] = ev
        for t in writes:
            b = _b(t)
            b.w = ev
            b.r = {}

    def op(self, e, fn, r=(), w=()):
        self._deps(e, r, w)
        ins = fn(self.eng[e])
        self.cnt[e] += 1
        ins.then_inc(self.sem[e], 1)
        self._commit((self.sem[e], self.cnt[e]), r, w)
        self.nins += 1
        return ins

    def dve(self, fn, r=(), w=()):
        return self.op("dve", fn, r, w)

    def act(self, fn, r=(), w=()):
        return self.op("act", fn, r, w)

    def pool(self, fn, r=(), w=()):
        return self.op("pool", fn, r, w)

    def mm(self, out, pairs, r=(), w=(), first=True, last=True):
        self._deps("pe", r, w)
        n = len(pairs)
        ins = None
        for i, (l, rh) in enumerate(pairs):
            ins = self.nc.tensor.matmul(out, l, rh, start=(first and i == 0), stop=(last and i == n - 1))
        self.cnt["pe"] += 1
        ins.then_inc(self.sem["pe"], 1)
        self._commit((self.sem["pe"], self.cnt["pe"]), r, w)
        self.nins += n

    def tr(self, out, in_, ident, r=(), w=()):
        return self.op("pe", lambda e: e.transpose(out, in_, ident), r, w)

    def dma(self, q, out, in_, r=(), w=(), fn=None):
        slot = self.dnext
        self.dnext = (self.dnext + 1) % self.NDMA
        sem = self.dsem[slot]
        if self.duse[slot] > 0:
            self._wait(q, (sem, 16 * self.duse[slot]))
        self._deps(q, r, w)
        if fn is None:
            ins = self.eng[q].dma_start(out=out, in_=in_)
        else:
            ins = fn(self.eng[q])
        ins.then_inc(sem, 16)
        self.duse[slot] += 1
        self._commit((sem, 16 * self.duse[slot]), r, w)
        self.nins += 1
        return ins

    def barrier(self):
        for e in self.eng:
            for e2 in self.eng:
                if e2 != e and self.cnt[e2] > 0:
                    self._wait(e, (self.sem[e2], self.cnt[e2]))
            for i in range(self.NDMA):
                if self.duse[i] > 0:
                    self._wait(e, (self.dsem[i], 16 * self.duse[i]))

    def finish(self):
        for i in range(self.NDMA):
            if self.duse[i] > 0:
                self._wait("sp", (self.dsem[i], 16 * self.duse[i]))


def build(NCH, NPV, C, debug=False):
    nc = bass.Bass("TRN2", target_bir_lowering=False)
    S = Sched(nc)
    NEB = NCH * 8 + 256
    NSLOT = NEB * 128

    def din(name, shape, dt=F32):
        return nc.dram_tensor(name, list(shape), dt, kind="ExternalInput").ap()

    x_cur = din("x_cur", [NCH * 128, D])
    x_prev = din("x_prev", [max(NPV, 1) * 128, D])
    flag = din("flag", [128, 1])
    c_b = din("c_b", [D])
    w_ada = din("w_ada", [D, 6 * D])
    b_ada = din("b_ada", [6 * D])
    w_in = din("w_in", [D, NIN])
    conv_w = din("conv_w", [4, 4096])
    conv_b = din("conv_b", [4096])
    dt_bias = din("dt_bias", [32])
    a_log = din("a_log", [32])
    d_skip = din("d_skip", [32])
    ssd_norm_w = din("ssd_norm_w", [2048])
    gmlp_ln_g = din("gmlp_ln_g", [D])
    gmlp_ln_b = din("gmlp_ln_b", [D])
    gmlp_ws = din("gmlp_ws", [8, 128, 128])
    gmlp_bs = din("gmlp_bs", [8, 128])
    w_proj_ssd = din("w_proj_ssd", [2048, D])
    w_proj_gmlp = din("w_proj_gmlp", [D, D])
    w_out = din("w_out", [D, D])
    ln1_g = din("ln1_g", [D])
    ln1_b = din("ln1_b", [D])
    w_router = din("w_router", [D, 256])
    router_bias = din("router_bias", [256])
    w_e_gate = din("w_e_gate", [256 * 128, 2048])
    w_e_up = din("w_e_up", [256 * 128, 2048])
    w_e_down = din("w_e_down", [256 * 128, 2048])
    w_sh_gate = din("w_sh_gate", [D, 256])
    w_sh_up = din("w_sh_up", [D, 256])
    w_sh_down = din("w_sh_down", [256, D])
    ln2_g = din("ln2_g", [D])
    ln2_b = din("ln2_b", [D])
    out = nc.dram_tensor("out", [NCH * 128, D], F32, kind="ExternalOutput").ap()
    dbg = None
    if debug:
        dbg = nc.dram_tensor("dbg", [NCH * 128, D], F32, kind="ExternalOutput").ap()

    blocks = []

    def wv(w, k):
        return w.rearrange("(p k) c -> p k c", k=k)

    win_v = wv(w_in, 8)
    for i in range(8):
        blocks.append(("xbc%d" % i, [(win_v[:, :, 2048 + 512 * i:2048 + 512 * (i + 1)], 0)], 8, 512))
    blocks.append(("dt", [(win_v[:, :, 6144:6176], 0)], 8, 32))
    for i in range(4):
        blocks.append(("z%d" % i, [(win_v[:, :, 512 * i:512 * (i + 1)], 0)], 8, 512))
    for i in range(4):
        blocks.append(("uv%d" % i, [(win_v[:, :, 6176 + 512 * i:6176 + 512 * (i + 1)], 0)], 8, 512))
    for i in range(2):
        blocks.append(("ga%d" % i, [(win_v[:, :, 8224 + 512 * i:8224 + 512 * (i + 1)], 0)], 8, 512))
    for i in range(2):
        blocks.append(("gb%d" % i, [(win_v[:, :, 9248 + 512 * i:9248 + 512 * (i + 1)], 0)], 8, 512))
    wps_v = wv(w_proj_ssd, 16)
    for ch in range(2):
        for kh in range(2):
            blocks.append(("ps%d%d" % (ch, kh), [(wps_v[:, kh * 8:(kh + 1) * 8, ch * 512:(ch + 1) * 512], 0)], 8, 512))
    wpg_v = wv(w_proj_gmlp, 8)
    for ch in range(2):
        blocks.append(("pg%d" % ch, [(wpg_v[:, :, ch * 512:(ch + 1) * 512], 0)], 8, 512))
    wo_v = wv(w_out, 8)
    for ch in range(2):
        blocks.append(("wo%d" % ch, [(wo_v[:, :, ch * 512:(ch + 1) * 512], 0)], 8, 512))
    blocks.append(("rt", [(wv(w_router, 8), 0)], 8, 256))
    blocks.append(("sgu", [(wv(w_sh_gate, 8), 0), (wv(w_sh_up, 8), 256)], 8, 512))
    blocks.append(("sd", [(w_sh_down.rearrange("(b q) c -> q b c", q=128), 0)], 2, 1024))
    bidx = {b[0]: i for i, b in enumerate(blocks)}
    NBLK = len(blocks)
    wblk = nc.dram_tensor("wblk", [NBLK, 128, 4096], BF16, kind="Internal").ap()
    wblk_b = [Buf("wblk%d" % i) for i in range(NBLK)]

    xs_d = nc.dram_tensor("xs_d", [NSLOT, D], BF16, kind="Internal").ap()
    ys_d = nc.dram_tensor("ys_d", [NSLOT, D], BF16, kind="Internal").ap()
    res2_d = nc.dram_tensor("res2_d", [NCH * 128, D], F32, kind="Internal").ap()
    m2_d = nc.dram_tensor("m2_d", [NCH * 128, D], BF16, kind="Internal").ap()
    m2_b = [Buf("m2d_%d" % i) for i in range(NCH)]
    xs_b = Buf("xs_d")
    ys_b = Buf("ys_d")
    res2_b = [Buf("res2_%d" % i) for i in range(NCH)]

    reg_slot = nc.gpsimd.to_reg(NSLOT - 1)
    reg_w = nc.gpsimd.to_reg(256 * 128 - 1)

    def sb(name, shape, dt, nsub=0):
        if scope[0] is None:
            return T(nc.alloc_sbuf_tensor(name, list(shape), dt), nsub, name)
        return T(scope[0].enter_context(nc.sbuf_tensor(name, list(shape), dt)), nsub, name)

    scope = [None]

    banks = [T(nc.alloc_psum_tensor("bank%d" % i, [128, 512], F32), 0, "bank%d" % i) for i in range(8)]
    bank_i = [0]

    pinned = set()

    def ps():
        while (bank_i[0] % 8) in pinned:
            bank_i[0] += 1
        t = banks[bank_i[0] % 8]
        bank_i[0] += 1
        return t

    def psb(t):
        return t[:].bitcast(BF16)

    ident = sb("ident", [128, 128], BF16)
    identf = sb("identf", [128, 128], F32)
    Uf = sb("Uf", [128, 128], F32)
    Ub = sb("Ub", [128, 128], BF16)
    SLb = sb("SLb", [128, 128], BF16)
    SUb = sb("SUb", [128, 128], BF16)
    onesb = sb("onesb", [128, 128], BF16)
    iota_e = sb("iota_e", [128, 256], F32)
    mhalf = sb("mhalf", [128, 1], F32)
    WsT = sb("WsT", [128, 8, 128], BF16)
    bsT = sb("bsT", [128, 8], F32)
    Dg = sb("Dg", [128, 32, 4, 128], BF16)
    brow = sb("brow", [1, 4096], BF16)
    a_row = sb("a_row", [128, 32], F32)
    dtb_row = sb("dtb_row", [128, 32], F32)
    dsk32 = sb("dsk32", [128, 32], F32)
    normw_pk = sb("normw_pk", [128, 16], F32)
    sh1_pk = sb("sh1_pk", [128, 8], F32)
    sc1_pk = sb("sc1_pk", [128, 8], F32)
    lng_row = sb("lng_row", [128, D], BF16)
    lnb_row = sb("lnb_row", [128, D], BF16)
    ln1g_row = sb("ln1g_row", [128, D], BF16)
    ln1b_row = sb("ln1b_row", [128, D], BF16)
    sc2_row = sb("sc2_row", [128, D], BF16)
    sh2_row = sb("sh2_row", [128, D], BF16)
    g1h_row = sb("g1h_row", [128, D], BF16)
    g2h_row = sb("g2h_row", [128, D], BF16)
    rb_row = sb("rb_row", [128, 256], F32)
    flag_t = sb("flag_t", [128, 1], F32)
    st6 = sb("st6", [128, 12], F32)
    mv = sb("mv", [128, 2], F32)
    rstd = sb("rstd", [128, 1], F32)
    nbias = sb("nbias", [128, 1], F32)
    slot_u = sb("slot_u", [128, NCH, 8], U32)
    wk_t = sb("wk_t", [128, NCH, 8], F32)
    Rcnt = sb("Rcnt", [128, 256], BF16)
    idx_all = sb("idx_all", [128, NCH, 8], F32)
    pos_all = sb("pos_all", [128, NCH, 8], F32)
    Sst = sb("Sst", [128, 2048], F32)
    Sbf = sb("Sbf", [128, 2048], BF16)
    xBCT = sb("xBCT", [128, 32, 131], BF16, nsub=8)

    def aff(t, pattern, cm, op, fill, r=(), extra_w=()):
        S.pool(lambda e: e.affine_select(out=t[:], in_=t[:], pattern=pattern, compare_op=op, fill=fill,
                                         base=0, channel_multiplier=cm), r=[t], w=[t])

    S.pool(lambda e: e.memset(identf[:], 0.0), w=[identf])
    aff(identf, [[1, 128]], -1, ALU.not_equal, 1.0)
    S.dve(lambda e: e.tensor_copy(ident[:], identf[:]), r=[identf], w=[ident])
    S.pool(lambda e: e.memset(Uf[:], 1.0), w=[Uf])
    aff(Uf, [[1, 128]], -1, ALU.is_ge, 0.0)
    S.dve(lambda e: e.tensor_copy(Ub[:], Uf[:]), r=[Uf], w=[Ub])
    tmpf = sb("tmpf", [128, 128], F32)
    S.pool(lambda e: e.memset(tmpf[:], 1.0), w=[tmpf])
    aff(tmpf, [[-1, 128]], 1, ALU.is_gt, 0.0)
    S.dve(lambda e: e.tensor_copy(SLb[:], tmpf[:]), r=[tmpf], w=[SLb])
    S.pool(lambda e: e.memset(tmpf[:], 1.0), r=[], w=[tmpf])
    aff(tmpf, [[1, 128]], -1, ALU.is_gt, 0.0)
    S.dve(lambda e: e.tensor_copy(SUb[:], tmpf[:]), r=[tmpf], w=[SUb])
    S.pool(lambda e: e.memset(onesb[:], 1.0), w=[onesb])
    S.pool(lambda e: e.iota(iota_e[:], pattern=[[1, 256]], base=0, channel_multiplier=0,
                            allow_small_or_imprecise_dtypes=True), w=[iota_e])
    S.pool(lambda e: e.memset(mhalf[:], -0.5), w=[mhalf])
    S.pool(lambda e: e.memset(Sst[:], 0.0), w=[Sst])
    S.pool(lambda e: e.memset(Sbf[:], 0.0), w=[Sbf])
    S.pool(lambda e: e.memset(xBCT[:], 0.0), w=[xBCT] + xBCT.subs)
    S.pool(lambda e: e.memset(Rcnt[:], 0.0), w=[Rcnt])
    S.pool(lambda e: e.memset(wk_t[:], 0.0), w=[wk_t])

    S.dma("sp", flag_t[:], flag, w=[flag_t])
    for row, src in ((lng_row, gmlp_ln_g), (lnb_row, gmlp_ln_b), (ln1g_row, ln1_g), (ln1b_row, ln1_b)):
        S.dma("pool", row[:], src.partition_broadcast(128), w=[row])
    S.dma("sp", rb_row[:], router_bias.partition_broadcast(128), w=[rb_row])
    S.dma("sp", dtb_row[:], dt_bias.partition_broadcast(128), w=[dtb_row])
    S.dma("sp", a_row[:], a_log.partition_broadcast(128), w=[a_row])
    S.dma("sp", normw_pk[:], ssd_norm_w.rearrange("(p k) -> p k", k=16), w=[normw_pk])
    S.act(lambda e: e.activation(a_row[:], a_row[:], AF.Exp), r=[a_row], w=[a_row])
    S.dve(lambda e: e.tensor_scalar_mul(a_row[:], a_row[:], -1.0), r=[a_row], w=[a_row])

    with ExitStack() as st1:
        scope[0] = st1
        stg = [sb("stg%d" % i, [128, 4096], BF16) for i in range(3)]
        cw4 = sb("cw4", [4, 4096], F32)
        cb1 = sb("cb1", [1, 4096], F32)
        wsl = sb("wsl", [128, 8, 128], F32)
        wslb = sb("wslb", [128, 8, 128], BF16)
        bs8 = sb("bs8", [8, 128], F32)
        wcol = sb("wcol", [128, 32, 4], F32)

        S.pool(lambda e: e.memset(stg[2][:], 0.0), w=[stg[2]])
        xs_z = xs_d.rearrange("(b p i) d -> b p (i d)", p=128, i=4)
        for b in range(NSLOT // 512):
            S.dma("sp", xs_z[b], stg[2][:], r=[stg[2]], w=[xs_b])

        for i, (name, srcs, K, N) in enumerate(blocks):
            st = stg[i % 2]
            v = st[:, 0:K * N].rearrange("p (k n) -> p k n", k=K)
            for (src, co) in srcs:
                n = src.shape[2]
                S.dma("pool", v[:, :, co:co + n], src, w=[st])
            S.dma("sp", wblk[i, :, 0:K * N], st[:, 0:K * N], r=[st], w=[wblk_b[i]])

        S.dma("sp", dsk32[:], d_skip.partition_broadcast(128), w=[dsk32])
        S.dma("sp", cw4[:], conv_w, w=[cw4])
        pw = ps()
        pwv = pw[:, 0:128].rearrange("p (j k) -> p j k", k=4)
        for j in range(32):
            S.mm(pwv[:, j, :], [(cw4[0:4, j * 128:(j + 1) * 128], identf[0:4, 0:4])], r=[cw4, identf], w=[pw])
        S.dve(lambda e: e.tensor_scalar_mul(wcol[:], pwv, 0.5), r=[pw], w=[wcol])
        for j in range(32):
            for k in range(4):
                eng = "dve" if (j + k) % 2 == 0 else "pool"
                S.op(eng, lambda e, j=j, k=k: e.tensor_scalar(Dg[:, j, k, :], identf[:], wcol[:, j, k:k + 1], None,
                                                              ALU.mult), r=[identf, wcol], w=[Dg])
        S.dma("sp", cb1[:], conv_b.rearrange("(o c) -> o c", o=1), w=[cb1])
        S.dve(lambda e: e.tensor_scalar_mul(brow[:], cb1[:], 0.5), r=[cb1], w=[brow])
        S.dma("sp", wsl[:], gmlp_ws.rearrange("g t s -> t g s"), w=[wsl])
        S.pool(lambda e: e.affine_select(out=wsl[:], in_=wsl[:], pattern=[[0, 8], [-1, 128]], compare_op=ALU.is_ge,
                                         fill=0.0, base=0, channel_multiplier=1), r=[wsl], w=[wsl])
        S.dve(lambda e: e.tensor_copy(wslb[:], wsl[:]), r=[wsl], w=[wslb])
        pt = ps()
        ptv = psb(pt).rearrange("p (g t) -> p g t", g=8)
        for g in range(8):
            S.tr(ptv[:, g, :], wslb[:, g, :], ident[:], r=[wslb, ident], w=[pt])
        S.dve(lambda e: e.tensor_copy(WsT[:], ptv), r=[pt], w=[WsT])
        S.dma("sp", bs8[:], gmlp_bs, w=[bs8])
        pb = ps()
        S.mm(pb[:, 0:8], [(bs8[0:8, :], identf[0:8, 0:8])], r=[bs8, identf], w=[pb])
        S.dve(lambda e: e.tensor_copy(bsT[:], pb[:, 0:8]), r=[pb], w=[bsT])

        S.barrier()
    with ExitStack() as st2:
        scope[0] = st2
        wa = [sb("wa%d" % i, [128, 8, 512], F32) for i in range(2)]
        bad = [sb("bad%d" % i, [128, 512], F32) for i in range(2)]
        adarow = sb("adarow", [128, 6 * D], F32)
        screp = sb("screp", [128, 8, 128], F32)
        dtmp = sb("dtmp", [128, 128, 8], F32)
        cpk = sb("cpk", [128, 8], F32)
        cpk2 = sb("cpk2", [128, 8], F32)
        S.dma("sp", cpk[:], c_b.rearrange("(p k) -> p k", k=8), w=[cpk])
        S.act(lambda e: e.activation(cpk2[:], cpk[:], AF.Tanh, scale=0.5), r=[cpk], w=[cpk2])
        S.dve(lambda e: e.scalar_tensor_tensor(cpk2[:], cpk2[:], 1.0, cpk[:], ALU.add, ALU.mult), r=[cpk2, cpk], w=[cpk2])
        S.dve(lambda e: e.tensor_scalar_mul(cpk2[:], cpk2[:], 0.5), r=[cpk2], w=[cpk2])
        S.dve(lambda e: e.tensor_copy(screp[:], cpk2[:].unsqueeze(2).to_broadcast([128, 8, 128])), r=[cpk2], w=[screp])
        wada_v = wv(w_ada, 8)
        for b in range(12):
            wt = wa[b % 2]
            S.dma("sp", wt[:], wada_v[:, :, b * 512:(b + 1) * 512], w=[wt])
            pa = ps()
            S.mm(pa[:], [(screp[:, k, :], wt[:, k, :]) for k in range(8)], r=[screp, wt], w=[pa])
            bd = bad[b % 2]
            S.dma("sp", bd[:], b_ada[b * 512:(b + 1) * 512].partition_broadcast(128), w=[bd])
            S.dve(lambda e, b=b, pa=pa, bd=bd: e.tensor_tensor(adarow[:, b * 512:(b + 1) * 512], pa[:], bd[:], ALU.add),
                  r=[pa, bd], w=[adarow])
        S.dve(lambda e: e.tensor_copy(sh2_row[:], adarow[:, 3 * D:4 * D]), r=[adarow], w=[sh2_row])
        S.dve(lambda e: e.tensor_scalar_add(sc2_row[:], adarow[:, 4 * D:5 * D], 1.0), r=[adarow], w=[sc2_row])
        S.dve(lambda e: e.tensor_scalar_mul(g1h_row[:], adarow[:, 2 * D:3 * D], 0.5), r=[adarow], w=[g1h_row])
        S.dve(lambda e: e.tensor_scalar_mul(g2h_row[:], adarow[:, 5 * D:6 * D], 0.5), r=[adarow], w=[g2h_row])
        for (dst, off, add1) in ((sh1_pk, 0, 0.0), (sc1_pk, D, 1.0)):
            S.dve(lambda e, off=off: e.tensor_tensor(dtmp[:], adarow[:, off:off + D].rearrange("p (q k) -> p q k", k=8),
                                                    identf[:].unsqueeze(2).to_broadcast([128, 128, 8]), ALU.mult),
                  r=[adarow, identf], w=[dtmp])
            S.dve(lambda e, dst=dst: e.tensor_reduce(dst[:], dtmp[:].rearrange("p q k -> p k q"), AX.X, ALU.add),
                  r=[dtmp], w=[dst])
            if add1:
                S.dve(lambda e, dst=dst: e.tensor_scalar_add(dst[:], dst[:], 1.0), r=[dst], w=[dst])
        S.barrier()
    scope[0] = None

    ph1 = ExitStack()
    scope[0] = ph1
    NB = 3
    ring = [sb("ring%d" % i, [128, 4096], BF16) for i in range(NB)]
    worder = []
    wstate = {"issued": 0, "next": 0}

    def chunk_blocks(kind, last=False):
        if kind == "A":
            l = ["xbc0", "xbc1", "xbc2", "xbc3", "xbc4", "xbc5"]
            if last:
                l += ["xbc6", "xbc7"]
            return l + ["dt"]
        return (["xbc%d" % i for i in range(8)] + (["rt", "sgu", "sd"] if not last else []) + ["dt"] +
                ["z0", "z1", "uv0", "uv1", "z2", "uv2", "uv3", "z3", "ga0", "ga1", "gb0", "gb1"] +
                ["ps00", "ps01", "ps10", "ps11", "pg0", "pg1", "wo0", "wo1"])

    for i in range(NPV):
        worder.extend(chunk_blocks("A", i == NPV - 1))
    worder.extend(chunk_blocks("M", last=True))
    for i in range(1, NCH):
        worder.extend(chunk_blocks("M", last=False))
    worder.extend(["rt", "sgu", "sd"])

    def wget(name):
        i = wstate["next"]
        assert worder[i] == name, (worder[i], name)
        while wstate["issued"] < min(len(worder), i + NB):
            j = wstate["issued"]
            bi = bidx[worder[j]]
            K, N = blocks[bi][2], blocks[bi][3]
            rt = ring[j % NB]
            S.dma("sp", rt[:, 0:K * N], wblk[bi, :, 0:K * N], r=[wblk_b[bi]], w=[rt])
            wstate["issued"] += 1
        wstate["next"] += 1
        rt = ring[i % NB]
        bi = bidx[name]
        K, N = blocks[bi][2], blocks[bi][3]
        return rt, rt[:, 0:K * N].rearrange("p (k n) -> p k n", k=K)

    xts = [sb("xt0", [128, D], F32), sb("xt1", [128, D], F32)]
    mT = sb("mT", [128, 8, 128], BF16)
    xc = sb("xc", [128, 32, 128], BF16, nsub=8)
    tnh = [sb("tnh%d" % i, [128, 512], F32) for i in range(2)]
    x_tm = sb("x_tm", [128, 2048], BF16)
    xdte = x_tm
    B_tm = sb("B_tm", [128, 1024], BF16)
    xd_tm = sb("xd_tm", [128, 2048], BF16)
    xdt = sb("xdt", [128, 2048], BF16)
    dtt = sb("dtt", [128, 32], F32)
    dtx = sb("dtx", [128, 32], F32)
    adt = sb("adt", [128, 32], F32)
    adt_hi = sb("adt_hi", [128, 32], BF16)
    adt_lo = sb("adt_lo", [128, 32], BF16)
    adt_r = sb("adt_r", [128, 32], F32)
    acs = sb("acs", [128, 32], F32)
    ea = sb("ea", [128, 32], F32)
    eend = sb("eend", [128, 32], F32)
    dec = sb("dec", [128, 32], F32)
    cbm = [sb("cbm%d" % i, [128, 128], F32) for i in range(2)]
    segr = [sb("segr%d" % i, [128, 4, 128], BF16) for i in range(2)]
    Eg = [sb("Eg%d" % i, [128, 4, 128], BF16) for i in range(2)]
    WTg = [sb("WTg%d" % i, [128, 4, 128], BF16) for i in range(2)]
    ytmp = [sb("ytmp%d" % i, [128, 256], F32) for i in range(2)]
    zg = sb("zg", [128, 2048], BF16, nsub=4)
    yg = sb("yg", [128, 2048], BF16)
    ssq = sb("ssq", [128, 8], F32)
    rinv = sb("rinv", [128, 1], F32)
    gA = sb("gA", [128, D], BF16, nsub=2)
    gB = sb("gB", [128, D], BF16, nsub=2)
    u_t = sb("u_t", [128, D], BF16, nsub=2)
    v_t = sb("v_t", [128, D], F32, nsub=2)
    vb = sb("vb", [128, D], BF16)
    gm = sb("gm", [128, D], BF16)
    gmT = sb("gmT", [128, 8, 128], BF16)
    mg = gm
    mgT = gmT
    mtmp = [sb("mtmp%d" % i, [128, 512], F32) for i in range(2)]
    h1 = sb("h1", [128, D], F32)
    m2 = sb("m2", [128, D], BF16)
    xn = sb("xn", [128, D], BF16)
    m2T = sb("m2T", [128, 8, 128], BF16)
    hsa = sb("hsa", [128, 256], F32)
    hs = sb("hs", [128, 2, 128], BF16)
    sco = sb("sco", [128, 256], F32)
    cho = sb("cho", [128, 256], F32)
    g8 = sb("g8", [128, 8, 8], F32)
    gs = sb("gs", [128, 8], F32)
    gs8 = sb("gs8", [128, 8], F32)
    gpen = sb("gpen", [128, 8], F32)
    top8 = sb("top8", [128, 8], F32)
    idx8 = sb("idx8", [128, 8], U32)
    idxf = sb("idxf", [128, 8], F32)
    selb = sb("selb", [128, 256], BF16)
    posf = sb("posf", [128, 256], F32)
    junk = sb("junk", [128, 256], F32)
    sqj = junk
    pk8 = sb("pk8", [128, 8], F32)
    sk8 = sb("sk8", [128, 8], F32)
    slf = sb("slf", [128, 8], F32)
    ssum = sb("ssum", [128, 1], F32)

    def layer_norm_stats(src, eps):
        S.dve(lambda e: e.bn_stats(st6[:, 0:6], src[:, 0:512]), r=[src], w=[st6])
        S.dve(lambda e: e.bn_stats(st6[:, 6:12], src[:, 512:1024]), r=[src], w=[st6])
        S.dve(lambda e: e.bn_aggr(mv[:], st6[:]), r=[st6], w=[mv])
        S.dve(lambda e: e.tensor_scalar_add(rstd[:], mv[:, 1:2], eps), r=[mv], w=[rstd])
        S.pool(lambda e: e.tensor_tensor(rstd[:], rstd[:], mhalf[:], ALU.pow), r=[rstd, mhalf], w=[rstd])

    def ln_apply(dst_ap, src_ap, r, w):
        S.act(lambda e: e.activation(dst_ap, src_ap, AF.Identity, bias=nbias[:, 0:1], scale=rstd[:, 0:1]),
              r=list(r) + [rstd, nbias], w=w)

    def transposes8(src, dst, k):
        sv = src[:].rearrange("t (p k) -> t k p", k=k)
        for b0 in range(0, k, 8):
            pt = ps()
            ptv = psb(pt).rearrange("p (g t) -> p g t", g=8)
            for kk in range(8):
                S.tr(ptv[:, kk, :], sv[:, b0 + kk, :], ident[:], r=[src, ident], w=[pt])
            yield b0, pt, ptv

    xstate = {"loaded": False}
    pending = []

    def chunk(ci, xsrc, mode, last_prev=False, next_src=None, part="all", hoist=None):
        full = (mode == "M")
        xt = xts[ci % 2]
        nxb = 8 if (full or last_prev) else 6
        if part != "rest":
            chunk_head(ci, xsrc, full, next_src, xt, nxb)
        if part == "head":
            return
        chunk_rest(ci, mode, full, xt, nxb, hoist)

    def chunk_head(ci, xsrc, full, next_src, xt, nxb):
        if not xstate["loaded"]:
            S.dma("sp", xt[:], xsrc, w=[xt])
        xstate["loaded"] = False
        layer_norm_stats(xt, LN_EPS)
        S.dve(lambda e: e.tensor_scalar(xn[:], xt[:], mv[:, 0:1], rstd[:, 0:1], ALU.subtract, ALU.mult),
              r=[xt, mv, rstd], w=[xn])
        for b0, pt, ptv in transposes8(xn, mT, 8):
            S.dve(lambda e, ptv=ptv: e.tensor_tensor(mT[:], ptv, sc1_pk[:].unsqueeze(2).to_broadcast([128, 8, 128]),
                                                    ALU.mult), r=[pt, sc1_pk], w=[mT])
            S.dve(lambda e: e.tensor_tensor(mT[:], mT[:], sh1_pk[:].unsqueeze(2).to_broadcast([128, 8, 128]),
                                            ALU.add), r=[mT, sh1_pk], w=[mT])
        for i in range(nxb):
            rt, wv_ = wget("xbc%d" % i)
            pb_ = ps()
            for sub in range(4):
                S.mm(pb_[:, sub * 128:(sub + 1) * 128],
                     [(wv_[:, k, sub * 128:(sub + 1) * 128], mT[:, k, :]) for k in range(8)],
                     r=[rt, mT], w=[pb_])
            S.act(lambda e, i=i, pb_=pb_: e.copy(xBCT[:, 4 * i:4 * i + 4, 3:131],
                                                pb_[:].rearrange("p (j t) -> p j t", j=4)),
                  r=[pb_], w=[xBCT.s(i)])

    def chunk_rest(ci, mode, full, xt, nxb, hoist):
        if full and pending:
            pending.pop()()
        for i in range(nxb):
            pc = ps()
            for sub in range(4):
                j = 4 * i + sub
                S.mm(pc[:, sub * 128:(sub + 1) * 128],
                     [(Dg[:, j, k, :], xBCT[:, j, k:k + 128]) for k in range(4)] +
                     [(brow[0:1, j * 128:(j + 1) * 128], onesb[0:1, :])],
                     r=[Dg, xBCT.s(i), brow, onesb], w=[pc])
            th = tnh[i % 2]
            S.act(lambda e, th=th, pc=pc: e.activation(th[:], pc[:], AF.Tanh), r=[pc], w=[th])
            S.dve(lambda e, i=i, th=th, pc=pc: e.scalar_tensor_tensor(
                xc[:, 4 * i:4 * i + 4, :].rearrange("p j t -> p (j t)"), th[:], 1.0, pc[:], ALU.add, ALU.mult),
                r=[th, pc], w=[xc.s(i)])
        S.pool(lambda e: e.tensor_copy(xBCT[:, :, 0:3], xBCT[:, :, 128:131]),
               r=[xBCT] + xBCT.subs, w=[xBCT] + xBCT.subs)
        for i in range(6):
            pt = ps()
            ptv = psb(pt)[:, 0:512]
            for sub in range(4):
                j = 4 * i + sub
                S.tr(ptv[:, sub * 128:(sub + 1) * 128], xc[:, j, :], ident[:], r=[xc.s(i), ident], w=[pt])
            if i < 4:
                S.act(lambda e, i=i, ptv=ptv: e.copy(x_tm[:, i * 512:(i + 1) * 512], ptv), r=[pt], w=[x_tm])
            else:
                S.act(lambda e, i=i, ptv=ptv: e.copy(B_tm[:, (i - 4) * 512:(i - 3) * 512], ptv), r=[pt], w=[B_tm])
        def tap(name, src, n=D, rr=()):
            if debug == name and full:
                S.dve(lambda e: e.tensor_copy(h1[:, 0:n], src), r=list(rr), w=[h1])
                S.dma("sp", dbg[ci * 128:(ci + 1) * 128, :], h1[:], r=[h1])
        tap("x_tm", x_tm[:, 0:D], rr=[x_tm])
        tap("B_tm", B_tm[:, 0:D], rr=[B_tm])
        rt, wv_ = wget("dt")
        pd = ps()
        S.mm(pd[:, 0:32], [(mT[:, k, :], wv_[:, k, :]) for k in range(8)], r=[rt, mT], w=[pd])
        if not full and hoist is not None:
            pd_id = banks.index(pd)
            pinned.add(pd_id)
            hoist()
            pinned.discard(pd_id)
        S.dve(lambda e: e.tensor_tensor(dtx[:], pd[:, 0:32], dtb_row[:], ALU.add), r=[pd, dtb_row], w=[dtx])
        S.act(lambda e: e.activation(dtx[:], dtx[:], AF.Exp), r=[dtx], w=[dtx])
        S.act(lambda e: e.activation(dtt[:], dtx[:], AF.Ln, bias=1.0), r=[dtx], w=[dtt])
        S.dve(lambda e: e.tensor_tensor(adt[:], dtt[:], a_row[:], ALU.mult), r=[dtt, a_row], w=[adt])
        S.dve(lambda e: e.tensor_copy(adt_hi[:], adt[:]), r=[adt], w=[adt_hi])
        S.dve(lambda e: e.tensor_tensor(adt_r[:], adt[:], adt_hi[:], ALU.subtract), r=[adt, adt_hi], w=[adt_r])
        S.dve(lambda e: e.tensor_copy(adt_lo[:], adt_r[:]), r=[adt_r], w=[adt_lo])
        pa_ = ps()
        S.mm(pa_[:, 0:32], [(Ub[:], adt_hi[:]), (Ub[:], adt_lo[:])], r=[Ub, adt_hi, adt_lo], w=[pa_])
        S.mm(pa_[:, 32:64], [(onesb[:], adt_hi[:]), (onesb[:], adt_lo[:])], r=[onesb, adt_hi, adt_lo], w=[pa_])
        S.act(lambda e: e.copy(acs[:], pa_[:, 0:32]), r=[pa_], w=[acs])
        S.act(lambda e: e.activation(ea[:], pa_[:, 0:32], AF.Exp), r=[pa_], w=[ea])
        S.act(lambda e: e.activation(dec[:], pa_[:, 32:64], AF.Exp), r=[pa_], w=[dec])
        S.dve(lambda e: e.tensor_tensor(eend[:], pa_[:, 32:64], acs[:], ALU.subtract), r=[pa_, acs], w=[eend])
        S.act(lambda e: e.activation(eend[:], eend[:], AF.Exp), r=[eend], w=[eend])
        x3 = x_tm[:].rearrange("p (h q) -> p h q", q=64)
        S.dve(lambda e: e.tensor_tensor(xdt[:].rearrange("p (h q) -> p h q", q=64), x3,
                                        dtt[:].unsqueeze(2).to_broadcast([128, 32, 64]), ALU.mult),
              r=[x_tm, dtt], w=[xdt])
        if full:
            S.pool(lambda e: e.tensor_tensor(xd_tm[:].rearrange("p (h q) -> p h q", q=64), x3,
                                             dsk32[:].unsqueeze(2).to_broadcast([128, 32, 64]), ALU.mult),
                   r=[x_tm, dsk32], w=[xd_tm])
        S.pool(lambda e: e.tensor_tensor(xdte[:].rearrange("p (h q) -> p h q", q=64),
                                        xdt[:].rearrange("p (h q) -> p h q", q=64),
                                        eend[:].unsqueeze(2).to_broadcast([128, 32, 64]), ALU.mult),
              r=[xdt, eend], w=[xdte])
        if full:
            def zblk(i):
                rt, wv_ = wget("z%d" % i)
                pz = ps()
                S.mm(pz[:], [(mT[:, k, :], wv_[:, k, :]) for k in range(8)], r=[rt, mT], w=[pz])
                th = tnh[i % 2]
                S.act(lambda e: e.activation(th[:], pz[:], AF.Tanh, scale=0.5), r=[pz], w=[th])
                S.dve(lambda e: e.scalar_tensor_tensor(zg[:, i * 512:(i + 1) * 512], th[:], 1.0, pz[:],
                                                       ALU.add, ALU.mult), r=[th, pz], w=[zg.s(i)])

            def uvblk(i):
                rt, wv_ = wget("uv%d" % i)
                pu = ps()
                S.mm(pu[:], [(mT[:, k, :], wv_[:, k, :]) for k in range(8)], r=[rt, mT], w=[pu])
                th = tnh[i % 2]
                mt = mtmp[i % 2]
                S.act(lambda e: e.activation(th[:], pu[:], AF.Square), r=[pu], w=[th])
                S.dve(lambda e: e.tensor_scalar(th[:], th[:], 0.044715 * GC, GC, ALU.mult, ALU.add), r=[th], w=[th])
                S.dve(lambda e: e.tensor_tensor(mt[:], th[:], pu[:], ALU.mult), r=[th, pu], w=[mt])
                S.act(lambda e: e.activation(mt[:], mt[:], AF.Tanh), r=[mt], w=[mt])
                if i < 2:
                    S.dve(lambda e: e.scalar_tensor_tensor(u_t[:, i * 512:(i + 1) * 512], mt[:], 1.0, pu[:],
                                                           ALU.add, ALU.mult), r=[mt, pu], w=[u_t.s(i)])
                else:
                    S.dve(lambda e: e.scalar_tensor_tensor(v_t[:, (i - 2) * 512:(i - 1) * 512], mt[:], 1.0, pu[:],
                                                           ALU.add, ALU.mult), r=[mt, pu], w=[v_t.s(i - 2)])

            def gblk(i):
                rt, wv_ = wget(("ga%d" % i) if i < 2 else ("gb%d" % (i - 2)))
                pg_ = ps()
                S.mm(pg_[:], [(mT[:, k, :], wv_[:, k, :]) for k in range(8)], r=[rt, mT], w=[pg_])
                dst = gA if i < 2 else gB
                hh_ = i % 2
                th = tnh[i % 2]
                S.act(lambda e: e.activation(th[:], pg_[:], AF.Tanh, scale=0.5), r=[pg_], w=[th])
                S.pool(lambda e: e.tensor_scalar(dst[:, hh_ * 512:(hh_ + 1) * 512], th[:], 1.0, 1.0,
                                                 ALU.add, ALU.mult), r=[th], w=[dst.s(hh_)])

            extra = {0: [lambda: zblk(1), lambda: uvblk(0)], 1: [lambda: uvblk(1)],
                     2: [lambda: zblk(2), lambda: uvblk(2)], 3: [lambda: uvblk(3)],
                     4: [lambda: zblk(3), lambda: gblk(0)], 5: [lambda: gblk(1)],
                     6: [lambda: gblk(2)], 7: [lambda: gblk(3)]}
            zblk(0)
            def s1(g):
                i2 = g % 2
                pcb = ps()
                S.mm(pcb[:, 0:128], [(xc[:, 16 + g, :], xc[:, 24 + g, :])], r=[xc.s(4 + g // 4), xc.s(6 + g // 4)], w=[pcb])
                S.dve(lambda e: e.tensor_tensor(cbm[i2][:], pcb[:, 0:128], Uf[:], ALU.mult), r=[pcb, Uf], w=[cbm[i2]])
                S.pool(lambda e: e.tensor_tensor(segr[i2][:], Ub[:].unsqueeze(1).to_broadcast([128, 4, 128]),
                                                 adt[:, 4 * g:4 * g + 4].unsqueeze(2).to_broadcast([128, 4, 128]),
                                                 ALU.mult), r=[Ub, adt], w=[segr[i2]])
                psg = ps()
                S.mm(psg[:], [(SLb[:], segr[i2][:].rearrange("p r l -> p (r l)"))], r=[SLb, segr[i2]], w=[psg])
                S.act(lambda e: e.activation(Eg[i2][:].rearrange("p r l -> p (r l)"), psg[:], AF.Exp), r=[psg], w=[Eg[i2]])
                S.dve(lambda e: e.tensor_tensor(WTg[i2][:], Eg[i2][:], cbm[i2][:].unsqueeze(1).to_broadcast([128, 4, 128]),
                                                ALU.mult), r=[Eg[i2], cbm[i2]], w=[WTg[i2]])

            def s2(g):
                i2 = g % 2
                py = ps()
                S.mm(py[:, 0:256], [(ident[:], xd_tm[:, g * 256:(g + 1) * 256])], r=[ident, xd_tm], w=[py], first=True, last=False)
                for r_ in range(4):
                    h = 4 * g + r_
                    S.mm(py[:, r_ * 64:(r_ + 1) * 64], [(WTg[i2][:, r_, :], xdt[:, h * 64:(h + 1) * 64])],
                         r=[WTg[i2], xdt], w=[py], first=False, last=True)
                S.mm(py[:, 256:512], [(xc[:, 24 + g, :], Sbf[:, g * 256:(g + 1) * 256])],
                     r=[xc.s(6 + g // 4), Sbf], w=[py])
                yt_ = ytmp[i2]
                S.dve(lambda e: e.tensor_tensor(
                    yt_[:].rearrange("p (r q) -> p r q", q=64), py[:, 256:512].rearrange("p (r q) -> p r q", q=64),
                    ea[:, 4 * g:4 * g + 4].unsqueeze(2).to_broadcast([128, 4, 64]), ALU.mult), r=[py, ea], w=[yt_])
                S.dve(lambda e: e.tensor_tensor(yt_[:], yt_[:], py[:, 0:256], ALU.add), r=[yt_, py], w=[yt_])
                S.dve(lambda e: e.tensor_tensor(yg[:, g * 256:(g + 1) * 256], yt_[:], zg[:, g * 256:(g + 1) * 256], ALU.mult),
                      r=[yt_, zg.s(g // 2)], w=[yg])
                S.act(lambda e: e.activation(sqj[:], yg[:, g * 256:(g + 1) * 256], AF.Square, accum_out=ssq[:, g:g + 1]),
                      r=[yg], w=[sqj, ssq])

            s1(0)
            for g in range(8):
                if g + 1 < 8:
                    s1(g + 1)
                s2(g)
                for f_ in extra[g]:
                    f_()
        if full:
            tap("yg", yg[:, 0:D], rr=[yg])
            tap("dt", dtt[:], n=32, rr=[dtt])
            tap("acs", acs[:], n=32, rr=[acs])
        for q4 in range(4):
            pst = ps()
            for gg in range(2):
                g = 2 * q4 + gg
                S.mm(pst[:, gg * 256:(gg + 1) * 256], [(B_tm[:, g * 128:(g + 1) * 128], xdte[:, g * 256:(g + 1) * 256])],
                     r=[B_tm, xdte], w=[pst])
            sl = slice(q4 * 512, (q4 + 1) * 512)
            S.dve(lambda e, sl=sl, q4=q4: e.tensor_tensor(Sst[:, sl].rearrange("p (h q) -> p h q", q=64),
                                                          Sst[:, sl].rearrange("p (h q) -> p h q", q=64),
                                                          dec[:, q4 * 8:(q4 + 1) * 8].unsqueeze(2).to_broadcast([128, 8, 64]),
                                                          ALU.mult), r=[Sst, dec], w=[Sst])
            S.dve(lambda e, sl=sl, pst=pst: e.tensor_tensor(Sst[:, sl], Sst[:, sl], pst[:], ALU.add), r=[Sst, pst], w=[Sst])
            S.act(lambda e, sl=sl: e.copy(Sbf[:, sl], Sst[:, sl]), r=[Sst], w=[Sbf])
        if not full:
            return
        S.dve(lambda e: e.tensor_reduce(rinv[:], ssq[:], AX.X, ALU.add), r=[ssq], w=[rinv])
        S.dve(lambda e: e.tensor_scalar(rinv[:], rinv[:], 1.0 / 2048.0, 4.0 * RMS_EPS, ALU.mult, ALU.add), r=[rinv], w=[rinv])
        S.pool(lambda e: e.tensor_tensor(rinv[:], rinv[:], mhalf[:], ALU.pow), r=[rinv, mhalf], w=[rinv])
        yTs = [xc.s(0), xc.s(1), xc.s(2), xc.s(3)]
        for b0, pt, ptv in transposes8(yg, None, 16):
            S.dve(lambda e, b0=b0, ptv=ptv: e.tensor_tensor(xc[:, b0:b0 + 8, :], ptv,
                                                           normw_pk[:, b0:b0 + 8].unsqueeze(2).to_broadcast([128, 8, 128]),
                                                           ALU.mult), r=[pt, normw_pk], w=yTs[b0 // 4:b0 // 4 + 2])
        S.dve(lambda e: e.bn_stats(st6[:, 0:6], v_t[:, 0:512]), r=[v_t.s(0)], w=[st6])
        S.dve(lambda e: e.bn_stats(st6[:, 6:12], v_t[:, 512:1024]), r=[v_t.s(1)], w=[st6])
        S.dve(lambda e: e.bn_aggr(mv[:], st6[:]), r=[st6], w=[mv])
        S.dve(lambda e: e.tensor_scalar_add(rstd[:], mv[:, 1:2], 4.0 * LN_EPS), r=[mv], w=[rstd])
        S.pool(lambda e: e.tensor_tensor(rstd[:], rstd[:], mhalf[:], ALU.pow), r=[rstd, mhalf], w=[rstd])
        S.dve(lambda e: e.tensor_scalar(v_t[:], v_t[:], mv[:, 0:1], rstd[:, 0:1], ALU.subtract, ALU.mult),
              r=[v_t.s(0), v_t.s(1), mv, rstd], w=[v_t.s(0), v_t.s(1)])
        S.dve(lambda e: e.tensor_tensor(v_t[:], v_t[:], lng_row[:], ALU.mult), r=[v_t.s(0), v_t.s(1), lng_row],
              w=[v_t.s(0), v_t.s(1)])
        S.dve(lambda e: e.tensor_tensor(vb[:], v_t[:], lnb_row[:], ALU.add), r=[v_t.s(0), v_t.s(1), lnb_row], w=[vb])
        for hf in range(2):
            pv = ps()
            for gg in range(4):
                g = 4 * hf + gg
                S.mm(pv[:, gg * 128:(gg + 1) * 128], [(WsT[:, g, :], vb[:, g * 128:(g + 1) * 128])], r=[WsT, vb], w=[pv])
            for gg in range(4):
                g = 4 * hf + gg
                S.dve(lambda e, g=g, gg=gg, pv=pv: e.scalar_tensor_tensor(
                    gm[:, g * 128:(g + 1) * 128], pv[:, gg * 128:(gg + 1) * 128], bsT[:, g:g + 1],
                    u_t[:, g * 128:(g + 1) * 128], ALU.add, ALU.mult), r=[pv, bsT, u_t.s(hf)], w=[gm])
        for b0, pt, ptv in transposes8(gm, gmT, 8):
            S.act(lambda e, ptv=ptv: e.copy(gmT[:], ptv), r=[pt], w=[gmT])
        pya = [ps(), ps()]
        for ch in range(2):
            for kh in range(2):
                rt, wv_ = wget("ps%d%d" % (ch, kh))
                S.mm(pya[ch][:], [(xc[:, kh * 8 + k, :], wv_[:, k, :]) for k in range(8)], r=[rt] + yTs, w=[pya[ch]],
                     first=(kh == 0), last=(kh == 1))
        pyb = [ps(), ps()]
        for ch in range(2):
            rt, wv_ = wget("pg%d" % ch)
            S.mm(pyb[ch][:], [(gmT[:, k, :], wv_[:, k, :]) for k in range(8)], r=[rt, gmT], w=[pyb[ch]])
        if debug in ("ya", "yb"):
            for ch in range(2):
                sl = slice(ch * 512, (ch + 1) * 512)
                if debug == "ya":
                    S.dve(lambda e, ch=ch, sl=sl: e.tensor_scalar(h1[:, sl], pya[ch][:], rinv[:, 0:1], None, ALU.mult),
                          r=[pya[ch], rinv], w=[h1])
                else:
                    S.dve(lambda e, ch=ch, sl=sl: e.tensor_scalar(h1[:, sl], pyb[ch][:], 0.5, None, ALU.mult),
                          r=[pyb[ch]], w=[h1])
            S.dma("sp", dbg[ci * 128:(ci + 1) * 128, :], h1[:], r=[h1])
        for ch in range(2):
            sl = slice(ch * 512, (ch + 1) * 512)
            S.dve(lambda e, ch=ch, sl=sl: e.scalar_tensor_tensor(mtmp[0][:], pya[ch][:], rinv[:, 0:1], gA[:, sl],
                                                                 ALU.mult, ALU.mult), r=[pya[ch], rinv, gA.s(ch)], w=[mtmp[0]])
            S.dve(lambda e, ch=ch, sl=sl: e.scalar_tensor_tensor(mtmp[1][:], pyb[ch][:], 0.5, gB[:, sl],
                                                                 ALU.mult, ALU.mult), r=[pyb[ch], gB.s(ch)], w=[mtmp[1]])
            S.dve(lambda e, sl=sl: e.tensor_tensor(mg[:, sl], mtmp[0][:], mtmp[1][:], ALU.add),
                  r=[mtmp[0], mtmp[1]], w=[mg])
        for b0, pt, ptv in transposes8(mg, mgT, 8):
            S.act(lambda e, ptv=ptv: e.copy(mgT[:], ptv), r=[pt], w=[mgT])
        pmx = [ps(), ps()]
        pin_ids = [banks.index(p_) for p_ in pmx]
        pinned.update(pin_ids)
        for ch in range(2):
            rt, wv_ = wget("wo%d" % ch)
            S.mm(pmx[ch][:], [(mgT[:, k, :], wv_[:, k, :]) for k in range(8)], r=[rt, mgT], w=[pmx[ch]])
        if full and hoist is not None:
            hoist()
        if debug == "mix":
            for ch in range(2):
                sl = slice(ch * 512, (ch + 1) * 512)
                S.dve(lambda e, ch=ch, sl=sl: e.tensor_scalar(h1[:, sl], pmx[ch][:], 0.5, None, ALU.mult),
                      r=[pmx[ch]], w=[h1])
            S.dma("sp", dbg[ci * 128:(ci + 1) * 128, :], h1[:], r=[h1])
        for ch in range(2):
            sl = slice(ch * 512, (ch + 1) * 512)
            S.dve(lambda e, ch=ch, sl=sl: e.tensor_tensor(mtmp[ch][:], pmx[ch][:], g1h_row[:, sl], ALU.mult),
                  r=[pmx[ch], g1h_row], w=[mtmp[ch]])
            S.dve(lambda e, ch=ch, sl=sl: e.scalar_tensor_tensor(xt[:, sl], xt[:, sl], ALPHA, mtmp[ch][:], ALU.mult, ALU.add),
                  r=[xt, mtmp[ch]], w=[xt])
        pinned.difference_update(pin_ids)
        layer_norm_stats(xt, LN_EPS)
        S.dve(lambda e: e.tensor_scalar(h1[:], xt[:], mv[:, 0:1], rstd[:, 0:1], ALU.subtract, ALU.mult),
              r=[xt, mv, rstd], w=[h1])
        S.dve(lambda e: e.tensor_tensor(h1[:], h1[:], ln1g_row[:], ALU.mult), r=[h1, ln1g_row], w=[h1])
        S.dve(lambda e: e.tensor_tensor(h1[:], h1[:], ln1b_row[:], ALU.add), r=[h1, ln1b_row], w=[h1])
        if debug == "h1":
            S.dma("sp", dbg[ci * 128:(ci + 1) * 128, :], h1[:], r=[h1])
        layer_norm_stats(h1, LN_EPS)
        S.dve(lambda e: e.tensor_scalar(xt[:], h1[:], mv[:, 0:1], rstd[:, 0:1], ALU.subtract, ALU.mult),
              r=[h1, mv, rstd], w=[xt])
        S.dve(lambda e: e.tensor_tensor(xt[:], xt[:], sc2_row[:], ALU.mult), r=[xt, sc2_row], w=[xt])
        S.dve(lambda e: e.tensor_tensor(m2[:], xt[:], sh2_row[:], ALU.add), r=[xt, sh2_row], w=[m2])
        def tail():
            for b0, pt, ptv in transposes8(m2, m2T, 8):
                S.act(lambda e, ptv=ptv: e.copy(m2T[:], ptv), r=[pt], w=[m2T])
            rt, wv_ = wget("rt")
            plg = ps()
            S.mm(plg[:, 0:256], [(m2T[:, k, :], wv_[:, k, :]) for k in range(8)], r=[rt, m2T], w=[plg])
            S.act(lambda e: e.activation(sco[:], plg[:, 0:256], AF.Tanh, scale=0.5), r=[plg], w=[sco])
            rt, wv_ = wget("sgu")
            phs = ps()
            for blk in range(4):
                S.mm(phs[:, blk * 128:(blk + 1) * 128], [(wv_[:, k, blk * 128:(blk + 1) * 128], m2T[:, k, :]) for k in range(8)],
                     r=[rt, m2T], w=[phs])
            S.act(lambda e: e.activation(hsa[:], phs[:, 0:256], AF.Tanh, scale=0.5), r=[phs], w=[hsa])
            S.dve(lambda e: e.scalar_tensor_tensor(hsa[:], hsa[:], 1.0, phs[:, 0:256], ALU.add, ALU.mult), r=[hsa, phs], w=[hsa])
            S.dve(lambda e: e.tensor_tensor(hs[:].rearrange("p b t -> p (b t)"), hsa[:], phs[:, 256:512], ALU.mult),
                  r=[hsa, phs], w=[hs])
            rt, wv_ = wget("sd")
            psd = [ps(), ps()]
            for ch in range(2):
                S.mm(psd[ch][:], [(hs[:, b, :], wv_[:, b, ch * 512:(ch + 1) * 512]) for b in range(2)], r=[rt, hs], w=[psd[ch]])
            for ch in range(2):
                sl = slice(ch * 512, (ch + 1) * 512)
                S.dve(lambda e, ch=ch, sl=sl: e.tensor_tensor(mtmp[ch][:], psd[ch][:], g2h_row[:, sl], ALU.mult),
                      r=[psd[ch], g2h_row], w=[mtmp[ch]])
                S.dve(lambda e, ch=ch, sl=sl: e.scalar_tensor_tensor(v_t[:, sl], h1[:, sl], ALPHA, mtmp[ch][:], ALU.mult, ALU.add),
                      r=[h1, mtmp[ch]], w=[v_t.s(ch)])
            S.dma(os.environ.get("K_STQ", "pool"), res2_d[ci * 128:(ci + 1) * 128, :], v_t[:], r=[v_t.s(0), v_t.s(1)], w=[res2_b[ci]])
            S.dve(lambda e: e.tensor_scalar(sco[:], sco[:], 0.5, 0.5, ALU.mult, ALU.add), r=[sco], w=[sco])
            S.dve(lambda e: e.tensor_tensor(cho[:], sco[:], rb_row[:], ALU.add), r=[sco, rb_row], w=[cho])
            for g in range(8):
                S.dve(lambda e, g=g: e.max(g8[:, g, :], cho[:, g * 32:(g + 1) * 32]), r=[cho], w=[g8])
            S.dve(lambda e: e.tensor_tensor(gs[:], g8[:, :, 0], g8[:, :, 1], ALU.add), r=[g8], w=[gs])
            S.dve(lambda e: e.max(gs8[:], gs[:]), r=[gs], w=[gs8])
            S.dve(lambda e: e.tensor_scalar(gpen[:], gs[:], gs8[:, 3:4], None, ALU.is_ge), r=[gs, gs8], w=[gpen])
            S.dve(lambda e: e.tensor_scalar(gpen[:], gpen[:], -1.0, BIG, ALU.add, ALU.mult), r=[gpen], w=[gpen])
            S.dve(lambda e: e.tensor_tensor(cho[:].rearrange("p (g q) -> p g q", q=32), cho[:].rearrange("p (g q) -> p g q", q=32),
                                            gpen[:].unsqueeze(2).to_broadcast([128, 8, 32]), ALU.add), r=[cho, gpen], w=[cho])
            S.dve(lambda e: e.max(top8[:], cho[:]), r=[cho], w=[top8])
            S.dve(lambda e: e.tensor_scalar(selb[:], cho[:], top8[:, 7:8], None, ALU.is_ge), r=[cho, top8], w=[selb])
            S.dve(lambda e: e.tensor_tensor(cho[:], sco[:], selb[:], ALU.mult), r=[sco, selb], w=[cho])
            S.dve(lambda e: e.max(sk8[:], cho[:]), r=[cho], w=[sk8])
            S.dve(lambda e: e.max_index(idx8[:], sk8[:], cho[:]), r=[sk8, cho], w=[idx8])
            S.dve(lambda e: e.tensor_copy(idxf[:], idx8[:]), r=[idx8], w=[idxf])
            ppos = ps()
            S.mm(ppos[:, 0:256], [(SUb[:], selb[:]), (onesb[:], Rcnt[:])], r=[SUb, selb, onesb, Rcnt], w=[ppos])
            S.act(lambda e: e.copy(posf[:], ppos[:, 0:256]), r=[ppos], w=[posf])
            S.pool(lambda e: e.tensor_tensor(Rcnt[:], Rcnt[:], selb[:], ALU.add), r=[Rcnt, selb], w=[Rcnt])
            for k in range(8):
                S.dve(lambda e, k=k: e.scalar_tensor_tensor(junk[:], iota_e[:], idxf[:, k:k + 1], posf[:], ALU.is_equal, ALU.mult,
                                                            accum_out=pk8[:, k:k + 1]), r=[iota_e, idxf, posf], w=[junk, pk8])
            S.dve(lambda e: e.tensor_copy(idx_all[:, ci, :], idxf[:]), r=[idxf], w=[idx_all])
            S.dve(lambda e: e.tensor_copy(pos_all[:, ci, :], pk8[:]), r=[pk8], w=[pos_all])
            S.dve(lambda e: e.tensor_reduce(ssum[:], sk8[:], AX.X, ALU.add), r=[sk8], w=[ssum])
            S.dve(lambda e: e.tensor_scalar_add(ssum[:], ssum[:], 1e-20), r=[ssum], w=[ssum])
            S.dve(lambda e: e.reciprocal(ssum[:], ssum[:]), r=[ssum], w=[ssum])
            S.dve(lambda e: e.tensor_scalar(wk_t[:, ci, :], sk8[:], ssum[:, 0:1], 1.25, ALU.mult, ALU.mult), r=[sk8, ssum], w=[wk_t])
            S.dma(os.environ.get("K_STQ", "pool"), m2_d[ci * 128:(ci + 1) * 128, :], m2[:], r=[m2], w=[m2_b[ci]])
        pending.append(tail)

    def m_head(j):
        chunk(j, x_cur[j * 128:(j + 1) * 128, :], "M", part="head")

    def a_head(j):
        chunk(j, x_prev[j * 128:(j + 1) * 128, :], "A", last_prev=(j == NPV - 1), part="head")

    if NPV > 0:
        a_head(0)
        for i in range(NPV):
            hz = (lambda j=i + 1: a_head(j)) if i + 1 < NPV else (lambda: m_head(0))
            chunk(i, None, "A", last_prev=(i == NPV - 1), part="rest", hoist=hz)
        S.dve(lambda e: e.tensor_scalar_mul(Sst[:], Sst[:], flag_t[:, 0:1]), r=[Sst, flag_t], w=[Sst])
        S.dve(lambda e: e.tensor_scalar_mul(Sbf[:], Sbf[:], flag_t[:, 0:1]), r=[Sbf, flag_t], w=[Sbf])
        S.dve(lambda e: e.tensor_scalar_mul(xBCT[:, :, 0:3], xBCT[:, :, 0:3], flag_t[:, 0:1]),
              r=[xBCT, flag_t] + xBCT.subs, w=[xBCT] + xBCT.subs)
    else:
        m_head(0)
    for i in range(NCH):
        hz = (lambda j=i + 1: m_head(j)) if i + 1 < NCH else None
        chunk(i, None, "M", part="rest", hoist=hz)
    pending.pop()()

    S.barrier()
    ph1.close()
    ph2 = ExitStack()
    scope[0] = ph2
    I32 = mybir.dt.int32
    cntc = sb("cntc", [128, 2], F32)
    ci32 = sb("ci32", [128, 2], I32)
    padc = sb("padc", [128, 2], F32)
    padb = sb("padb", [128, 2], BF16)
    pendc = sb("pendc", [128, 2], F32)
    pstc = sb("pstc", [128, 2], F32)
    tot = sb("tot", [128, 1], F32)
    onesf = sb("onesf", [128, 128], F32)
    dgf = sb("dgf", [128, 128], F32)
    PSrow = sb("PSrow", [128, 256], F32)
    iob = sb("iob", [128, NEB], F32)
    iop = sb("iop", [128, 1], F32)
    cmpb = [sb("cmpb%d" % i, [128, NEB], BF16) for i in range(2)]
    BEf = sb("BEf", [128, NEB], F32)
    usedf = sb("usedf", [128, NEB], F32)
    IDXW = sb("IDXW", [128, NEB], U32)
    psk = sb("psk", [128, 8], F32)
    junk2 = sb("junk2", [128, 256], F32)
    m2r = [sb("m2r%d" % i, [128, D], BF16) for i in range(2)]
    S.pool(lambda e: e.memset(onesf[:], 1.0), w=[onesf])
    S.pool(lambda e: e.iota(iob[:], pattern=[[128, NEB]], base=0, channel_multiplier=0,
                            allow_small_or_imprecise_dtypes=True), w=[iob])
    S.pool(lambda e: e.iota(iop[:], pattern=[[0, 1]], base=0, channel_multiplier=1,
                            allow_small_or_imprecise_dtypes=True), w=[iop])
    pc_ = ps()
    for h in range(2):
        S.mm(pc_[:, h:h + 1], [(Rcnt[:, h * 128:(h + 1) * 128], onesb[:, 0:1])], r=[Rcnt, onesb], w=[pc_])
    S.dve(lambda e: e.tensor_copy(cntc[:], pc_[:, 0:2]), r=[pc_], w=[cntc])
    S.dve(lambda e: e.tensor_scalar_add(ci32[:], cntc[:], 127.0), r=[cntc], w=[ci32])
    S.dve(lambda e: e.tensor_scalar(ci32[:], ci32[:], 7, 7, ALU.arith_shift_right, ALU.logical_shift_left), r=[ci32], w=[ci32])
    S.dve(lambda e: e.tensor_copy(padc[:], ci32[:]), r=[ci32], w=[padc])
    S.dve(lambda e: e.tensor_copy(padb[:], padc[:]), r=[padc], w=[padb])
    pq = ps()
    S.mm(pq[:, 0:1], [(Ub[:], padb[:, 0:1])], r=[Ub, padb], w=[pq])
    S.mm(pq[:, 1:2], [(Ub[:], padb[:, 1:2]), (onesb[:], padb[:, 0:1])], r=[Ub, onesb, padb], w=[pq])
    S.mm(pq[:, 2:3], [(onesb[:], padb[:, 0:1]), (onesb[:], padb[:, 1:2])], r=[onesb, padb], w=[pq])
    S.dve(lambda e: e.tensor_copy(pendc[:], pq[:, 0:2]), r=[pq], w=[pendc])
    S.dve(lambda e: e.tensor_copy(tot[:], pq[:, 2:3]), r=[pq], w=[tot])
    S.dve(lambda e: e.tensor_tensor(pstc[:], pendc[:], padc[:], ALU.subtract), r=[pendc, padc], w=[pstc])
    pr_ = ps()
    for h in range(2):
        S.dve(lambda e, h=h: e.tensor_scalar(dgf[:], identf[:], pstc[:, h:h + 1], None, ALU.mult), r=[identf, pstc], w=[dgf])
        S.mm(pr_[:, h * 128:(h + 1) * 128], [(onesf[:], dgf[:])], r=[onesf, dgf], w=[pr_])
    S.dve(lambda e: e.tensor_copy(PSrow[:], pr_[:, 0:256]), r=[pr_], w=[PSrow])
    pbe = ps()
    for h in range(2):
        S.dve(lambda e, h=h: e.tensor_scalar(cmpb[h][:], iob[:], pendc[:, h:h + 1], None, ALU.is_ge), r=[iob, pendc], w=[cmpb[h]])
    S.mm(pbe[:, 0:NEB], [(onesb[:], cmpb[0][:]), (onesb[:], cmpb[1][:])], r=[onesb, cmpb[0], cmpb[1]], w=[pbe])
    S.dve(lambda e: e.tensor_scalar(BEf[:], pbe[:, 0:NEB], 255.0, 128.0, ALU.min, ALU.mult), r=[pbe], w=[BEf])
    S.dve(lambda e: e.tensor_scalar(BEf[:], BEf[:], iop[:, 0:1], None, ALU.add), r=[BEf, iop], w=[BEf])
    S.dve(lambda e: e.tensor_scalar(usedf[:], iob[:], tot[:, 0:1], 1.0e6, ALU.is_ge, ALU.mult), r=[iob, tot], w=[usedf])
    S.dve(lambda e: e.tensor_tensor(BEf[:], BEf[:], usedf[:], ALU.add), r=[BEf, usedf], w=[BEf])
    S.dve(lambda e: e.tensor_copy(IDXW[:], BEf[:]), r=[BEf], w=[IDXW])
    for ci in range(NCH):
        mr = m2r[ci % 2]
        S.dma("sp", mr[:], m2_d[ci * 128:(ci + 1) * 128, :], r=[m2_b[ci]], w=[mr])
        for k in range(8):
            S.dve(lambda e, k=k, ci=ci: e.scalar_tensor_tensor(junk2[:], iota_e[:], idx_all[:, ci, k:k + 1], PSrow[:],
                                                               ALU.is_equal, ALU.mult, accum_out=psk[:, k:k + 1]),
                  r=[iota_e, idx_all, PSrow], w=[junk2, psk])
        S.dve(lambda e, ci=ci: e.tensor_tensor(psk[:], psk[:], pos_all[:, ci, :], ALU.add), r=[psk, pos_all], w=[psk])
        S.dve(lambda e, ci=ci: e.tensor_copy(slot_u[:, ci, :], psk[:]), r=[psk], w=[slot_u])
        for k in range(8):
            S.dma("pool", None, None, r=[mr, slot_u], w=[],
                  fn=lambda e, k=k, ci=ci, mr=mr: e.indirect_dma_start(
                      out=xs_d[:, :], out_offset=bass.IndirectOffsetOnAxis(ap=slot_u[:, ci, k:k + 1], axis=0),
                      in_=mr[:, :], in_offset=None, bounds_check=reg_slot, oob_is_err=False))
    S.barrier()

    NWB = 3
    wg = [sb("wg%d" % i, [128, 8, 256], BF16) for i in range(NWB)]
    wu = [sb("wu%d" % i, [128, 8, 256], BF16) for i in range(NWB)]
    wd = [sb("wd%d" % i, [128, 2, D], BF16) for i in range(NWB)]
    xsb = [sb("xsb%d" % i, [128, D], BF16) for i in range(4)]
    xsT = [sb("xsT%d" % i, [128, 8, 128], BF16) for i in range(2)]
    hga = [sb("hga%d" % i, [128, 2, 128], F32) for i in range(2)]
    hh = [sb("hh%d" % i, [128, 2, 128], BF16) for i in range(2)]
    yo = [sb("yo%d" % i, [128, D], BF16) for i in range(3)]
    for t_ in wg + wu + wd:
        S.pool(lambda e, t_=t_: e.memset(t_[:], 0.0), w=[t_])
    weg = w_e_gate[:, :]
    weu = w_e_up[:, :]
    wed = w_e_down[:, :]
    def xs_load(b_):
        if b_ < NEB:
            S.dma("sp", xsb[b_ % 4][:], xs_d[b_ * 128:(b_ + 1) * 128, :], r=[xs_b], w=[xsb[b_ % 4]])

    xs_load(0)
    xs_load(1)
    for bk in range(NEB):
        i2 = bk % 2
        iw = bk % NWB
        xs_load(bk + 2)
        for (wt_, src) in ((wg[iw], weg), (wu[iw], weu), (wd[iw], wed)):
            S.dma("pool", None, None, r=[IDXW], w=[wt_],
                  fn=lambda e, wt_=wt_, src=src, bk=bk: e.indirect_dma_start(
                      out=wt_[:].rearrange("p a b -> p (a b)"), out_offset=None, in_=src,
                      in_offset=bass.IndirectOffsetOnAxis(ap=IDXW[:, bk:bk + 1], axis=0),
                      bounds_check=reg_w, oob_is_err=False))
        xb = xsb[bk % 4]
        sv = xb[:].rearrange("t (p k) -> t k p", k=8)
        pt = ps()
        ptv = psb(pt).rearrange("p (g t) -> p g t", g=8)
        for kk in range(8):
            S.tr(ptv[:, kk, :], sv[:, kk, :], ident[:], r=[xb, ident], w=[pt])
        S.act(lambda e, i2=i2, ptv=ptv: e.copy(xsT[i2][:], ptv), r=[pt], w=[xsT[i2]])
        pgu = ps()
        for fb in range(2):
            wgv = wg[iw][:].rearrange("p k (q b) -> p k b q", b=2)
            wuv = wu[iw][:].rearrange("p k (q b) -> p k b q", b=2)
            S.mm(pgu[:, fb * 128:(fb + 1) * 128], [(wgv[:, k, fb, :], xsT[i2][:, k, :]) for k in range(8)],
                 r=[wg[iw], xsT[i2]], w=[pgu])
            S.mm(pgu[:, 256 + fb * 128:256 + (fb + 1) * 128], [(wuv[:, k, fb, :], xsT[i2][:, k, :]) for k in range(8)],
                 r=[wu[iw], xsT[i2]], w=[pgu])
        hg_ = hga[i2][:].rearrange("p b t -> p (b t)")
        S.act(lambda e, hg_=hg_, pgu=pgu: e.activation(hg_, pgu[:, 0:256], AF.Tanh, scale=0.5), r=[pgu], w=[hga[i2]])
        S.dve(lambda e, hg_=hg_, pgu=pgu: e.scalar_tensor_tensor(hg_, hg_, 1.0, pgu[:, 0:256], ALU.add, ALU.mult),
              r=[hga[i2], pgu], w=[hga[i2]])
        S.dve(lambda e, i2=i2, hg_=hg_, pgu=pgu: e.tensor_tensor(hh[i2][:].rearrange("p b t -> p (b t)"), hg_, pgu[:, 256:512],
                                                                 ALU.mult), r=[hga[i2], pgu], w=[hh[i2]])
        yb_ = yo[bk % 3]
        for ch in range(2):
            pd_ = ps()
            S.mm(pd_[:], [(hh[i2][:, b_, :], wd[iw][:, b_, ch * 512:(ch + 1) * 512]) for b_ in range(2)],
                 r=[hh[i2], wd[iw]], w=[pd_])
            S.act(lambda e, yb_=yb_, ch=ch, pd_=pd_: e.copy(yb_[:, ch * 512:(ch + 1) * 512], pd_[:]), r=[pd_], w=[yb_])
        S.dma(os.environ.get("K_YSQ", "act"), ys_d[bk * 128:(bk + 1) * 128, :], yb_[:], r=[yb_], w=[])

    S.barrier()
    ph2.close()
    ph3 = ExitStack()
    scope[0] = ph3
    ln2g_row = sb("ln2g_row", [128, D], F32)
    ln2b_row = sb("ln2b_row", [128, D], F32)
    g2_row = sb("g2_row", [128, D], F32)
    S.dma("sp", ln2g_row[:], ln2_g.partition_broadcast(128), w=[ln2g_row])
    S.dma("sp", ln2b_row[:], ln2_b.partition_broadcast(128), w=[ln2b_row])
    S.dve(lambda e: e.tensor_scalar_mul(g2_row[:], g2h_row[:], 2.0), r=[g2h_row], w=[g2_row])
    yk = [sb("yk%d" % i, [128, D], BF16) for i in range(4)]
    facc = [sb("facc%d" % i, [128, D], F32) for i in range(2)]
    r2 = [sb("r2_%d" % i, [128, D], F32) for i in range(2)]
    for t_ in yk:
        S.pool(lambda e, t_=t_: e.memset(t_[:], 0.0), w=[t_])
    gi = 0
    for ci in range(NCH):
        i2 = ci % 2
        S.dma("sp", r2[i2][:], res2_d[ci * 128:(ci + 1) * 128, :], r=[res2_b[ci]], w=[r2[i2]])
        fa = facc[i2]
        for k in range(8):
            yt_ = yk[gi % 4]
            gi += 1
            S.dma("pool", None, None, r=[ys_b, slot_u], w=[yt_],
                  fn=lambda e, k=k, yt_=yt_: e.indirect_dma_start(
                      out=yt_[:, :], out_offset=None, in_=ys_d[:, :],
                      in_offset=bass.IndirectOffsetOnAxis(ap=slot_u[:, ci, k:k + 1], axis=0),
                      bounds_check=reg_slot, oob_is_err=False))
            if k == 0:
                S.dve(lambda e, yt_=yt_, fa=fa: e.tensor_scalar(fa[:], yt_[:], wk_t[:, ci, 0:1], None, ALU.mult),
                      r=[yt_, wk_t], w=[fa])
            else:
                S.dve(lambda e, yt_=yt_, fa=fa, k=k: e.scalar_tensor_tensor(fa[:], yt_[:], wk_t[:, ci, k:k + 1], fa[:],
                                                                          ALU.mult, ALU.add), r=[yt_, wk_t, fa], w=[fa])
        S.dve(lambda e, fa=fa: e.tensor_tensor(fa[:], fa[:], g2_row[:], ALU.mult), r=[fa, g2_row], w=[fa])
        S.dve(lambda e, fa=fa, i2=i2: e.tensor_tensor(fa[:], fa[:], r2[i2][:], ALU.add), r=[fa, r2[i2]], w=[fa])
        layer_norm_stats(fa, LN_EPS)
        S.dve(lambda e, fa=fa: e.tensor_scalar(fa[:], fa[:], mv[:, 0:1], rstd[:, 0:1], ALU.subtract, ALU.mult),
              r=[fa, mv, rstd], w=[fa])
        S.dve(lambda e, fa=fa: e.tensor_tensor(fa[:], fa[:], ln2g_row[:], ALU.mult), r=[fa, ln2g_row], w=[fa])
        S.dve(lambda e, fa=fa: e.tensor_tensor(fa[:], fa[:], ln2b_row[:], ALU.add), r=[fa, ln2b_row], w=[fa])
        S.dma("sp", out[ci * 128:(ci + 1) * 128, :], fa[:], r=[fa])
    S.finish()
    S.barrier()
    ph3.close()
    return nc, S


_NAMES = ["w_ada", "b_ada", "w_in", "conv_w", "conv_b", "dt_bias", "a_log", "d_skip", "ssd_norm_w",
          "gmlp_ln_g", "gmlp_ln_b", "gmlp_ws", "gmlp_bs", "w_proj_ssd", "w_proj_gmlp", "w_out",
          "ln1_g", "ln1_b", "w_router", "router_bias", "w_e_gate", "w_e_up", "w_e_down",
          "w_sh_gate", "w_sh_up", "w_sh_down", "ln2_g", "ln2_b"]


def make_in_maps(inputs, NCH, NPV, seq_per_core=None):
    x = np.asarray(inputs["x"], dtype=np.float32)
    c = np.asarray(inputs["c"], dtype=np.float32)
    shared = {n: np.ascontiguousarray(np.asarray(inputs[n], dtype=np.float32)[0]) for n in _NAMES}
    for n in ("w_e_gate", "w_e_up", "w_e_down"):
        shared[n] = shared[n].reshape(256 * 128, 2048)
    maps = []
    ncores = 2 * x.shape[0]
    for core in range(ncores):
        b, half = core // 2, core % 2
        cur0 = half * NPV * 128 if NPV > 0 else 0
        m = dict(shared)
        m["x_cur"] = np.ascontiguousarray(x[b, cur0:cur0 + NCH * 128, :])
        m["x_prev"] = np.ascontiguousarray(x[b, 0:max(NPV, 1) * 128, :])
        m["flag"] = np.full((128, 1), float(half), dtype=np.float32)
        m["c_b"] = np.ascontiguousarray(c[b])
        maps.append(m)
    return maps


def kernel(**inputs):
    NCH, NPV, C = 32, 32, 256
    nc, _ = build(NCH, NPV, C)
    maps = make_in_maps(inputs, NCH, NPV)
    res = run_bass_kernel_spmd(nc, maps, core_ids=list(range(8)))
    x = np.asarray(inputs["x"])
    out = np.empty(x.shape, dtype=np.float32)
    for core in range(8):
        b, half = core // 2, core % 2
        out[b, half * NCH * 128:(half + 1) * NCH * 128, :] = res.results[core]["out"]
    return out
```

```python
import os
import numpy as np
from contextlib import ExitStack
import concourse.bass as bass
import concourse.mybir as mybir
from concourse.bass_utils import run_bass_kernel_spmd

F32 = mybir.dt.float32
BF16 = mybir.dt.bfloat16
U32 = mybir.dt.uint32
AF = mybir.ActivationFunctionType
ALU = mybir.AluOpType
AX = mybir.AxisListType

D = 1024
NIN = 10272
ALPHA = float(2.0 ** 0.25)
LN_EPS = 1e-5
RMS_EPS = 1e-5
GC = 0.7978845608028654
BIG = 1.0e4


class Buf:
    __slots__ = ("name", "w", "r")

    def __init__(self, name=""):
        self.name = name
        self.w = None
        self.r = {}


class T:
    def __init__(self, h, nsub=0, name=""):
        self.h = h
        self.b = Buf(name)
        self.subs = [Buf("%s.%d" % (name, i)) for i in range(nsub)]

    def __getitem__(self, k):
        return self.h[k]

    def s(self, i):
        return self.subs[i]


def _b(t):
    return t.b if isinstance(t, T) else t


class Sched:
    NDMA = 40

    def __init__(self, nc):
        self.nc = nc
        self.eng = {"pe": nc.tensor, "act": nc.scalar, "dve": nc.vector,
                    "pool": nc.gpsimd, "sp": nc.sync}
        self.sem = {k: nc.alloc_semaphore("q_" + k) for k in self.eng}
        self.cnt = {k: 0 for k in self.eng}
        self.waited = {k: {} for k in self.eng}
        self.dsem = [nc.alloc_semaphore("d%d" % i) for i in range(self.NDMA)]
        self.duse = [0] * self.NDMA
        self.dnext = 0
        self.nins = 0

    def _wait(self, e, ev):
        sem, val = ev
        if e == "pe" and sem is self.sem["pe"]:
            return
        w = self.waited[e]
        if w.get(sem.num, 0) >= val:
            return
        w[sem.num] = val
        self.eng[e].wait_ge(sem, val)
        self.nins += 1

    def _deps(self, e, reads, writes):
        for t in reads:
            b = _b(t)
            if b.w is not None:
                self._wait(e, b.w)
        for t in writes:
            b = _b(t)
            if b.w is not None:
                self._wait(e, b.w)
            for ev in b.r.values():
                self._wait(e, ev)

    def _commit(self, ev, reads, writes):
        sem, val = ev
        for t in reads:
            b = _b(t)
            old = b.r.get(sem.num)
            if old is None or old[1] < val:
                b.r[sem.num] = ev
        for t in writes:
            b = _b(t)
            b.w = ev
            b.r = {}

    def op(self, e, fn, r=(), w=()):
        self._deps(e, r, w)
        ins = fn(self.eng[e])
        self.cnt[e] += 1
        ins.then_inc(self.sem[e], 1)
        self._commit((self.sem[e], self.cnt[e]), r, w)
        self.nins += 1
        return ins

    def dve(self, fn, r=(), w=()):
        return self.op("dve", fn, r, w)

    def act(self, fn, r=(), w=()):
        return self.op("act", fn, r, w)

    def pool(self, fn, r=(), w=()):
        return self.op("pool", fn, r, w)

    def mm(self, out, pairs, r=(), w=(), first=True, last=True):
        self._deps("pe", r, w)
        n = len(pairs)
        ins = None
        for i, (l, rh) in enumerate(pairs):
            ins = self.nc.tensor.matmul(out, l, rh, start=(first and i == 0), stop=(last and i == n - 1))
        self.cnt["pe"] += 1
        ins.then_inc(self.sem["pe"], 1)
        self._commit((self.sem["pe"], self.cnt["pe"]), r, w)
        self.nins += n

    def tr(self, out, in_, ident, r=(), w=()):
        return self.op("pe", lambda e: e.transpose(out, in_, ident), r, w)

    def dma(self, q, out, in_, r=(), w=(), fn=None):
        slot = self.dnext
        self.dnext = (self.dnext + 1) % self.NDMA
        sem = self.dsem[slot]
        if self.duse[slot] > 0:
            self._wait(q, (sem, 16 * self.duse[slot]))
        self._deps(q, r, w)
        if fn is None:
            ins = self.eng[q].dma_start(out=out, in_=in_)
        else:
            ins = fn(self.eng[q])
        ins.then_inc(sem, 16)
        self.duse[slot] += 1
        self._commit((sem, 16 * self.duse[slot]), r, w)
        self.nins += 1
        return ins

    def barrier(self):
        for e in self.eng:
            for e2 in self.eng:
                if e2 != e and self.cnt[e2] > 0:
                    self._wait(e, (self.sem[e2], self.cnt[e2]))
            for i in range(self.NDMA):
                if self.duse[i] > 0:
                    self._wait(e, (self.dsem[i], 16 * self.duse[i]))

    def finish(self):
        for i in range(self.NDMA):
            if self.duse[i] > 0:
                self._wait("sp", (self.dsem[i], 16 * self.duse[i]))


def build(NCH, NPV, C, debug=False):
    nc = bass.Bass("TRN2", target_bir_lowering=False)
    S = Sched(nc)
    NEB = NCH * 8 + 256
    NSLOT = NEB * 128

    def din(name, shape, dt=F32):
        return nc.dram_tensor(name, list(shape), dt, kind="ExternalInput").ap()

    x_cur = din("x_cur", [NCH * 128, D])
    x_prev = din("x_prev", [max(NPV, 1) * 128, D])
    flag = din("flag", [128, 1])
    c_b = din("c_b", [D])
    w_ada = din("w_ada", [D, 6 * D])
    b_ada = din("b_ada", [6 * D])
    w_in = din("w_in", [D, NIN])
    conv_w = din("conv_w", [4, 4096])
    conv_b = din("conv_b", [4096])
    dt_bias = din("dt_bias", [32])
    a_log = din("a_log", [32])
    d_skip = din("d_skip", [32])
    ssd_norm_w = din("ssd_norm_w", [2048])
    gmlp_ln_g = din("gmlp_ln_g", [D])
    gmlp_ln_b = din("gmlp_ln_b", [D])
    gmlp_ws = din("gmlp_ws", [8, 128, 128])
    gmlp_bs = din("gmlp_bs", [8, 128])
    w_proj_ssd = din("w_proj_ssd", [2048, D])
    w_proj_gmlp = din("w_proj_gmlp", [D, D])
    w_out = din("w_out", [D, D])
    ln1_g = din("ln1_g", [D])
    ln1_b = din("ln1_b", [D])
    w_router = din("w_router", [D, 256])
    router_bias = din("router_bias", [256])
    w_e_gate = din("w_e_gate", [256 * 128, 2048])
    w_e_up = din("w_e_up", [256 * 128, 2048])
    w_e_down = din("w_e_down", [256 * 128, 2048])
    w_sh_gate = din("w_sh_gate", [D, 256])
    w_sh_up = din("w_sh_up", [D, 256])
    w_sh_down = din("w_sh_down", [256, D])
    ln2_g = din("ln2_g", [D])
    ln2_b = din("ln2_b", [D])
    out = nc.dram_tensor("out", [NCH * 128, D], F32, kind="ExternalOutput").ap()
    dbg = None
    if debug:
        dbg = nc.dram_tensor("dbg", [NCH * 128, D], F32, kind="ExternalOutput").ap()

    blocks = []

    def wv(w, k):
        return w.rearrange("(p k) c -> p k c", k=k)

    win_v = wv(w_in, 8)
    for i in range(8):
        blocks.append(("xbc%d" % i, [(win_v[:, :, 2048 + 512 * i:2048 + 512 * (i + 1)], 0)], 8, 512))
    blocks.append(("dt", [(win_v[:, :, 6144:6176], 0)], 8, 32))
    for i in range(4):
        blocks.append(("z%d" % i, [(win_v[:, :, 512 * i:512 * (i + 1)], 0)], 8, 512))
    for i in range(4):
        blocks.append(("uv%d" % i, [(win_v[:, :, 6176 + 512 * i:6176 + 512 * (i + 1)], 0)], 8, 512))
    for i in range(2):
        blocks.append(("ga%d" % i, [(win_v[:, :, 8224 + 512 * i:8224 + 512 * (i + 1)], 0)], 8, 512))
    for i in range(2):
        blocks.append(("gb%d" % i, [(win_v[:, :, 9248 + 512 * i:9248 + 512 * (i + 1)], 0)], 8, 512))
    wps_v = wv(w_proj_ssd, 16)
    for ch in range(2):
        for kh in range(2):
            blocks.append(("ps%d%d" % (ch, kh), [(wps_v[:, kh * 8:(kh + 1) * 8, ch * 512:(ch + 1) * 512], 0)], 8, 512))
    wpg_v = wv(w_proj_gmlp, 8)
    for ch in range(2):
        blocks.append(("pg%d" % ch, [(wpg_v[:, :, ch * 512:(ch + 1) * 512], 0)], 8, 512))
    wo_v = wv(w_out, 8)
    for ch in range(2):
        blocks.append(("wo%d" % ch, [(wo_v[:, :, ch * 512:(ch + 1) * 512], 0)], 8, 512))
    blocks.append(("rt", [(wv(w_router, 8), 0)], 8, 256))
    blocks.append(("sgu", [(wv(w_sh_gate, 8), 0), (wv(w_sh_up, 8), 256)], 8, 512))
    blocks.append(("sd", [(w_sh_down.rearrange("(b q) c -> q b c", q=128), 0)], 2, 1024))
    bidx = {b[0]: i for i, b in enumerate(blocks)}
    NBLK = len(blocks)
    wblk = nc.dram_tensor("wblk", [NBLK, 128, 4096], BF16, kind="Internal").ap()
    wblk_b = [Buf("wblk%d" % i) for i in range(NBLK)]

    xs_d = nc.dram_tensor("xs_d", [NSLOT, D], BF16, kind="Internal").ap()
    ys_d = nc.dram_tensor("ys_d", [NSLOT, D], BF16, kind="Internal").ap()
    res2_d = nc.dram_tensor("res2_d", [NCH * 128, D], F32, kind="Internal").ap()
    m2_d = nc.dram_tensor("m2_d", [NCH * 128, D], BF16, kind="Internal").ap()
    m2_b = [Buf("m2d_%d" % i) for i in range(NCH)]
    xs_b = Buf("xs_d")
    ys_b = Buf("ys_d")
    res2_b = [Buf("res2_%d" % i) for i in range(NCH)]

    reg_slot = nc.gpsimd.to_reg(NSLOT - 1)
    reg_w = nc.gpsimd.to_reg(256 * 128 - 1)

    def sb(name, shape, dt, nsub=0):
        if scope[0] is None:
            return T(nc.alloc_sbuf_tensor(name, list(shape), dt), nsub, name)
        return T(scope[0].enter_context(nc.sbuf_tensor(name, list(shape), dt)), nsub, name)

    scope = [None]

    banks = [T(nc.alloc_psum_tensor("bank%d" % i, [128, 512], F32), 0, "bank%d" % i) for i in range(8)]
    bank_i = [0]

    pinned = set()

    def ps():
        while (bank_i[0] % 8) in pinned:
            bank_i[0] += 1
        t = banks[bank_i[0] % 8]
        bank_i[0] += 1
        return t

    def psb(t):
        return t[:].bitcast(BF16)

    ident = sb("ident", [128, 128], BF16)
    identf = sb("identf", [128, 128], F32)
    Uf = sb("Uf", [128, 128], F32)
    Ub = sb("Ub", [128, 128], BF16)
    SLb = sb("SLb", [128, 128], BF16)
    SUb = sb("SUb", [128, 128], BF16)
    onesb = sb("onesb", [128, 128], BF16)
    iota_e = sb("iota_e", [128, 256], F32)
    mhalf = sb("mhalf", [128, 1], F32)
    WsT = sb("WsT", [128, 8, 128], BF16)
    bsT = sb("bsT", [128, 8], F32)
    Dg = sb("Dg", [128, 32, 4, 128], BF16)
    brow = sb("brow", [1, 4096], BF16)
    a_row = sb("a_row", [128, 32], F32)
    dtb_row = sb("dtb_row", [128, 32], F32)
    dsk32 = sb("dsk32", [128, 32], F32)
    normw_pk = sb("normw_pk", [128, 16], F32)
    sh1_pk = sb("sh1_pk", [128, 8], F32)
    sc1_pk = sb("sc1_pk", [128, 8], F32)
    lng_row = sb("lng_row", [128, D], BF16)
    lnb_row = sb("lnb_row", [128, D], BF16)
    ln1g_row = sb("ln1g_row", [128, D], BF16)
    ln1b_row = sb("ln1b_row", [128, D], BF16)
    sc2_row = sb("sc2_row", [128, D], BF16)
    sh2_row = sb("sh2_row", [128, D], BF16)
    g1h_row = sb("g1h_row", [128, D], BF16)
    g2h_row = sb("g2h_row", [128, D], BF16)
    rb_row = sb("rb_row", [128, 256], F32)
    flag_t = sb("flag_t", [128, 1], F32)
    st6 = sb("st6", [128, 12], F32)
    mv = sb("mv", [128, 2], F32)
    rstd = sb("rstd", [128, 1], F32)
    nbias = sb("nbias", [128, 1], F32)
    slot_u = sb("slot_u", [128, NCH, 8], U32)
    wk_t = sb("wk_t", [128, NCH, 8], F32)
    Rcnt = sb("Rcnt", [128, 256], BF16)
    idx_all = sb("idx_all", [128, NCH, 8], F32)
    pos_all = sb("pos_all", [128, NCH, 8], F32)
    Sst = sb("Sst", [128, 2048], F32)
    Sbf = sb("Sbf", [128, 2048], BF16)
    xBCT = sb("xBCT", [128, 32, 131], BF16, nsub=8)

    def aff(t, pattern, cm, op, fill, r=(), extra_w=()):
        S.pool(lambda e: e.affine_select(out=t[:], in_=t[:], pattern=pattern, compare_op=op, fill=fill,
                                         base=0, channel_multiplier=cm), r=[t], w=[t])

    S.pool(lambda e: e.memset(identf[:], 0.0), w=[identf])
    aff(identf, [[1, 128]], -1, ALU.not_equal, 1.0)
    S.dve(lambda e: e.tensor_copy(ident[:], identf[:]), r=[identf], w=[ident])
    S.pool(lambda e: e.memset(Uf[:], 1.0), w=[Uf])
    aff(Uf, [[1, 128]], -1, ALU.is_ge, 0.0)
    S.dve(lambda e: e.tensor_copy(Ub[:], Uf[:]), r=[Uf], w=[Ub])
    tmpf = sb("tmpf", [128, 128], F32)
    S.pool(lambda e: e.memset(tmpf[:], 1.0), w=[tmpf])
    aff(tmpf, [[-1, 128]], 1, ALU.is_gt, 0.0)
    S.dve(lambda e: e.tensor_copy(SLb[:], tmpf[:]), r=[tmpf], w=[SLb])
    S.pool(lambda e: e.memset(tmpf[:], 1.0), r=[], w=[tmpf])
    aff(tmpf, [[1, 128]], -1, ALU.is_gt, 0.0)
    S.dve(lambda e: e.tensor_copy(SUb[:], tmpf[:]), r=[tmpf], w=[SUb])
    S.pool(lambda e: e.memset(onesb[:], 1.0), w=[onesb])
    S.pool(lambda e: e.iota(iota_e[:], pattern=[[1, 256]], base=0, channel_multiplier=0,
                            allow_small_or_imprecise_dtypes=True), w=[iota_e])
    S.pool(lambda e: e.memset(mhalf[:], -0.5), w=[mhalf])
    S.pool(lambda e: e.memset(Sst[:], 0.0), w=[Sst])
    S.pool(lambda e: e.memset(Sbf[:], 0.0), w=[Sbf])
    S.pool(lambda e: e.memset(xBCT[:], 0.0), w=[xBCT] + xBCT.subs)
    S.pool(lambda e: e.memset(Rcnt[:], 0.0), w=[Rcnt])
    S.pool(lambda e: e.memset(wk_t[:], 0.0), w=[wk_t])

    S.dma("sp", flag_t[:], flag, w=[flag_t])
    for row, src in ((lng_row, gmlp_ln_g), (lnb_row, gmlp_ln_b), (ln1g_row, ln1_g), (ln1b_row, ln1_b)):
        S.dma("pool", row[:], src.partition_broadcast(128), w=[row])
    S.dma("sp", rb_row[:], router_bias.partition_broadcast(128), w=[rb_row])
    S.dma("sp", dtb_row[:], dt_bias.partition_broadcast(128), w=[dtb_row])
    S.dma("sp", a_row[:], a_log.partition_broadcast(128), w=[a_row])
    S.dma("sp", normw_pk[:], ssd_norm_w.rearrange("(p k) -> p k", k=16), w=[normw_pk])
    S.act(lambda e: e.activation(a_row[:], a_row[:], AF.Exp), r=[a_row], w=[a_row])
    S.dve(lambda e: e.tensor_scalar_mul(a_row[:], a_row[:], -1.0), r=[a_row], w=[a_row])

    with ExitStack() as st1:
        scope[0] = st1
        stg = [sb("stg%d" % i, [128, 4096], BF16) for i in range(3)]
        cw4 = sb("cw4", [4, 4096], F32)
        cb1 = sb("cb1", [1, 4096], F32)
        wsl = sb("wsl", [128, 8, 128], F32)
        wslb = sb("wslb", [128, 8, 128], BF16)
        bs8 = sb("bs8", [8, 128], F32)
        wcol = sb("wcol", [128, 32, 4], F32)

        S.pool(lambda e: e.memset(stg[2][:], 0.0), w=[stg[2]])
        xs_z = xs_d.rearrange("(b p i) d -> b p (i d)", p=128, i=4)
        for b in range(NSLOT // 512):
            S.dma("sp", xs_z[b], stg[2][:], r=[stg[2]], w=[xs_b])

        for i, (name, srcs, K, N) in enumerate(blocks):
            st = stg[i % 2]
            v = st[:, 0:K * N].rearrange("p (k n) -> p k n", k=K)
            for (src, co) in srcs:
                n = src.shape[2]
                S.dma("pool", v[:, :, co:co + n], src, w=[st])
            S.dma("sp", wblk[i, :, 0:K * N], st[:, 0:K * N], r=[st], w=[wblk_b[i]])

        S.dma("sp", dsk32[:], d_skip.partition_broadcast(128), w=[dsk32])
        S.dma("sp", cw4[:], conv_w, w=[cw4])
        pw = ps()
        pwv = pw[:, 0:128].rearrange("p (j k) -> p j k", k=4)
        for j in range(32):
            S.mm(pwv[:, j, :], [(cw4[0:4, j * 128:(j + 1) * 128], identf[0:4, 0:4])], r=[cw4, identf], w=[pw])
        S.dve(lambda e: e.tensor_scalar_mul(wcol[:], pwv, 0.5), r=[pw], w=[wcol])
        for j in range(32):
            for k in range(4):
                eng = "dve" if (j + k) % 2 == 0 else "pool"
                S.op(eng, lambda e, j=j, k=k: e.tensor_scalar(Dg[:, j, k, :], identf[:], wcol[:, j, k:k + 1], None,
                                                              ALU.mult), r=[identf, wcol], w=[Dg])
        S.dma("sp", cb1[:], conv_b.rearrange("(o c) -> o c", o=1), w=[cb1])
        S.dve(lambda e: e.tensor_scalar_mul(brow[:], cb1[:], 0.5), r=[cb1], w=[brow])
        S.dma("sp", wsl[:], gmlp_ws.rearrange("g t s -> t g s"), w=[wsl])
        S.pool(lambda e: e.affine_select(out=wsl[:], in_=wsl[:], pattern=[[0, 8], [-1, 128]], compare_op=ALU.is_ge,
                                         fill=0.0, base=0, channel_multiplier=1), r=[wsl], w=[wsl])
        S.dve(lambda e: e.tensor_copy(wslb[:], wsl[:]), r=[wsl], w=[wslb])
        pt = ps()
        ptv = psb(pt).rearrange("p (g t) -> p g t", g=8)
        for g in range(8):
            S.tr(ptv[:, g, :], wslb[:, g, :], ident[:], r=[wslb, ident], w=[pt])
        S.dve(lambda e: e.tensor_copy(WsT[:], ptv), r=[pt], w=[WsT])
        S.dma("sp", bs8[:], gmlp_bs, w=[bs8])
        pb = ps()
        S.mm(pb[:, 0:8], [(bs8[0:8, :], identf[0:8, 0:8])], r=[bs8, identf], w=[pb])
        S.dve(lambda e: e.tensor_copy(bsT[:], pb[:, 0:8]), r=[pb], w=[bsT])

        S.barrier()
    with ExitStack() as st2:
        scope[0] = st2
        wa = [sb("wa%d" % i, [128, 8, 512], F32) for i in range(2)]
        bad = [sb("bad%d" % i, [128, 512], F32) for i in range(2)]
        adarow = sb("adarow", [128, 6 * D], F32)
        screp = sb("screp", [128, 8, 128], F32)
        dtmp = sb("dtmp", [128, 128, 8], F32)
        cpk = sb("cpk", [128, 8], F32)
        cpk2 = sb("cpk2", [128, 8], F32)
        S.dma("sp", cpk[:], c_b.rearrange("(p k) -> p k", k=8), w=[cpk])
        S.act(lambda e: e.activation(cpk2[:], cpk[:], AF.Tanh, scale=0.5), r=[cpk], w=[cpk2])
        S.dve(lambda e: e.scalar_tensor_tensor(cpk2[:], cpk2[:], 1.0, cpk[:], ALU.add, ALU.mult), r=[cpk2, cpk], w=[cpk2])
        S.dve(lambda e: e.tensor_scalar_mul(cpk2[:], cpk2[:], 0.5), r=[cpk2], w=[cpk2])
        S.dve(lambda e: e.tensor_copy(screp[:], cpk2[:].unsqueeze(2).to_broadcast([128, 8, 128])), r=[cpk2], w=[screp])
        wada_v = wv(w_ada, 8)
        for b in range(12):
            wt = wa[b % 2]
            S.dma("sp", wt[:], wada_v[:, :, b * 512:(b + 1) * 512], w=[wt])
            pa = ps()
            S.mm(pa[:], [(screp[:, k, :], wt[:, k, :]) for k in range(8)], r=[screp, wt], w=[pa])
            bd = bad[b % 2]
            S.dma("sp", bd[:], b_ada[b * 512:(b + 1) * 512].partition_broadcast(128), w=[bd])
            S.dve(lambda e, b=b, pa=pa, bd=bd: e.tensor_tensor(adarow[:, b * 512:(b + 1) * 512], pa[:], bd[:], ALU.add),
                  r=[pa, bd], w=[adarow])
        S.dve(lambda e: e.tensor_copy(sh2_row[:], adarow[:, 3 * D:4 * D]), r=[adarow], w=[sh2_row])
        S.dve(lambda e: e.tensor_scalar_add(sc2_row[:], adarow[:, 4 * D:5 * D], 1.0), r=[adarow], w=[sc2_row])
        S.dve(lambda e: e.tensor_scalar_mul(g1h_row[:], adarow[:, 2 * D:3 * D], 0.5), r=[adarow], w=[g1h_row])
        S.dve(lambda e: e.tensor_scalar_mul(g2h_row[:], adarow[:, 5 * D:6 * D], 0.5), r=[adarow], w=[g2h_row])
        for (dst, off, add1) in ((sh1_pk, 0, 0.0), (sc1_pk, D, 1.0)):
            S.dve(lambda e, off=off: e.tensor_tensor(dtmp[:], adarow[:, off:off + D].rearrange("p (q k) -> p q k", k=8),
                                                    identf[:].unsqueeze(2).to_broadcast([128, 128, 8]), ALU.mult),
                  r=[adarow, identf], w=[dtmp])
            S.dve(lambda e, dst=dst: e.tensor_reduce(dst[:], dtmp[:].rearrange("p q k -> p k q"), AX.X, ALU.add),
                  r=[dtmp], w=[dst])
            if add1:
                S.dve(lambda e, dst=dst: e.tensor_scalar_add(dst[:], dst[:], 1.0), r=[dst], w=[dst])
        S.barrier()
    scope[0] = None

    ph1 = ExitStack()
    scope[0] = ph1
    NB = 3
    ring = [sb("ring%d" % i, [128, 4096], BF16) for i in range(NB)]
    worder = []
    wstate = {"issued": 0, "next": 0}

    def chunk_blocks(kind, last=False):
        if kind == "A":
            l = ["xbc0", "xbc1", "xbc2", "xbc3", "xbc4", "xbc5"]
            if last:
                l += ["xbc6", "xbc7"]
            return l + ["dt"]
        return (["xbc%d" % i for i in range(8)] + (["rt", "sgu", "sd"] if not last else []) + ["dt"] +
                ["z0", "z1", "uv0", "uv1", "z2", "uv2", "uv3", "z3", "ga0", "ga1", "gb0", "gb1"] +
                ["pg0", "pg1", "ps00", "ps01", "ps10", "ps11", "wo0", "wo1"])

    for i in range(NPV):
        worder.extend(chunk_blocks("A", i == NPV - 1))
    worder.extend(chunk_blocks("M", last=True))
    for i in range(1, NCH):
        worder.extend(chunk_blocks("M", last=False))
    worder.extend(["rt", "sgu", "sd"])

    def wget(name):
        i = wstate["next"]
        assert worder[i] == name, (worder[i], name)
        while wstate["issued"] < min(len(worder), i + NB):
            j = wstate["issued"]
            bi = bidx[worder[j]]
            K, N = blocks[bi][2], blocks[bi][3]
            rt = ring[j % NB]
            S.dma("sp", rt[:, 0:K * N], wblk[bi, :, 0:K * N], r=[wblk_b[bi]], w=[rt])
            wstate["issued"] += 1
        wstate["next"] += 1
        rt = ring[i % NB]
        bi = bidx[name]
        K, N = blocks[bi][2], blocks[bi][3]
        return rt, rt[:, 0:K * N].rearrange("p (k n) -> p k n", k=K)

    xts = [sb("xt0", [128, D], F32), sb("xt1", [128, D], F32)]
    mT = sb("mT", [128, 8, 128], BF16)
    xc = sb("xc", [128, 32, 128], BF16, nsub=8)
    tnh = [sb("tnh%d" % i, [128, 512], F32) for i in range(2)]
    x_tm = sb("x_tm", [128, 2048], BF16)
    xdte = x_tm
    B_tm = sb("B_tm", [128, 1024], BF16)
    xd_tm = sb("xd_tm", [128, 2048], BF16)
    xdt = sb("xdt", [128, 2048], BF16)
    dtt = sb("dtt", [128, 32], F32)
    dtx = sb("dtx", [128, 32], F32)
    adt = sb("adt", [128, 32], F32)
    adt_hi = sb("adt_hi", [128, 32], BF16)
    adt_lo = sb("adt_lo", [128, 32], BF16)
    adt_r = sb("adt_r", [128, 32], F32)
    acs = sb("acs", [128, 32], F32)
    ea = sb("ea", [128, 32], F32)
    eend = sb("eend", [128, 32], F32)
    dec = sb("dec", [128, 32], F32)
    cbm = [sb("cbm%d" % i, [128, 128], F32) for i in range(2)]
    segr = [sb("segr%d" % i, [128, 4, 128], BF16) for i in range(2)]
    Eg = [sb("Eg%d" % i, [128, 4, 128], BF16) for i in range(2)]
    WTg = [sb("WTg%d" % i, [128, 4, 128], BF16) for i in range(2)]
    ytmp = [sb("ytmp%d" % i, [128, 256], F32) for i in range(2)]
    zg = sb("zg", [128, 2048], BF16, nsub=4)
    yg = sb("yg", [128, 2048], BF16)
    ssq = sb("ssq", [128, 8], F32)
    rinv = sb("rinv", [128, 1], F32)
    gA = sb("gA", [128, D], BF16, nsub=2)
    gB = sb("gB", [128, D], BF16, nsub=2)
    u_t = sb("u_t", [128, D], BF16, nsub=2)
    v_t = sb("v_t", [128, D], F32, nsub=2)
    vb = sb("vb", [128, D], BF16)
    gm = sb("gm", [128, D], BF16)
    gmT = sb("gmT", [128, 8, 128], BF16)
    mg = gm
    mgT = gmT
    mtmp = [sb("mtmp%d" % i, [128, 512], F32) for i in range(2)]
    h1 = sb("h1", [128, D], F32)
    m2 = sb("m2", [128, D], BF16)
    xn = sb("xn", [128, D], BF16)
    m2T = sb("m2T", [128, 8, 128], BF16)
    hsa = sb("hsa", [128, 256], F32)
    hs = sb("hs", [128, 2, 128], BF16)
    sco = sb("sco", [128, 256], F32)
    cho = sb("cho", [128, 256], F32)
    g8 = sb("g8", [128, 8, 8], F32)
    gs = sb("gs", [128, 8], F32)
    gs8 = sb("gs8", [128, 8], F32)
    gpen = sb("gpen", [128, 8], F32)
    top8 = sb("top8", [128, 8], F32)
    idx8 = sb("idx8", [128, 8], U32)
    idxf = sb("idxf", [128, 8], F32)
    selb = sb("selb", [128, 256], BF16)
    posf = sb("posf", [128, 256], F32)
    junk = sb("junk", [128, 256], F32)
    sqj = junk
    pk8 = sb("pk8", [128, 8], F32)
    sk8 = sb("sk8", [128, 8], F32)
    slf = sb("slf", [128, 8], F32)
    ssum = sb("ssum", [128, 1], F32)

    def layer_norm_stats(src, eps):
        S.dve(lambda e: e.bn_stats(st6[:, 0:6], src[:, 0:512]), r=[src], w=[st6])
        S.dve(lambda e: e.bn_stats(st6[:, 6:12], src[:, 512:1024]), r=[src], w=[st6])
        S.dve(lambda e: e.bn_aggr(mv[:], st6[:]), r=[st6], w=[mv])
        S.dve(lambda e: e.tensor_scalar_add(rstd[:], mv[:, 1:2], eps), r=[mv], w=[rstd])
        S.pool(lambda e: e.tensor_tensor(rstd[:], rstd[:], mhalf[:], ALU.pow), r=[rstd, mhalf], w=[rstd])

    def ln_apply(dst_ap, src_ap, r, w):
        S.act(lambda e: e.activation(dst_ap, src_ap, AF.Identity, bias=nbias[:, 0:1], scale=rstd[:, 0:1]),
              r=list(r) + [rstd, nbias], w=w)

    def transposes8(src, dst, k):
        sv = src[:].rearrange("t (p k) -> t k p", k=k)
        for b0 in range(0, k, 8):
            pt = ps()
            ptv = psb(pt).rearrange("p (g t) -> p g t", g=8)
            for kk in range(8):
                S.tr(ptv[:, kk, :], sv[:, b0 + kk, :], ident[:], r=[src, ident], w=[pt])
            yield b0, pt, ptv

    xstate = {"loaded": False}
    pending = []

    def chunk(ci, xsrc, mode, last_prev=False, next_src=None, part="all", hoist=None):
        full = (mode == "M")
        xt = xts[ci % 2]
        nxb = 8 if (full or last_prev) else 6
        if part != "rest":
            chunk_head(ci, xsrc, full, next_src, xt, nxb)
        if part == "head":
            return
        chunk_rest(ci, mode, full, xt, nxb, hoist)

    def chunk_head(ci, xsrc, full, next_src, xt, nxb):
        if not xstate["loaded"]:
            S.dma("sp", xt[:], xsrc, w=[xt])
        xstate["loaded"] = False
        layer_norm_stats(xt, LN_EPS)
        S.dve(lambda e: e.tensor_scalar(xn[:], xt[:], mv[:, 0:1], rstd[:, 0:1], ALU.subtract, ALU.mult),
              r=[xt, mv, rstd], w=[xn])
        for b0, pt, ptv in transposes8(xn, mT, 8):
            S.dve(lambda e, ptv=ptv: e.tensor_tensor(mT[:], ptv, sc1_pk[:].unsqueeze(2).to_broadcast([128, 8, 128]),
                                                    ALU.mult), r=[pt, sc1_pk], w=[mT])
            S.dve(lambda e: e.tensor_tensor(mT[:], mT[:], sh1_pk[:].unsqueeze(2).to_broadcast([128, 8, 128]),
                                            ALU.add), r=[mT, sh1_pk], w=[mT])
        for i in range(nxb):
            rt, wv_ = wget("xbc%d" % i)
            pb_ = ps()
            for sub in range(4):
                S.mm(pb_[:, sub * 128:(sub + 1) * 128],
                     [(wv_[:, k, sub * 128:(sub + 1) * 128], mT[:, k, :]) for k in range(8)],
                     r=[rt, mT], w=[pb_])
            S.act(lambda e, i=i, pb_=pb_: e.copy(xBCT[:, 4 * i:4 * i + 4, 3:131],
                                                pb_[:].rearrange("p (j t) -> p j t", j=4)),
                  r=[pb_], w=[xBCT.s(i)])

    def chunk_rest(ci, mode, full, xt, nxb, hoist):
        stx = {}
        if full and pending:
            pending.pop()()
        for i in range(nxb):
            pc = ps()
            for sub in range(4):
                j = 4 * i + sub
                S.mm(pc[:, sub * 128:(sub + 1) * 128],
                     [(Dg[:, j, k, :], xBCT[:, j, k:k + 128]) for k in range(4)] +
                     [(brow[0:1, j * 128:(j + 1) * 128], onesb[0:1, :])],
                     r=[Dg, xBCT.s(i), brow, onesb], w=[pc])
            th = tnh[i % 2]
            S.act(lambda e, th=th, pc=pc: e.activation(th[:], pc[:], AF.Tanh), r=[pc], w=[th])
            S.dve(lambda e, i=i, th=th, pc=pc: e.scalar_tensor_tensor(
                xc[:, 4 * i:4 * i + 4, :].rearrange("p j t -> p (j t)"), th[:], 1.0, pc[:], ALU.add, ALU.mult),
                r=[th, pc], w=[xc.s(i)])
        S.pool(lambda e: e.tensor_copy(xBCT[:, :, 0:3], xBCT[:, :, 128:131]),
               r=[xBCT] + xBCT.subs, w=[xBCT] + xBCT.subs)
        for i in range(6):
            pt = ps()
            ptv = psb(pt)[:, 0:512]
            for sub in range(4):
                j = 4 * i + sub
                S.tr(ptv[:, sub * 128:(sub + 1) * 128], xc[:, j, :], ident[:], r=[xc.s(i), ident], w=[pt])
            if i < 4:
                S.act(lambda e, i=i, ptv=ptv: e.copy(x_tm[:, i * 512:(i + 1) * 512], ptv), r=[pt], w=[x_tm])
            else:
                S.act(lambda e, i=i, ptv=ptv: e.copy(B_tm[:, (i - 4) * 512:(i - 3) * 512], ptv), r=[pt], w=[B_tm])
        def tap(name, src, n=D, rr=()):
            if debug == name and full:
                S.dve(lambda e: e.tensor_copy(h1[:, 0:n], src), r=list(rr), w=[h1])
                S.dma("sp", dbg[ci * 128:(ci + 1) * 128, :], h1[:], r=[h1])
        tap("x_tm", x_tm[:, 0:D], rr=[x_tm])
        tap("B_tm", B_tm[:, 0:D], rr=[B_tm])
        rt, wv_ = wget("dt")
        pd = ps()
        S.mm(pd[:, 0:32], [(mT[:, k, :], wv_[:, k, :]) for k in range(8)], r=[rt, mT], w=[pd])
        if not full and hoist is not None:
            pd_id = banks.index(pd)
            pinned.add(pd_id)
            hoist()
            pinned.discard(pd_id)
        S.dve(lambda e: e.tensor_tensor(dtx[:], pd[:, 0:32], dtb_row[:], ALU.add), r=[pd, dtb_row], w=[dtx])
        S.act(lambda e: e.activation(dtx[:], dtx[:], AF.Exp), r=[dtx], w=[dtx])
        S.act(lambda e: e.activation(dtt[:], dtx[:], AF.Ln, bias=1.0), r=[dtx], w=[dtt])
        S.dve(lambda e: e.tensor_tensor(adt[:], dtt[:], a_row[:], ALU.mult), r=[dtt, a_row], w=[adt])
        S.dve(lambda e: e.tensor_copy(adt_hi[:], adt[:]), r=[adt], w=[adt_hi])
        S.dve(lambda e: e.tensor_tensor(adt_r[:], adt[:], adt_hi[:], ALU.subtract), r=[adt, adt_hi], w=[adt_r])
        S.dve(lambda e: e.tensor_copy(adt_lo[:], adt_r[:]), r=[adt_r], w=[adt_lo])
        pa_ = ps()
        S.mm(pa_[:, 0:32], [(Ub[:], adt_hi[:]), (Ub[:], adt_lo[:])], r=[Ub, adt_hi, adt_lo], w=[pa_])
        S.mm(pa_[:, 32:64], [(onesb[:], adt_hi[:]), (onesb[:], adt_lo[:])], r=[onesb, adt_hi, adt_lo], w=[pa_])
        S.act(lambda e: e.copy(acs[:], pa_[:, 0:32]), r=[pa_], w=[acs])
        S.act(lambda e: e.activation(ea[:], pa_[:, 0:32], AF.Exp), r=[pa_], w=[ea])
        S.act(lambda e: e.activation(dec[:], pa_[:, 32:64], AF.Exp), r=[pa_], w=[dec])
        S.dve(lambda e: e.tensor_tensor(eend[:], pa_[:, 32:64], acs[:], ALU.subtract), r=[pa_, acs], w=[eend])
        S.act(lambda e: e.activation(eend[:], eend[:], AF.Exp), r=[eend], w=[eend])
        x3 = x_tm[:].rearrange("p (h q) -> p h q", q=64)
        S.dve(lambda e: e.tensor_tensor(xdt[:].rearrange("p (h q) -> p h q", q=64), x3,
                                        dtt[:].unsqueeze(2).to_broadcast([128, 32, 64]), ALU.mult),
              r=[x_tm, dtt], w=[xdt])
        if full:
            S.pool(lambda e: e.tensor_tensor(xd_tm[:].rearrange("p (h q) -> p h q", q=64), x3,
                                             dsk32[:].unsqueeze(2).to_broadcast([128, 32, 64]), ALU.mult),
                   r=[x_tm, dsk32], w=[xd_tm])
        S.pool(lambda e: e.tensor_tensor(xdte[:].rearrange("p (h q) -> p h q", q=64),
                                        xdt[:].rearrange("p (h q) -> p h q", q=64),
                                        eend[:].unsqueeze(2).to_broadcast([128, 32, 64]), ALU.mult),
              r=[xdt, eend], w=[xdte])
        if full:
            def zblk(i):
                rt, wv_ = wget("z%d" % i)
                pz = ps()
                S.mm(pz[:], [(mT[:, k, :], wv_[:, k, :]) for k in range(8)], r=[rt, mT], w=[pz])
                th = tnh[i % 2]
                S.act(lambda e: e.activation(th[:], pz[:], AF.Tanh, scale=0.5), r=[pz], w=[th])
                S.dve(lambda e: e.scalar_tensor_tensor(zg[:, i * 512:(i + 1) * 512], th[:], 1.0, pz[:],
                                                       ALU.add, ALU.mult), r=[th, pz], w=[zg.s(i)])

            def uvblk(i):
                rt, wv_ = wget("uv%d" % i)
                pu = ps()
                S.mm(pu[:], [(mT[:, k, :], wv_[:, k, :]) for k in range(8)], r=[rt, mT], w=[pu])
                th = tnh[i % 2]
                mt = mtmp[i % 2]
                S.act(lambda e: e.activation(th[:], pu[:], AF.Square), r=[pu], w=[th])
                S.dve(lambda e: e.tensor_scalar(th[:], th[:], 0.044715 * GC, GC, ALU.mult, ALU.add), r=[th], w=[th])
                S.dve(lambda e: e.tensor_tensor(mt[:], th[:], pu[:], ALU.mult), r=[th, pu], w=[mt])
                S.act(lambda e: e.activation(mt[:], mt[:], AF.Tanh), r=[mt], w=[mt])
                if i < 2:
                    S.dve(lambda e: e.scalar_tensor_tensor(u_t[:, i * 512:(i + 1) * 512], mt[:], 1.0, pu[:],
                                                           ALU.add, ALU.mult), r=[mt, pu], w=[u_t.s(i)])
                else:
                    S.dve(lambda e: e.scalar_tensor_tensor(v_t[:, (i - 2) * 512:(i - 1) * 512], mt[:], 1.0, pu[:],
                                                           ALU.add, ALU.mult), r=[mt, pu], w=[v_t.s(i - 2)])

            def gblk(i):
                rt, wv_ = wget(("ga%d" % i) if i < 2 else ("gb%d" % (i - 2)))
                pg_ = ps()
                S.mm(pg_[:], [(mT[:, k, :], wv_[:, k, :]) for k in range(8)], r=[rt, mT], w=[pg_])
                dst = gA if i < 2 else gB
                hh_ = i % 2
                th = tnh[i % 2]
                S.act(lambda e: e.activation(th[:], pg_[:], AF.Tanh, scale=0.5), r=[pg_], w=[th])
                S.pool(lambda e: e.tensor_scalar(dst[:, hh_ * 512:(hh_ + 1) * 512], th[:], 1.0, 1.0,
                                                 ALU.add, ALU.mult), r=[th], w=[dst.s(hh_)])

            def lnv():
                S.dve(lambda e: e.bn_stats(st6[:, 0:6], v_t[:, 0:512]), r=[v_t.s(0)], w=[st6])
                S.dve(lambda e: e.bn_stats(st6[:, 6:12], v_t[:, 512:1024]), r=[v_t.s(1)], w=[st6])
                S.dve(lambda e: e.bn_aggr(mv[:], st6[:]), r=[st6], w=[mv])
                S.dve(lambda e: e.tensor_scalar_add(rstd[:], mv[:, 1:2], 4.0 * LN_EPS), r=[mv], w=[rstd])
                S.pool(lambda e: e.tensor_tensor(rstd[:], rstd[:], mhalf[:], ALU.pow), r=[rstd, mhalf], w=[rstd])
                S.dve(lambda e: e.tensor_scalar(v_t[:], v_t[:], mv[:, 0:1], rstd[:, 0:1], ALU.subtract, ALU.mult),
                      r=[v_t.s(0), v_t.s(1), mv, rstd], w=[v_t.s(0), v_t.s(1)])
                S.dve(lambda e: e.tensor_tensor(v_t[:], v_t[:], lng_row[:], ALU.mult), r=[v_t.s(0), v_t.s(1), lng_row],
                      w=[v_t.s(0), v_t.s(1)])
                S.dve(lambda e: e.tensor_tensor(vb[:], v_t[:], lnb_row[:], ALU.add), r=[v_t.s(0), v_t.s(1), lnb_row], w=[vb])

            def spat(hf):
                pv = ps()
                for gg in range(4):
                    g = 4 * hf + gg
                    S.mm(pv[:, gg * 128:(gg + 1) * 128], [(WsT[:, g, :], vb[:, g * 128:(g + 1) * 128])], r=[WsT, vb], w=[pv])
                for gg in range(4):
                    g = 4 * hf + gg
                    S.dve(lambda e, g=g, gg=gg, pv=pv: e.scalar_tensor_tensor(
                        gm[:, g * 128:(g + 1) * 128], pv[:, gg * 128:(gg + 1) * 128], bsT[:, g:g + 1],
                        u_t[:, g * 128:(g + 1) * 128], ALU.add, ALU.mult), r=[pv, bsT, u_t.s(hf)], w=[gm])

            def gmt_pg():
                for b0, pt, ptv in transposes8(gm, gmT, 8):
                    S.act(lambda e, ptv=ptv: e.copy(gmT[:], ptv), r=[pt], w=[gmT])
                pyb = [ps(), ps()]
                stx["pyb"] = pyb
                stx["pyb_ids"] = [banks.index(p_) for p_ in pyb]
                pinned.update(stx["pyb_ids"])
                for ch in range(2):
                    rt, wv_ = wget("pg%d" % ch)
                    S.mm(pyb[ch][:], [(gmT[:, k, :], wv_[:, k, :]) for k in range(8)], r=[rt, gmT], w=[pyb[ch]])

            extra = {0: [lambda: zblk(1), lambda: uvblk(0)], 1: [lambda: uvblk(1)],
                     2: [lambda: zblk(2), lambda: uvblk(2)], 3: [lambda: uvblk(3)],
                     4: [lambda: zblk(3), lambda: gblk(0), lnv], 5: [lambda: gblk(1), lambda: spat(0)],
                     6: [lambda: gblk(2), lambda: spat(1)], 7: [lambda: gblk(3), gmt_pg]}
            zblk(0)
            def s1(g):
                i2 = g % 2
                pcb = ps()
                S.mm(pcb[:, 0:128], [(xc[:, 16 + g, :], xc[:, 24 + g, :])], r=[xc.s(4 + g // 4), xc.s(6 + g // 4)], w=[pcb])
                S.dve(lambda e: e.tensor_tensor(cbm[i2][:], pcb[:, 0:128], Uf[:], ALU.mult), r=[pcb, Uf], w=[cbm[i2]])
                S.pool(lambda e: e.tensor_tensor(segr[i2][:], Ub[:].unsqueeze(1).to_broadcast([128, 4, 128]),
                                                 adt[:, 4 * g:4 * g + 4].unsqueeze(2).to_broadcast([128, 4, 128]),
                                                 ALU.mult), r=[Ub, adt], w=[segr[i2]])
                psg = ps()
                S.mm(psg[:], [(SLb[:], segr[i2][:].rearrange("p r l -> p (r l)"))], r=[SLb, segr[i2]], w=[psg])
                S.act(lambda e: e.activation(Eg[i2][:].rearrange("p r l -> p (r l)"), psg[:], AF.Exp), r=[psg], w=[Eg[i2]])
                S.dve(lambda e: e.tensor_tensor(WTg[i2][:], Eg[i2][:], cbm[i2][:].unsqueeze(1).to_broadcast([128, 4, 128]),
                                                ALU.mult), r=[Eg[i2], cbm[i2]], w=[WTg[i2]])

            def s2(g):
                i2 = g % 2
                py = ps()
                S.mm(py[:, 0:256], [(ident[:], xd_tm[:, g * 256:(g + 1) * 256])], r=[ident, xd_tm], w=[py], first=True, last=False)
                for r_ in range(4):
                    h = 4 * g + r_
                    S.mm(py[:, r_ * 64:(r_ + 1) * 64], [(WTg[i2][:, r_, :], xdt[:, h * 64:(h + 1) * 64])],
                         r=[WTg[i2], xdt], w=[py], first=False, last=True)
                S.mm(py[:, 256:512], [(xc[:, 24 + g, :], Sbf[:, g * 256:(g + 1) * 256])],
                     r=[xc.s(6 + g // 4), Sbf], w=[py])
                yt_ = ytmp[i2]
                S.dve(lambda e: e.tensor_tensor(
                    yt_[:].rearrange("p (r q) -> p r q", q=64), py[:, 256:512].rearrange("p (r q) -> p r q", q=64),
                    ea[:, 4 * g:4 * g + 4].unsqueeze(2).to_broadcast([128, 4, 64]), ALU.mult), r=[py, ea], w=[yt_])
                S.dve(lambda e: e.tensor_tensor(yt_[:], yt_[:], py[:, 0:256], ALU.add), r=[yt_, py], w=[yt_])
                S.dve(lambda e: e.tensor_tensor(yg[:, g * 256:(g + 1) * 256], yt_[:], zg[:, g * 256:(g + 1) * 256], ALU.mult),
                      r=[yt_, zg.s(g // 2)], w=[yg])
                S.act(lambda e: e.activation(sqj[:], yg[:, g * 256:(g + 1) * 256], AF.Square, accum_out=ssq[:, g:g + 1]),
                      r=[yg], w=[sqj, ssq])

            s1(0)
            for g in range(8):
                if g + 1 < 8:
                    s1(g + 1)
                s2(g)
                for f_ in extra[g]:
                    f_()
        if full:
            tap("yg", yg[:, 0:D], rr=[yg])
            tap("dt", dtt[:], n=32, rr=[dtt])
            tap("acs", acs[:], n=32, rr=[acs])
        for q4 in range(4):
            pst = ps()
            for gg in range(2):
                g = 2 * q4 + gg
                S.mm(pst[:, gg * 256:(gg + 1) * 256], [(B_tm[:, g * 128:(g + 1) * 128], xdte[:, g * 256:(g + 1) * 256])],
                     r=[B_tm, xdte], w=[pst])
            sl = slice(q4 * 512, (q4 + 1) * 512)
            S.dve(lambda e, sl=sl, q4=q4: e.tensor_tensor(Sst[:, sl].rearrange("p (h q) -> p h q", q=64),
                                                          Sst[:, sl].rearrange("p (h q) -> p h q", q=64),
                                                          dec[:, q4 * 8:(q4 + 1) * 8].unsqueeze(2).to_broadcast([128, 8, 64]),
                                                          ALU.mult), r=[Sst, dec], w=[Sst])
            S.dve(lambda e, sl=sl, pst=pst: e.tensor_tensor(Sst[:, sl], Sst[:, sl], pst[:], ALU.add), r=[Sst, pst], w=[Sst])
            S.act(lambda e, sl=sl: e.copy(Sbf[:, sl], Sst[:, sl]), r=[Sst], w=[Sbf])
        if not full:
            return
        S.dve(lambda e: e.tensor_reduce(rinv[:], ssq[:], AX.X, ALU.add), r=[ssq], w=[rinv])
        S.dve(lambda e: e.tensor_scalar(rinv[:], rinv[:], 1.0 / 2048.0, 4.0 * RMS_EPS, ALU.mult, ALU.add), r=[rinv], w=[rinv])
        S.pool(lambda e: e.tensor_tensor(rinv[:], rinv[:], mhalf[:], ALU.pow), r=[rinv, mhalf], w=[rinv])
        yTs = [xc.s(0), xc.s(1), xc.s(2), xc.s(3)]
        for b0, pt, ptv in transposes8(yg, None, 16):
            S.dve(lambda e, b0=b0, ptv=ptv: e.tensor_tensor(xc[:, b0:b0 + 8, :], ptv,
                                                           normw_pk[:, b0:b0 + 8].unsqueeze(2).to_broadcast([128, 8, 128]),
                                                           ALU.mult), r=[pt, normw_pk], w=yTs[b0 // 4:b0 // 4 + 2])
        pya = [ps(), ps()]
        for ch in range(2):
            for kh in range(2):
                rt, wv_ = wget("ps%d%d" % (ch, kh))
                S.mm(pya[ch][:], [(xc[:, kh * 8 + k, :], wv_[:, k, :]) for k in range(8)], r=[rt] + yTs, w=[pya[ch]],
                     first=(kh == 0), last=(kh == 1))
        pyb = stx["pyb"]
        if debug in ("ya", "yb"):
            for ch in range(2):
                sl = slice(ch * 512, (ch + 1) * 512)
                if debug == "ya":
                    S.dve(lambda e, ch=ch, sl=sl: e.tensor_scalar(h1[:, sl], pya[ch][:], rinv[:, 0:1], None, ALU.mult),
                          r=[pya[ch], rinv], w=[h1])
                else:
                    S.dve(lambda e, ch=ch, sl=sl: e.tensor_scalar(h1[:, sl], pyb[ch][:], 0.5, None, ALU.mult),
                          r=[pyb[ch]], w=[h1])
            S.dma("sp", dbg[ci * 128:(ci + 1) * 128, :], h1[:], r=[h1])
        for ch in range(2):
            sl = slice(ch * 512, (ch + 1) * 512)
            S.dve(lambda e, ch=ch, sl=sl: e.scalar_tensor_tensor(mtmp[0][:], pya[ch][:], rinv[:, 0:1], gA[:, sl],
                                                                 ALU.mult, ALU.mult), r=[pya[ch], rinv, gA.s(ch)], w=[mtmp[0]])
            S.dve(lambda e, ch=ch, sl=sl: e.scalar_tensor_tensor(mtmp[1][:], pyb[ch][:], 0.5, gB[:, sl],
                                                                 ALU.mult, ALU.mult), r=[pyb[ch], gB.s(ch)], w=[mtmp[1]])
            S.dve(lambda e, sl=sl: e.tensor_tensor(mg[:, sl], mtmp[0][:], mtmp[1][:], ALU.add),
                  r=[mtmp[0], mtmp[1]], w=[mg])
        pinned.difference_update(stx["pyb_ids"])
        for b0, pt, ptv in transposes8(mg, mgT, 8):
            S.act(lambda e, ptv=ptv: e.copy(mgT[:], ptv), r=[pt], w=[mgT])
        pmx = [ps(), ps()]
        pin_ids = [banks.index(p_) for p_ in pmx]
        pinned.update(pin_ids)
        for ch in range(2):
            rt, wv_ = wget("wo%d" % ch)
            S.mm(pmx[ch][:], [(mgT[:, k, :], wv_[:, k, :]) for k in range(8)], r=[rt, mgT], w=[pmx[ch]])
        if full and hoist is not None:
            hoist()
        if debug == "mix":
            for ch in range(2):
                sl = slice(ch * 512, (ch + 1) * 512)
                S.dve(lambda e, ch=ch, sl=sl: e.tensor_scalar(h1[:, sl], pmx[ch][:], 0.5, None, ALU.mult),
                      r=[pmx[ch]], w=[h1])
            S.dma("sp", dbg[ci * 128:(ci + 1) * 128, :], h1[:], r=[h1])
        for ch in range(2):
            sl = slice(ch * 512, (ch + 1) * 512)
            S.dve(lambda e, ch=ch, sl=sl: e.tensor_tensor(mtmp[ch][:], pmx[ch][:], g1h_row[:, sl], ALU.mult),
                  r=[pmx[ch], g1h_row], w=[mtmp[ch]])
            S.dve(lambda e, ch=ch, sl=sl: e.scalar_tensor_tensor(xt[:, sl], xt[:, sl], ALPHA, mtmp[ch][:], ALU.mult, ALU.add),
                  r=[xt, mtmp[ch]], w=[xt])
        pinned.difference_update(pin_ids)
        layer_norm_stats(xt, LN_EPS)
        S.dve(lambda e: e.tensor_scalar(h1[:], xt[:], mv[:, 0:1], rstd[:, 0:1], ALU.subtract, ALU.mult),
              r=[xt, mv, rstd], w=[h1])
        S.dve(lambda e: e.tensor_tensor(h1[:], h1[:], ln1g_row[:], ALU.mult), r=[h1, ln1g_row], w=[h1])
        S.dve(lambda e: e.tensor_tensor(h1[:], h1[:], ln1b_row[:], ALU.add), r=[h1, ln1b_row], w=[h1])
        if debug == "h1":
            S.dma("sp", dbg[ci * 128:(ci + 1) * 128, :], h1[:], r=[h1])
        layer_norm_stats(h1, LN_EPS)
        S.dve(lambda e: e.tensor_scalar(xt[:], h1[:], mv[:, 0:1], rstd[:, 0:1], ALU.subtract, ALU.mult),
              r=[h1, mv, rstd], w=[xt])
        S.dve(lambda e: e.tensor_tensor(xt[:], xt[:], sc2_row[:], ALU.mult), r=[xt, sc2_row], w=[xt])
        S.dve(lambda e: e.tensor_tensor(m2[:], xt[:], sh2_row[:], ALU.add), r=[xt, sh2_row], w=[m2])
        def tail():
            for b0, pt, ptv in transposes8(m2, m2T, 8):
                S.act(lambda e, ptv=ptv: e.copy(m2T[:], ptv), r=[pt], w=[m2T])
            rt, wv_ = wget("rt")
            plg = ps()
            S.mm(plg[:, 0:256], [(m2T[:, k, :], wv_[:, k, :]) for k in range(8)], r=[rt, m2T], w=[plg])
            S.act(lambda e: e.activation(sco[:], plg[:, 0:256], AF.Tanh, scale=0.5), r=[plg], w=[sco])
            rt, wv_ = wget("sgu")
            phs = ps()
            for blk in range(4):
                S.mm(phs[:, blk * 128:(blk + 1) * 128], [(wv_[:, k, blk * 128:(blk + 1) * 128], m2T[:, k, :]) for k in range(8)],
                     r=[rt, m2T], w=[phs])
            S.act(lambda e: e.activation(hsa[:], phs[:, 0:256], AF.Tanh, scale=0.5), r=[phs], w=[hsa])
            S.dve(lambda e: e.scalar_tensor_tensor(hsa[:], hsa[:], 1.0, phs[:, 0:256], ALU.add, ALU.mult), r=[hsa, phs], w=[hsa])
            S.dve(lambda e: e.tensor_tensor(hs[:].rearrange("p b t -> p (b t)"), hsa[:], phs[:, 256:512], ALU.mult),
                  r=[hsa, phs], w=[hs])
            rt, wv_ = wget("sd")
            psd = [ps(), ps()]
            for ch in range(2):
                S.mm(psd[ch][:], [(hs[:, b, :], wv_[:, b, ch * 512:(ch + 1) * 512]) for b in range(2)], r=[rt, hs], w=[psd[ch]])
            for ch in range(2):
                sl = slice(ch * 512, (ch + 1) * 512)
                S.dve(lambda e, ch=ch, sl=sl: e.tensor_tensor(mtmp[ch][:], psd[ch][:], g2h_row[:, sl], ALU.mult),
                      r=[psd[ch], g2h_row], w=[mtmp[ch]])
                S.dve(lambda e, ch=ch, sl=sl: e.scalar_tensor_tensor(v_t[:, sl], h1[:, sl], ALPHA, mtmp[ch][:], ALU.mult, ALU.add),
                      r=[h1, mtmp[ch]], w=[v_t.s(ch)])
            S.dma(os.environ.get("K_STQ", "pool"), res2_d[ci * 128:(ci + 1) * 128, :], v_t[:], r=[v_t.s(0), v_t.s(1)], w=[res2_b[ci]])
            S.dve(lambda e: e.tensor_scalar(sco[:], sco[:], 0.5, 0.5, ALU.mult, ALU.add), r=[sco], w=[sco])
            S.dve(lambda e: e.tensor_tensor(cho[:], sco[:], rb_row[:], ALU.add), r=[sco, rb_row], w=[cho])
            for g in range(8):
                S.dve(lambda e, g=g: e.max(g8[:, g, :], cho[:, g * 32:(g + 1) * 32]), r=[cho], w=[g8])
            S.dve(lambda e: e.tensor_tensor(gs[:], g8[:, :, 0], g8[:, :, 1], ALU.add), r=[g8], w=[gs])
            S.dve(lambda e: e.max(gs8[:], gs[:]), r=[gs], w=[gs8])
            S.dve(lambda e: e.tensor_scalar(gpen[:], gs[:], gs8[:, 3:4], None, ALU.is_ge), r=[gs, gs8], w=[gpen])
            S.dve(lambda e: e.tensor_scalar(gpen[:], gpen[:], -1.0, BIG, ALU.add, ALU.mult), r=[gpen], w=[gpen])
            S.dve(lambda e: e.tensor_tensor(cho[:].rearrange("p (g q) -> p g q", q=32), cho[:].rearrange("p (g q) -> p g q", q=32),
                                            gpen[:].unsqueeze(2).to_broadcast([128, 8, 32]), ALU.add), r=[cho, gpen], w=[cho])
            S.dve(lambda e: e.max(top8[:], cho[:]), r=[cho], w=[top8])
            S.dve(lambda e: e.tensor_scalar(selb[:], cho[:], top8[:, 7:8], None, ALU.is_ge), r=[cho, top8], w=[selb])
            S.dve(lambda e: e.tensor_tensor(cho[:], sco[:], selb[:], ALU.mult), r=[sco, selb], w=[cho])
            S.dve(lambda e: e.max(sk8[:], cho[:]), r=[cho], w=[sk8])
            S.dve(lambda e: e.max_index(idx8[:], sk8[:], cho[:]), r=[sk8, cho], w=[idx8])
            S.dve(lambda e: e.tensor_copy(idxf[:], idx8[:]), r=[idx8], w=[idxf])
            ppos = ps()
            S.mm(ppos[:, 0:256], [(SUb[:], selb[:]), (onesb[:], Rcnt[:])], r=[SUb, selb, onesb, Rcnt], w=[ppos])
            S.act(lambda e: e.copy(posf[:], ppos[:, 0:256]), r=[ppos], w=[posf])
            S.pool(lambda e: e.tensor_tensor(Rcnt[:], Rcnt[:], selb[:], ALU.add), r=[Rcnt, selb], w=[Rcnt])
            for k in range(8):
                S.dve(lambda e, k=k: e.scalar_tensor_tensor(junk[:], iota_e[:], idxf[:, k:k + 1], posf[:], ALU.is_equal, ALU.mult,
                                                            accum_out=pk8[:, k:k + 1]), r=[iota_e, idxf, posf], w=[junk, pk8])
            S.dve(lambda e: e.tensor_copy(idx_all[:, ci, :], idxf[:]), r=[idxf], w=[idx_all])
            S.dve(lambda e: e.tensor_copy(pos_all[:, ci, :], pk8[:]), r=[pk8], w=[pos_all])
            S.dve(lambda e: e.tensor_reduce(ssum[:], sk8[:], AX.X, ALU.add), r=[sk8], w=[ssum])
            S.dve(lambda e: e.tensor_scalar_add(ssum[:], ssum[:], 1e-20), r=[ssum], w=[ssum])
            S.dve(lambda e: e.reciprocal(ssum[:], ssum[:]), r=[ssum], w=[ssum])
            S.dve(lambda e: e.tensor_scalar(wk_t[:, ci, :], sk8[:], ssum[:, 0:1], 1.25, ALU.mult, ALU.mult), r=[sk8, ssum], w=[wk_t])
            S.dma(os.environ.get("K_STQ", "pool"), m2_d[ci * 128:(ci + 1) * 128, :], m2[:], r=[m2], w=[m2_b[ci]])
        pending.append(tail)

    def m_head(j):
        chunk(j, x_cur[j * 128:(j + 1) * 128, :], "M", part="head")

    def a_head(j):
        chunk(j, x_prev[j * 128:(j + 1) * 128, :], "A", last_prev=(j == NPV - 1), part="head")

    if NPV > 0:
        a_head(0)
        for i in range(NPV):
            hz = (lambda j=i + 1: a_head(j)) if i + 1 < NPV else (lambda: m_head(0))
            chunk(i, None, "A", last_prev=(i == NPV - 1), part="rest", hoist=hz)
        S.dve(lambda e: e.tensor_scalar_mul(Sst[:], Sst[:], flag_t[:, 0:1]), r=[Sst, flag_t], w=[Sst])
        S.dve(lambda e: e.tensor_scalar_mul(Sbf[:], Sbf[:], flag_t[:, 0:1]), r=[Sbf, flag_t], w=[Sbf])
        S.dve(lambda e: e.tensor_scalar_mul(xBCT[:, :, 0:3], xBCT[:, :, 0:3], flag_t[:, 0:1]),
              r=[xBCT, flag_t] + xBCT.subs, w=[xBCT] + xBCT.subs)
    else:
        m_head(0)
    for i in range(NCH):
        hz = (lambda j=i + 1: m_head(j)) if i + 1 < NCH else None
        chunk(i, None, "M", part="rest", hoist=hz)
    pending.pop()()

    S.barrier()
    ph1.close()
    ph2 = ExitStack()
    scope[0] = ph2
    I32 = mybir.dt.int32
    cntc = sb("cntc", [128, 2], F32)
    ci32 = sb("ci32", [128, 2], I32)
    padc = sb("padc", [128, 2], F32)
    padb = sb("padb", [128, 2], BF16)
    pendc = sb("pendc", [128, 2], F32)
    pstc = sb("pstc", [128, 2], F32)
    tot = sb("tot", [128, 1], F32)
    onesf = sb("onesf", [128, 128], F32)
    dgf = sb("dgf", [128, 128], F32)
    PSrow = sb("PSrow", [128, 256], F32)
    iob = sb("iob", [128, NEB], F32)
    iop = sb("iop", [128, 1], F32)
    cmpb = [sb("cmpb%d" % i, [128, NEB], BF16) for i in range(2)]
    BEf = sb("BEf", [128, NEB], F32)
    usedf = sb("usedf", [128, NEB], F32)
    IDXW = sb("IDXW", [128, NEB], U32)
    psk = sb("psk", [128, 8], F32)
    junk2 = sb("junk2", [128, 256], F32)
    m2r = [sb("m2r%d" % i, [128, D], BF16) for i in range(2)]
    S.pool(lambda e: e.memset(onesf[:], 1.0), w=[onesf])
    S.pool(lambda e: e.iota(iob[:], pattern=[[128, NEB]], base=0, channel_multiplier=0,
                            allow_small_or_imprecise_dtypes=True), w=[iob])
    S.pool(lambda e: e.iota(iop[:], pattern=[[0, 1]], base=0, channel_multiplier=1,
                            allow_small_or_imprecise_dtypes=True), w=[iop])
    pc_ = ps()
    for h in range(2):
        S.mm(pc_[:, h:h + 1], [(Rcnt[:, h * 128:(h + 1) * 128], onesb[:, 0:1])], r=[Rcnt, onesb], w=[pc_])
    S.dve(lambda e: e.tensor_copy(cntc[:], pc_[:, 0:2]), r=[pc_], w=[cntc])
    S.dve(lambda e: e.tensor_scalar_add(ci32[:], cntc[:], 127.0), r=[cntc], w=[ci32])
    S.dve(lambda e: e.tensor_scalar(ci32[:], ci32[:], 7, 7, ALU.arith_shift_right, ALU.logical_shift_left), r=[ci32], w=[ci32])
    S.dve(lambda e: e.tensor_copy(padc[:], ci32[:]), r=[ci32], w=[padc])
    S.dve(lambda e: e.tensor_copy(padb[:], padc[:]), r=[padc], w=[padb])
    pq = ps()
    S.mm(pq[:, 0:1], [(Ub[:], padb[:, 0:1])], r=[Ub, padb], w=[pq])
    S.mm(pq[:, 1:2], [(Ub[:], padb[:, 1:2]), (onesb[:], padb[:, 0:1])], r=[Ub, onesb, padb], w=[pq])
    S.mm(pq[:, 2:3], [(onesb[:], padb[:, 0:1]), (onesb[:], padb[:, 1:2])], r=[onesb, padb], w=[pq])
    S.dve(lambda e: e.tensor_copy(pendc[:], pq[:, 0:2]), r=[pq], w=[pendc])
    S.dve(lambda e: e.tensor_copy(tot[:], pq[:, 2:3]), r=[pq], w=[tot])
    S.dve(lambda e: e.tensor_tensor(pstc[:], pendc[:], padc[:], ALU.subtract), r=[pendc, padc], w=[pstc])
    pr_ = ps()
    for h in range(2):
        S.dve(lambda e, h=h: e.tensor_scalar(dgf[:], identf[:], pstc[:, h:h + 1], None, ALU.mult), r=[identf, pstc], w=[dgf])
        S.mm(pr_[:, h * 128:(h + 1) * 128], [(onesf[:], dgf[:])], r=[onesf, dgf], w=[pr_])
    S.dve(lambda e: e.tensor_copy(PSrow[:], pr_[:, 0:256]), r=[pr_], w=[PSrow])
    pbe = ps()
    for h in range(2):
        S.dve(lambda e, h=h: e.tensor_scalar(cmpb[h][:], iob[:], pendc[:, h:h + 1], None, ALU.is_ge), r=[iob, pendc], w=[cmpb[h]])
    S.mm(pbe[:, 0:NEB], [(onesb[:], cmpb[0][:]), (onesb[:], cmpb[1][:])], r=[onesb, cmpb[0], cmpb[1]], w=[pbe])
    S.dve(lambda e: e.tensor_scalar(BEf[:], pbe[:, 0:NEB], 255.0, 128.0, ALU.min, ALU.mult), r=[pbe], w=[BEf])
    S.dve(lambda e: e.tensor_scalar(BEf[:], BEf[:], iop[:, 0:1], None, ALU.add), r=[BEf, iop], w=[BEf])
    S.dve(lambda e: e.tensor_scalar(usedf[:], iob[:], tot[:, 0:1], 1.0e6, ALU.is_ge, ALU.mult), r=[iob, tot], w=[usedf])
    S.dve(lambda e: e.tensor_tensor(BEf[:], BEf[:], usedf[:], ALU.add), r=[BEf, usedf], w=[BEf])
    S.dve(lambda e: e.tensor_copy(IDXW[:], BEf[:]), r=[BEf], w=[IDXW])
    for ci in range(NCH):
        mr = m2r[ci % 2]
        S.dma("sp", mr[:], m2_d[ci * 128:(ci + 1) * 128, :], r=[m2_b[ci]], w=[mr])
        for k in range(8):
            S.dve(lambda e, k=k, ci=ci: e.scalar_tensor_tensor(junk2[:], iota_e[:], idx_all[:, ci, k:k + 1], PSrow[:],
                                                               ALU.is_equal, ALU.mult, accum_out=psk[:, k:k + 1]),
                  r=[iota_e, idx_all, PSrow], w=[junk2, psk])
        S.dve(lambda e, ci=ci: e.tensor_tensor(psk[:], psk[:], pos_all[:, ci, :], ALU.add), r=[psk, pos_all], w=[psk])
        S.dve(lambda e, ci=ci: e.tensor_copy(slot_u[:, ci, :], psk[:]), r=[psk], w=[slot_u])
        for k in range(8):
            S.dma("pool", None, None, r=[mr, slot_u], w=[],
                  fn=lambda e, k=k, ci=ci, mr=mr: e.indirect_dma_start(
                      out=xs_d[:, :], out_offset=bass.IndirectOffsetOnAxis(ap=slot_u[:, ci, k:k + 1], axis=0),
                      in_=mr[:, :], in_offset=None, bounds_check=reg_slot, oob_is_err=False))
    S.barrier()

    NWB = 3
    wg = [sb("wg%d" % i, [128, 8, 256], BF16) for i in range(NWB)]
    wu = [sb("wu%d" % i, [128, 8, 256], BF16) for i in range(NWB)]
    wd = [sb("wd%d" % i, [128, 2, D], BF16) for i in range(NWB)]
    xsb = [sb("xsb%d" % i, [128, D], BF16) for i in range(4)]
    xsT = [sb("xsT%d" % i, [128, 8, 128], BF16) for i in range(2)]
    hga = [sb("hga%d" % i, [128, 2, 128], F32) for i in range(2)]
    hh = [sb("hh%d" % i, [128, 2, 128], BF16) for i in range(2)]
    yo = [sb("yo%d" % i, [128, D], BF16) for i in range(3)]
    for t_ in wg + wu + wd:
        S.pool(lambda e, t_=t_: e.memset(t_[:], 0.0), w=[t_])
    weg = w_e_gate[:, :]
    weu = w_e_up[:, :]
    wed = w_e_down[:, :]
    def xs_load(b_):
        if b_ < NEB:
            S.dma("sp", xsb[b_ % 4][:], xs_d[b_ * 128:(b_ + 1) * 128, :], r=[xs_b], w=[xsb[b_ % 4]])

    xs_load(0)
    xs_load(1)
    for bk in range(NEB):
        i2 = bk % 2
        iw = bk % NWB
        xs_load(bk + 2)
        for (wt_, src) in ((wg[iw], weg), (wu[iw], weu), (wd[iw], wed)):
            S.dma("pool", None, None, r=[IDXW], w=[wt_],
                  fn=lambda e, wt_=wt_, src=src, bk=bk: e.indirect_dma_start(
                      out=wt_[:].rearrange("p a b -> p (a b)"), out_offset=None, in_=src,
                      in_offset=bass.IndirectOffsetOnAxis(ap=IDXW[:, bk:bk + 1], axis=0),
                      bounds_check=reg_w, oob_is_err=False))
        xb = xsb[bk % 4]
        sv = xb[:].rearrange("t (p k) -> t k p", k=8)
        pt = ps()
        ptv = psb(pt).rearrange("p (g t) -> p g t", g=8)
        for kk in range(8):
            S.tr(ptv[:, kk, :], sv[:, kk, :], ident[:], r=[xb, ident], w=[pt])
        S.act(lambda e, i2=i2, ptv=ptv: e.copy(xsT[i2][:], ptv), r=[pt], w=[xsT[i2]])
        pgu = ps()
        for fb in range(2):
            wgv = wg[iw][:].rearrange("p k (q b) -> p k b q", b=2)
            wuv = wu[iw][:].rearrange("p k (q b) -> p k b q", b=2)
            S.mm(pgu[:, fb * 128:(fb + 1) * 128], [(wgv[:, k, fb, :], xsT[i2][:, k, :]) for k in range(8)],
                 r=[wg[iw], xsT[i2]], w=[pgu])
            S.mm(pgu[:, 256 + fb * 128:256 + (fb + 1) * 128], [(wuv[:, k, fb, :], xsT[i2][:, k, :]) for k in range(8)],
                 r=[wu[iw], xsT[i2]], w=[pgu])
        hg_ = hga[i2][:].rearrange("p b t -> p (b t)")
        S.act(lambda e, hg_=hg_, pgu=pgu: e.activation(hg_, pgu[:, 0:256], AF.Tanh, scale=0.5), r=[pgu], w=[hga[i2]])
        S.dve(lambda e, hg_=hg_, pgu=pgu: e.scalar_tensor_tensor(hg_, hg_, 1.0, pgu[:, 0:256], ALU.add, ALU.mult),
              r=[hga[i2], pgu], w=[hga[i2]])
        S.dve(lambda e, i2=i2, hg_=hg_, pgu=pgu: e.tensor_tensor(hh[i2][:].rearrange("p b t -> p (b t)"), hg_, pgu[:, 256:512],
                                                                 ALU.mult), r=[hga[i2], pgu], w=[hh[i2]])
        yb_ = yo[bk % 3]
        for ch in range(2):
            pd_ = ps()
            S.mm(pd_[:], [(hh[i2][:, b_, :], wd[iw][:, b_, ch * 512:(ch + 1) * 512]) for b_ in range(2)],
                 r=[hh[i2], wd[iw]], w=[pd_])
            S.act(lambda e, yb_=yb_, ch=ch, pd_=pd_: e.copy(yb_[:, ch * 512:(ch + 1) * 512], pd_[:]), r=[pd_], w=[yb_])
        S.dma(os.environ.get("K_YSQ", "act"), ys_d[bk * 128:(bk + 1) * 128, :], yb_[:], r=[yb_], w=[])

    S.barrier()
    ph2.close()
    ph3 = ExitStack()
    scope[0] = ph3
    ln2g_row = sb("ln2g_row", [128, D], F32)
    ln2b_row = sb("ln2b_row", [128, D], F32)
    g2_row = sb("g2_row", [128, D], F32)
    S.dma("sp", ln2g_row[:], ln2_g.partition_broadcast(128), w=[ln2g_row])
    S.dma("sp", ln2b_row[:], ln2_b.partition_broadcast(128), w=[ln2b_row])
    S.dve(lambda e: e.tensor_scalar_mul(g2_row[:], g2h_row[:], 2.0), r=[g2h_row], w=[g2_row])
    yk = [sb("yk%d" % i, [128, D], BF16) for i in range(4)]
    facc = [sb("facc%d" % i, [128, D], F32) for i in range(2)]
    r2 = [sb("r2_%d" % i, [128, D], F32) for i in range(2)]
    for t_ in yk:
        S.pool(lambda e, t_=t_: e.memset(t_[:], 0.0), w=[t_])
    gi = 0
    for ci in range(NCH):
        i2 = ci % 2
        S.dma("sp", r2[i2][:], res2_d[ci * 128:(ci + 1) * 128, :], r=[res2_b[ci]], w=[r2[i2]])
        fa = facc[i2]
        for k in range(8):
            yt_ = yk[gi % 4]
            gi += 1
            S.dma("pool", None, None, r=[ys_b, slot_u], w=[yt_],
                  fn=lambda e, k=k, yt_=yt_: e.indirect_dma_start(
                      out=yt_[:, :], out_offset=None, in_=ys_d[:, :],
                      in_offset=bass.IndirectOffsetOnAxis(ap=slot_u[:, ci, k:k + 1], axis=0),
                      bounds_check=reg_slot, oob_is_err=False))
            if k == 0:
                S.dve(lambda e, yt_=yt_, fa=fa: e.tensor_scalar(fa[:], yt_[:], wk_t[:, ci, 0:1], None, ALU.mult),
                      r=[yt_, wk_t], w=[fa])
            else:
                S.dve(lambda e, yt_=yt_, fa=fa, k=k: e.scalar_tensor_tensor(fa[:], yt_[:], wk_t[:, ci, k:k + 1], fa[:],
                                                                          ALU.mult, ALU.add), r=[yt_, wk_t, fa], w=[fa])
        S.dve(lambda e, fa=fa: e.tensor_tensor(fa[:], fa[:], g2_row[:], ALU.mult), r=[fa, g2_row], w=[fa])
        S.dve(lambda e, fa=fa, i2=i2: e.tensor_tensor(fa[:], fa[:], r2[i2][:], ALU.add), r=[fa, r2[i2]], w=[fa])
        layer_norm_stats(fa, LN_EPS)
        S.dve(lambda e, fa=fa: e.tensor_scalar(fa[:], fa[:], mv[:, 0:1], rstd[:, 0:1], ALU.subtract, ALU.mult),
              r=[fa, mv, rstd], w=[fa])
        S.dve(lambda e, fa=fa: e.tensor_tensor(fa[:], fa[:], ln2g_row[:], ALU.mult), r=[fa, ln2g_row], w=[fa])
        S.dve(lambda e, fa=fa: e.tensor_tensor(fa[:], fa[:], ln2b_row[:], ALU.add), r=[fa, ln2b_row], w=[fa])
        S.dma("sp", out[ci * 128:(ci + 1) * 128, :], fa[:], r=[fa])
    S.finish()
    S.barrier()
    ph3.close()
    return nc, S


_NAMES = ["w_ada", "b_ada", "w_in", "conv_w", "conv_b", "dt_bias", "a_log", "d_skip", "ssd_norm_w",
          "gmlp_ln_g", "gmlp_ln_b", "gmlp_ws", "gmlp_bs", "w_proj_ssd", "w_proj_gmlp", "w_out",
          "ln1_g", "ln1_b", "w_router", "router_bias", "w_e_gate", "w_e_up", "w_e_down",
          "w_sh_gate", "w_sh_up", "w_sh_down", "ln2_g", "ln2_b"]


def make_in_maps(inputs, NCH, NPV, seq_per_core=None):
    x = np.asarray(inputs["x"], dtype=np.float32)
    c = np.asarray(inputs["c"], dtype=np.float32)
    shared = {n: np.ascontiguousarray(np.asarray(inputs[n], dtype=np.float32)[0]) for n in _NAMES}
    for n in ("w_e_gate", "w_e_up", "w_e_down"):
        shared[n] = shared[n].reshape(256 * 128, 2048)
    maps = []
    ncores = 2 * x.shape[0]
    for core in range(ncores):
        b, half = core // 2, core % 2
        cur0 = half * NPV * 128 if NPV > 0 else 0
        m = dict(shared)
        m["x_cur"] = np.ascontiguousarray(x[b, cur0:cur0 + NCH * 128, :])
        m["x_prev"] = np.ascontiguousarray(x[b, 0:max(NPV, 1) * 128, :])
        m["flag"] = np.full((128, 1), float(half), dtype=np.float32)
        m["c_b"] = np.ascontiguousarray(c[b])
        maps.append(m)
    return maps


def kernel(**inputs):
    NCH, NPV, C = 32, 32, 256
    nc, _ = build(NCH, NPV, C)
    maps = make_in_maps(inputs, NCH, NPV)
    res = run_bass_kernel_spmd(nc, maps, core_ids=list(range(8)))
    x = np.asarray(inputs["x"])
    out = np.empty(x.shape, dtype=np.float32)
    for core in range(8):
        b, half = core // 2, core % 2
        out[b, half * NCH * 128:(half + 1) * NCH * 128, :] = res.results[core]["out"]
    return out
```

```python
import os
import numpy as np
from contextlib import ExitStack
import concourse.bass as bass
import concourse.mybir as mybir
from concourse.bass_utils import run_bass_kernel_spmd

F32 = mybir.dt.float32
BF16 = mybir.dt.bfloat16
U32 = mybir.dt.uint32
AF = mybir.ActivationFunctionType
ALU = mybir.AluOpType
AX = mybir.AxisListType

D = 1024
NIN = 10272
ALPHA = float(2.0 ** 0.25)
LN_EPS = 1e-5
RMS_EPS = 1e-5
GC = 0.7978845608028654
BIG = 1.0e4


class Buf:
    __slots__ = ("name", "w", "r")

    def __init__(self, name=""):
        self.name = name
        self.w = None
        self.r = {}


class T:
    def __init__(self, h, nsub=0, name=""):
        self.h = h
        self.b = Buf(name)
        self.subs = [Buf("%s.%d" % (name, i)) for i in range(nsub)]

    def __getitem__(self, k):
        return self.h[k]

    def s(self, i):
        return self.subs[i]


def _b(t):
    return t.b if isinstance(t, T) else t


class Sched:
    NDMA = 40

    def __init__(self, nc):
        self.nc = nc
        self.eng = {"pe": nc.tensor, "act": nc.scalar, "dve": nc.vector,
                    "pool": nc.gpsimd, "sp": nc.sync}
        self.sem = {k: nc.alloc_semaphore("q_" + k) for k in self.eng}
        self.cnt = {k: 0 for k in self.eng}
        self.waited = {k: {} for k in self.eng}
        self.dsem = [nc.alloc_semaphore("d%d" % i) for i in range(self.NDMA)]
        self.duse = [0] * self.NDMA
        self.dnext = 0
        self.nins = 0

    def _wait(self, e, ev):
        sem, val = ev
        if e == "pe" and sem is self.sem["pe"]:
            return
        w = self.waited[e]
        if w.get(sem.num, 0) >= val:
            return
        w[sem.num] = val
        self.eng[e].wait_ge(sem, val)
        self.nins += 1

    def _deps(self, e, reads, writes):
        for t in reads:
            b = _b(t)
            if b.w is not None:
                self._wait(e, b.w)
        for t in writes:
            b = _b(t)
            if b.w is not None:
                self._wait(e, b.w)
            for ev in b.r.values():
                self._wait(e, ev)

    def _commit(self, ev, reads, writes):
        sem, val = ev
        for t in reads:
            b = _b(t)
            old = b.r.get(sem.num)
            if old is None or old[1] < val:
                b.r[sem.num] = ev
        for t in writes:
            b = _b(t)
            b.w = ev
            b.r = {}

    def op(self, e, fn, r=(), w=()):
        self._deps(e, r, w)
        ins = fn(self.eng[e])
        self.cnt[e] += 1
        ins.then_inc(self.sem[e], 1)
        self._commit((self.sem[e], self.cnt[e]), r, w)
        self.nins += 1
        return ins

    def dve(self, fn, r=(), w=()):
        return self.op("dve", fn, r, w)

    def act(self, fn, r=(), w=()):
        return self.op("act", fn, r, w)

    def pool(self, fn, r=(), w=()):
        return self.op("pool", fn, r, w)

    def mm(self, out, pairs, r=(), w=(), first=True, last=True):
        self._deps("pe", r, w)
        n = len(pairs)
        ins = None
        for i, (l, rh) in enumerate(pairs):
            ins = self.nc.tensor.matmul(out, l, rh, start=(first and i == 0), stop=(last and i == n - 1))
        self.cnt["pe"] += 1
        ins.then_inc(self.sem["pe"], 1)
        self._commit((self.sem["pe"], self.cnt["pe"]), r, w)
        self.nins += n

    def tr(self, out, in_, ident, r=(), w=()):
        return self.op("pe", lambda e: e.transpose(out, in_, ident), r, w)

    def dma(self, q, out, in_, r=(), w=(), fn=None):
        slot = self.dnext
        self.dnext = (self.dnext + 1) % self.NDMA
        sem = self.dsem[slot]
        if self.duse[slot] > 0:
            self._wait(q, (sem, 16 * self.duse[slot]))
        self._deps(q, r, w)
        if fn is None:
            ins = self.eng[q].dma_start(out=out, in_=in_)
        else:
            ins = fn(self.eng[q])
        ins.then_inc(sem, 16)
        self.duse[slot] += 1
        self._commit((sem, 16 * self.duse[slot]), r, w)
        self.nins += 1
        return ins

    def barrier(self):
        for e in self.eng:
            for e2 in self.eng:
                if e2 != e and self.cnt[e2] > 0:
                    self._wait(e, (self.sem[e2], self.cnt[e2]))
            for i in range(self.NDMA):
                if self.duse[i] > 0:
                    self._wait(e, (self.dsem[i], 16 * self.duse[i]))

    def finish(self):
        for i in range(self.NDMA):
            if self.duse[i] > 0:
                self._wait("sp", (self.dsem[i], 16 * self.duse[i]))


def build(NCH, NPV, C, debug=False):
    nc = bass.Bass("TRN2", target_bir_lowering=False)
    S = Sched(nc)
    NEB = NCH * 8 + 256
    NSLOT = NEB * 128

    def din(name, shape, dt=F32):
        return nc.dram_tensor(name, list(shape), dt, kind="ExternalInput").ap()

    x_cur = din("x_cur", [NCH * 128, D])
    x_prev = din("x_prev", [max(NPV, 1) * 128, D])
    flag = din("flag", [128, 1])
    c_b = din("c_b", [D])
    w_ada = din("w_ada", [D, 6 * D])
    b_ada = din("b_ada", [6 * D])
    w_in = din("w_in", [D, NIN])
    conv_w = din("conv_w", [4, 4096])
    conv_b = din("conv_b", [4096])
    dt_bias = din("dt_bias", [32])
    a_log = din("a_log", [32])
    d_skip = din("d_skip", [32])
    ssd_norm_w = din("ssd_norm_w", [2048])
    gmlp_ln_g = din("gmlp_ln_g", [D])
    gmlp_ln_b = din("gmlp_ln_b", [D])
    gmlp_ws = din("gmlp_ws", [8, 128, 128])
    gmlp_bs = din("gmlp_bs", [8, 128])
    w_proj_ssd = din("w_proj_ssd", [2048, D])
    w_proj_gmlp = din("w_proj_gmlp", [D, D])
    w_out = din("w_out", [D, D])
    ln1_g = din("ln1_g", [D])
    ln1_b = din("ln1_b", [D])
    w_router = din("w_router", [D, 256])
    router_bias = din("router_bias", [256])
    w_e_gate = din("w_e_gate", [256 * 128, 2048])
    w_e_up = din("w_e_up", [256 * 128, 2048])
    w_e_down = din("w_e_down", [256 * 128, 2048])
    w_sh_gate = din("w_sh_gate", [D, 256])
    w_sh_up = din("w_sh_up", [D, 256])
    w_sh_down = din("w_sh_down", [256, D])
    ln2_g = din("ln2_g", [D])
    ln2_b = din("ln2_b", [D])
    out = nc.dram_tensor("out", [NCH * 128, D], F32, kind="ExternalOutput").ap()
    dbg = None
    if debug:
        dbg = nc.dram_tensor("dbg", [NCH * 128, D], F32, kind="ExternalOutput").ap()

    blocks = []

    def wv(w, k):
        return w.rearrange("(p k) c -> p k c", k=k)

    win_v = wv(w_in, 8)
    for i in range(8):
        blocks.append(("xbc%d" % i, [(win_v[:, :, 2048 + 512 * i:2048 + 512 * (i + 1)], 0)], 8, 512))
    blocks.append(("dt", [(win_v[:, :, 6144:6176], 0)], 8, 32))
    for i in range(4):
        blocks.append(("z%d" % i, [(win_v[:, :, 512 * i:512 * (i + 1)], 0)], 8, 512))
    for i in range(4):
        blocks.append(("uv%d" % i, [(win_v[:, :, 6176 + 512 * i:6176 + 512 * (i + 1)], 0)], 8, 512))
    for i in range(2):
        blocks.append(("ga%d" % i, [(win_v[:, :, 8224 + 512 * i:8224 + 512 * (i + 1)], 0)], 8, 512))
    for i in range(2):
        blocks.append(("gb%d" % i, [(win_v[:, :, 9248 + 512 * i:9248 + 512 * (i + 1)], 0)], 8, 512))
    wps_v = wv(w_proj_ssd, 16)
    for ch in range(2):
        for kh in range(2):
            blocks.append(("ps%d%d" % (ch, kh), [(wps_v[:, kh * 8:(kh + 1) * 8, ch * 512:(ch + 1) * 512], 0)], 8, 512))
    wpg_v = wv(w_proj_gmlp, 8)
    for ch in range(2):
        blocks.append(("pg%d" % ch, [(wpg_v[:, :, ch * 512:(ch + 1) * 512], 0)], 8, 512))
    wo_v = wv(w_out, 8)
    for ch in range(2):
        blocks.append(("wo%d" % ch, [(wo_v[:, :, ch * 512:(ch + 1) * 512], 0)], 8, 512))
    blocks.append(("rt", [(wv(w_router, 8), 0)], 8, 256))
    blocks.append(("sgu", [(wv(w_sh_gate, 8), 0), (wv(w_sh_up, 8), 256)], 8, 512))
    blocks.append(("sd", [(w_sh_down.rearrange("(b q) c -> q b c", q=128), 0)], 2, 1024))
    bidx = {b[0]: i for i, b in enumerate(blocks)}
    NBLK = len(blocks)
    wblk = nc.dram_tensor("wblk", [NBLK, 128, 4096], BF16, kind="Internal").ap()
    wblk_b = [Buf("wblk%d" % i) for i in range(NBLK)]

    xs_d = nc.dram_tensor("xs_d", [NSLOT, D], BF16, kind="Internal").ap()
    ys_d = nc.dram_tensor("ys_d", [NSLOT, D], BF16, kind="Internal").ap()
    res2_d = nc.dram_tensor("res2_d", [NCH * 128, D], F32, kind="Internal").ap()
    m2_d = nc.dram_tensor("m2_d", [NCH * 128, D], BF16, kind="Internal").ap()
    m2_b = [Buf("m2d_%d" % i) for i in range(NCH)]
    xs_b = Buf("xs_d")
    ys_b = Buf("ys_d")
    res2_b = [Buf("res2_%d" % i) for i in range(NCH)]

    reg_slot = nc.gpsimd.to_reg(NSLOT - 1)
    reg_w = nc.gpsimd.to_reg(256 * 128 - 1)

    def sb(name, shape, dt, nsub=0):
        if scope[0] is None:
            return T(nc.alloc_sbuf_tensor(name, list(shape), dt), nsub, name)
        return T(scope[0].enter_context(nc.sbuf_tensor(name, list(shape), dt)), nsub, name)

    scope = [None]

    banks = [T(nc.alloc_psum_tensor("bank%d" % i, [128, 512], F32), 0, "bank%d" % i) for i in range(8)]
    bank_i = [0]

    pinned = set()

    def ps():
        while (bank_i[0] % 8) in pinned:
            bank_i[0] += 1
        t = banks[bank_i[0] % 8]
        bank_i[0] += 1
        return t

    def psb(t):
        return t[:].bitcast(BF16)

    ident = sb("ident", [128, 128], BF16)
    identf = sb("identf", [128, 128], F32)
    Uf = sb("Uf", [128, 128], F32)
    Ub = sb("Ub", [128, 128], BF16)
    SLb = sb("SLb", [128, 128], BF16)
    SUb = sb("SUb", [128, 128], BF16)
    onesb = sb("onesb", [128, 128], BF16)
    iota_e = sb("iota_e", [128, 256], F32)
    mhalf = sb("mhalf", [128, 1], F32)
    WsT = sb("WsT", [128, 8, 128], BF16)
    bsT = sb("bsT", [128, 8], F32)
    Dg = sb("Dg", [128, 32, 4, 128], BF16)
    brow = sb("brow", [1, 4096], BF16)
    a_row = sb("a_row", [128, 32], F32)
    dtb_row = sb("dtb_row", [128, 32], F32)
    dsk32 = sb("dsk32", [128, 32], F32)
    normw_pk = sb("normw_pk", [128, 16], F32)
    sh1_pk = sb("sh1_pk", [128, 8], F32)
    sc1_pk = sb("sc1_pk", [128, 8], F32)
    lng_row = sb("lng_row", [128, D], BF16)
    lnb_row = sb("lnb_row", [128, D], BF16)
    ln1g_row = sb("ln1g_row", [128, D], BF16)
    ln1b_row = sb("ln1b_row", [128, D], BF16)
    sc2_row = sb("sc2_row", [128, D], BF16)
    sh2_row = sb("sh2_row", [128, D], BF16)
    g1h_row = sb("g1h_row", [128, D], BF16)
    g2h_row = sb("g2h_row", [128, D], BF16)
    rb_row = sb("rb_row", [128, 256], F32)
    flag_t = sb("flag_t", [128, 1], F32)
    st6 = sb("st6", [128, 12], F32)
    mv = sb("mv", [128, 2], F32)
    rstd = sb("rstd", [128, 1], F32)
    nbias = sb("nbias", [128, 1], F32)
    slot_u = sb("slot_u", [128, NCH, 8], U32)
    wk_t = sb("wk_t", [128, NCH, 8], F32)
    Rcnt = sb("Rcnt", [128, 256], BF16)
    idx_all = sb("idx_all", [128, NCH, 8], F32)
    pos_all = sb("pos_all", [128, NCH, 8], F32)
    Sst = sb("Sst", [128, 2048], F32)
    Sbf = sb("Sbf", [128, 2048], BF16)
    xBCT = sb("xBCT", [128, 32, 131], BF16, nsub=8)

    def aff(t, pattern, cm, op, fill, r=(), extra_w=()):
        S.pool(lambda e: e.affine_select(out=t[:], in_=t[:], pattern=pattern, compare_op=op, fill=fill,
                                         base=0, channel_multiplier=cm), r=[t], w=[t])

    S.pool(lambda e: e.memset(identf[:], 0.0), w=[identf])
    aff(identf, [[1, 128]], -1, ALU.not_equal, 1.0)
    S.dve(lambda e: e.tensor_copy(ident[:], identf[:]), r=[identf], w=[ident])
    S.pool(lambda e: e.memset(Uf[:], 1.0), w=[Uf])
    aff(Uf, [[1, 128]], -1, ALU.is_ge, 0.0)
    S.dve(lambda e: e.tensor_copy(Ub[:], Uf[:]), r=[Uf], w=[Ub])
    tmpf = sb("tmpf", [128, 128], F32)
    S.pool(lambda e: e.memset(tmpf[:], 1.0), w=[tmpf])
    aff(tmpf, [[-1, 128]], 1, ALU.is_gt, 0.0)
    S.dve(lambda e: e.tensor_copy(SLb[:], tmpf[:]), r=[tmpf], w=[SLb])
    S.pool(lambda e: e.memset(tmpf[:], 1.0), r=[], w=[tmpf])
    aff(tmpf, [[1, 128]], -1, ALU.is_gt, 0.0)
    S.dve(lambda e: e.tensor_copy(SUb[:], tmpf[:]), r=[tmpf], w=[SUb])
    S.pool(lambda e: e.memset(onesb[:], 1.0), w=[onesb])
    S.pool(lambda e: e.iota(iota_e[:], pattern=[[1, 256]], base=0, channel_multiplier=0,
                            allow_small_or_imprecise_dtypes=True), w=[iota_e])
    S.pool(lambda e: e.memset(mhalf[:], -0.5), w=[mhalf])
    S.pool(lambda e: e.memset(Sst[:], 0.0), w=[Sst])
    S.pool(lambda e: e.memset(Sbf[:], 0.0), w=[Sbf])
    S.pool(lambda e: e.memset(xBCT[:], 0.0), w=[xBCT] + xBCT.subs)
    S.pool(lambda e: e.memset(Rcnt[:], 0.0), w=[Rcnt])
    S.pool(lambda e: e.memset(wk_t[:], 0.0), w=[wk_t])

    S.dma("sp", flag_t[:], flag, w=[flag_t])
    for row, src in ((lng_row, gmlp_ln_g), (lnb_row, gmlp_ln_b), (ln1g_row, ln1_g), (ln1b_row, ln1_b)):
        S.dma("pool", row[:], src.partition_broadcast(128), w=[row])
    S.dma("sp", rb_row[:], router_bias.partition_broadcast(128), w=[rb_row])
    S.dma("sp", dtb_row[:], dt_bias.partition_broadcast(128), w=[dtb_row])
    S.dma("sp", a_row[:], a_log.partition_broadcast(128), w=[a_row])
    S.dma("sp", normw_pk[:], ssd_norm_w.rearrange("(p k) -> p k", k=16), w=[normw_pk])
    S.act(lambda e: e.activation(a_row[:], a_row[:], AF.Exp), r=[a_row], w=[a_row])
    S.dve(lambda e: e.tensor_scalar_mul(a_row[:], a_row[:], -1.0), r=[a_row], w=[a_row])

    with ExitStack() as st1:
        scope[0] = st1
        stg = [sb("stg%d" % i, [128, 4096], BF16) for i in range(3)]
        cw4 = sb("cw4", [4, 4096], F32)
        cb1 = sb("cb1", [1, 4096], F32)
        wsl = sb("wsl", [128, 8, 128], F32)
        wslb = sb("wslb", [128, 8, 128], BF16)
        bs8 = sb("bs8", [8, 128], F32)
        wcol = sb("wcol", [128, 32, 4], F32)

        S.pool(lambda e: e.memset(stg[2][:], 0.0), w=[stg[2]])
        xs_z = xs_d.rearrange("(b p i) d -> b p (i d)", p=128, i=4)
        for b in range(NSLOT // 512):
            S.dma("sp", xs_z[b], stg[2][:], r=[stg[2]], w=[xs_b])

        for i, (name, srcs, K, N) in enumerate(blocks):
            st = stg[i % 3]
            v = st[:, 0:K * N].rearrange("p (k n) -> p k n", k=K)
            for (src, co) in srcs:
                n = src.shape[2]
                S.dma("pool", v[:, :, co:co + n], src, w=[st])
            S.dma("sp", wblk[i, :, 0:K * N], st[:, 0:K * N], r=[st], w=[wblk_b[i]])

        S.dma("sp", dsk32[:], d_skip.partition_broadcast(128), w=[dsk32])
        S.dma("sp", cw4[:], conv_w, w=[cw4])
        pw = ps()
        pwv = pw[:, 0:128].rearrange("p (j k) -> p j k", k=4)
        for j in range(32):
            S.mm(pwv[:, j, :], [(cw4[0:4, j * 128:(j + 1) * 128], identf[0:4, 0:4])], r=[cw4, identf], w=[pw])
        S.dve(lambda e: e.tensor_scalar_mul(wcol[:], pwv, 0.5), r=[pw], w=[wcol])
        for j in range(32):
            for k in range(4):
                eng = "dve" if (j + k) % 2 == 0 else "pool"
                S.op(eng, lambda e, j=j, k=k: e.tensor_scalar(Dg[:, j, k, :], identf[:], wcol[:, j, k:k + 1], None,
                                                              ALU.mult), r=[identf, wcol], w=[Dg])
        S.dma("sp", cb1[:], conv_b.rearrange("(o c) -> o c", o=1), w=[cb1])
        S.dve(lambda e: e.tensor_scalar_mul(brow[:], cb1[:], 0.5), r=[cb1], w=[brow])
        S.dma("sp", wsl[:], gmlp_ws.rearrange("g t s -> t g s"), w=[wsl])
        S.pool(lambda e: e.affine_select(out=wsl[:], in_=wsl[:], pattern=[[0, 8], [-1, 128]], compare_op=ALU.is_ge,
                                         fill=0.0, base=0, channel_multiplier=1), r=[wsl], w=[wsl])
        S.dve(lambda e: e.tensor_copy(wslb[:], wsl[:]), r=[wsl], w=[wslb])
        pt = ps()
        ptv = psb(pt).rearrange("p (g t) -> p g t", g=8)
        for g in range(8):
            S.tr(ptv[:, g, :], wslb[:, g, :], ident[:], r=[wslb, ident], w=[pt])
        S.dve(lambda e: e.tensor_copy(WsT[:], ptv), r=[pt], w=[WsT])
        S.dma("sp", bs8[:], gmlp_bs, w=[bs8])
        pb = ps()
        S.mm(pb[:, 0:8], [(bs8[0:8, :], identf[0:8, 0:8])], r=[bs8, identf], w=[pb])
        S.dve(lambda e: e.tensor_copy(bsT[:], pb[:, 0:8]), r=[pb], w=[bsT])

        S.barrier()
    with ExitStack() as st2:
        scope[0] = st2
        wa = [sb("wa%d" % i, [128, 8, 512], F32) for i in range(2)]
        bad = [sb("bad%d" % i, [128, 512], F32) for i in range(2)]
        adarow = sb("adarow", [128, 6 * D], F32)
        screp = sb("screp", [128, 8, 128], F32)
        dtmp = sb("dtmp", [128, 128, 8], F32)
        cpk = sb("cpk", [128, 8], F32)
        cpk2 = sb("cpk2", [128, 8], F32)
        S.dma("sp", cpk[:], c_b.rearrange("(p k) -> p k", k=8), w=[cpk])
        S.act(lambda e: e.activation(cpk2[:], cpk[:], AF.Tanh, scale=0.5), r=[cpk], w=[cpk2])
        S.dve(lambda e: e.scalar_tensor_tensor(cpk2[:], cpk2[:], 1.0, cpk[:], ALU.add, ALU.mult), r=[cpk2, cpk], w=[cpk2])
        S.dve(lambda e: e.tensor_scalar_mul(cpk2[:], cpk2[:], 0.5), r=[cpk2], w=[cpk2])
        S.dve(lambda e: e.tensor_copy(screp[:], cpk2[:].unsqueeze(2).to_broadcast([128, 8, 128])), r=[cpk2], w=[screp])
        wada_v = wv(w_ada, 8)
        for b in range(12):
            wt = wa[b % 2]
            S.dma("sp", wt[:], wada_v[:, :, b * 512:(b + 1) * 512], w=[wt])
            pa = ps()
            S.mm(pa[:], [(screp[:, k, :], wt[:, k, :]) for k in range(8)], r=[screp, wt], w=[pa])
            bd = bad[b % 2]
            S.dma("sp", bd[:], b_ada[b * 512:(b + 1) * 512].partition_broadcast(128), w=[bd])
            S.dve(lambda e, b=b, pa=pa, bd=bd: e.tensor_tensor(adarow[:, b * 512:(b + 1) * 512], pa[:], bd[:], ALU.add),
                  r=[pa, bd], w=[adarow])
        S.dve(lambda e: e.tensor_copy(sh2_row[:], adarow[:, 3 * D:4 * D]), r=[adarow], w=[sh2_row])
        S.dve(lambda e: e.tensor_scalar_add(sc2_row[:], adarow[:, 4 * D:5 * D], 1.0), r=[adarow], w=[sc2_row])
        S.dve(lambda e: e.tensor_scalar_mul(g1h_row[:], adarow[:, 2 * D:3 * D], 0.5), r=[adarow], w=[g1h_row])
        S.dve(lambda e: e.tensor_scalar_mul(g2h_row[:], adarow[:, 5 * D:6 * D], 0.5), r=[adarow], w=[g2h_row])
        for (dst, off, add1) in ((sh1_pk, 0, 0.0), (sc1_pk, D, 1.0)):
            S.dve(lambda e, off=off: e.tensor_tensor(dtmp[:], adarow[:, off:off + D].rearrange("p (q k) -> p q k", k=8),
                                                    identf[:].unsqueeze(2).to_broadcast([128, 128, 8]), ALU.mult),
                  r=[adarow, identf], w=[dtmp])
            S.dve(lambda e, dst=dst: e.tensor_reduce(dst[:], dtmp[:].rearrange("p q k -> p k q"), AX.X, ALU.add),
                  r=[dtmp], w=[dst])
            if add1:
                S.dve(lambda e, dst=dst: e.tensor_scalar_add(dst[:], dst[:], 1.0), r=[dst], w=[dst])
        S.barrier()
    scope[0] = None

    ph1 = ExitStack()
    scope[0] = ph1
    NB = 3
    ring = [sb("ring%d" % i, [128, 4096], BF16) for i in range(NB)]
    worder = []
    wstate = {"issued": 0, "next": 0}

    def chunk_blocks(kind, last=False):
        if kind == "A":
            l = ["xbc0", "xbc1", "xbc2", "xbc3", "xbc4", "xbc5"]
            if last:
                l += ["xbc6", "xbc7"]
            return l + ["dt"]
        return (["xbc%d" % i for i in range(8)] + (["rt", "sgu", "sd"] if not last else []) + ["dt"] +
                ["z0", "z1", "uv0", "uv1", "z2", "uv2", "uv3", "z3", "ga0", "ga1", "gb0", "gb1"] +
                ["ps00", "ps01", "ps10", "ps11", "pg0", "pg1", "wo0", "wo1"])

    for i in range(NPV):
        worder.extend(chunk_blocks("A", i == NPV - 1))
    for i in range(NCH):
        worder.extend(chunk_blocks("M", last=(i == 0)))
    worder.extend(["rt", "sgu", "sd"])

    def wget(name):
        i = wstate["next"]
        assert worder[i] == name, (worder[i], name)
        while wstate["issued"] < min(len(worder), i + NB):
            j = wstate["issued"]
            bi = bidx[worder[j]]
            K, N = blocks[bi][2], blocks[bi][3]
            rt = ring[j % NB]
            S.dma("sp", rt[:, 0:K * N], wblk[bi, :, 0:K * N], r=[wblk_b[bi]], w=[rt])
            wstate["issued"] += 1
        wstate["next"] += 1
        rt = ring[i % NB]
        bi = bidx[name]
        K, N = blocks[bi][2], blocks[bi][3]
        return rt, rt[:, 0:K * N].rearrange("p (k n) -> p k n", k=K)

    xts = [sb("xt0", [128, D], F32), sb("xt1", [128, D], F32)]
    mT = sb("mT", [128, 8, 128], BF16)
    xc = sb("xc", [128, 32, 128], BF16, nsub=8)
    tnh = [sb("tnh%d" % i, [128, 512], F32) for i in range(2)]
    x_tm = sb("x_tm", [128, 2048], BF16)
    xdte = x_tm
    B_tm = sb("B_tm", [128, 1024], BF16)
    xd_tm = sb("xd_tm", [128, 2048], BF16)
    xdt = sb("xdt", [128, 2048], BF16)
    dtt = sb("dtt", [128, 32], F32)
    dtx = sb("dtx", [128, 32], F32)
    adt = sb("adt", [128, 32], F32)
    adt_hi = sb("adt_hi", [128, 32], BF16)
    adt_lo = sb("adt_lo", [128, 32], BF16)
    adt_r = sb("adt_r", [128, 32], F32)
    acs = sb("acs", [128, 32], F32)
    ea = sb("ea", [128, 32], F32)
    eend = sb("eend", [128, 32], F32)
    dec = sb("dec", [128, 32], F32)
    cbm = [sb("cbm%d" % i, [128, 128], F32) for i in range(2)]
    segr = [sb("segr%d" % i, [128, 4, 128], BF16) for i in range(2)]
    Eg = [sb("Eg%d" % i, [128, 4, 128], BF16) for i in range(2)]
    WTg = [sb("WTg%d" % i, [128, 4, 128], BF16) for i in range(2)]
    ytmp = [sb("ytmp%d" % i, [128, 256], F32) for i in range(2)]
    zg = sb("zg", [128, 2048], BF16, nsub=4)
    yg = sb("yg", [128, 2048], BF16)
    ssq = sb("ssq", [128, 8], F32)
    rinv = sb("rinv", [128, 1], F32)
    gA = sb("gA", [128, D], BF16, nsub=2)
    gB = sb("gB", [128, D], BF16, nsub=2)
    u_t = sb("u_t", [128, D], BF16, nsub=2)
    v_t = sb("v_t", [128, D], F32, nsub=2)
    vb = sb("vb", [128, D], BF16)
    gm = sb("gm", [128, D], BF16)
    gmT = sb("gmT", [128, 8, 128], BF16)
    mg = gm
    mgT = gmT
    mtmp = [sb("mtmp%d" % i, [128, 512], F32) for i in range(2)]
    h1 = sb("h1", [128, D], F32)
    m2 = sb("m2", [128, D], BF16)
    xn = sb("xn", [128, D], BF16)
    m2T = sb("m2T", [128, 8, 128], BF16)
    hsa = sb("hsa", [128, 256], F32)
    hs = sb("hs", [128, 2, 128], BF16)
    sco = sb("sco", [128, 256], F32)
    cho = sb("cho", [128, 256], F32)
    g8 = sb("g8", [128, 8, 8], F32)
    gs = sb("gs", [128, 8], F32)
    gs8 = sb("gs8", [128, 8], F32)
    gpen = sb("gpen", [128, 8], F32)
    top8 = sb("top8", [128, 8], F32)
    idx8 = sb("idx8", [128, 8], U32)
    idxf = sb("idxf", [128, 8], F32)
    selb = sb("selb", [128, 256], BF16)
    posf = sb("posf", [128, 256], F32)
    junk = sb("junk", [128, 256], F32)
    sqj = junk
    pk8 = sb("pk8", [128, 8], F32)
    sk8 = sb("sk8", [128, 8], F32)
    slf = sb("slf", [128, 8], F32)
    ssum = sb("ssum", [128, 1], F32)

    def layer_norm_stats(src, eps):
        S.dve(lambda e: e.bn_stats(st6[:, 0:6], src[:, 0:512]), r=[src], w=[st6])
        S.dve(lambda e: e.bn_stats(st6[:, 6:12], src[:, 512:1024]), r=[src], w=[st6])
        S.dve(lambda e: e.bn_aggr(mv[:], st6[:]), r=[st6], w=[mv])
        S.dve(lambda e: e.tensor_scalar_add(rstd[:], mv[:, 1:2], eps), r=[mv], w=[rstd])
        S.pool(lambda e: e.tensor_tensor(rstd[:], rstd[:], mhalf[:], ALU.pow), r=[rstd, mhalf], w=[rstd])

    def ln_apply(dst_ap, src_ap, r, w):
        S.act(lambda e: e.activation(dst_ap, src_ap, AF.Identity, bias=nbias[:, 0:1], scale=rstd[:, 0:1]),
              r=list(r) + [rstd, nbias], w=w)

    def transposes8(src, dst, k):
        sv = src[:].rearrange("t (p k) -> t k p", k=k)
        for b0 in range(0, k, 8):
            pt = ps()
            ptv = psb(pt).rearrange("p (g t) -> p g t", g=8)
            for kk in range(8):
                S.tr(ptv[:, kk, :], sv[:, b0 + kk, :], ident[:], r=[src, ident], w=[pt])
            yield b0, pt, ptv

    xstate = {"loaded": False}
    pending = []

    def chunk(ci, xsrc, mode, last_prev=False, next_src=None, part="all", hoist=None):
        full = (mode == "M")
        xt = xts[ci % 2] if full else xts[0]
        nxb = 8 if (full or last_prev) else 6
        if part != "rest":
            chunk_head(ci, xsrc, full, next_src, xt, nxb)
        if part == "head":
            return
        chunk_rest(ci, mode, full, xt, nxb, hoist)

    def chunk_head(ci, xsrc, full, next_src, xt, nxb):
        if not xstate["loaded"]:
            S.dma("sp", xt[:], xsrc, w=[xt])
        xstate["loaded"] = False
        layer_norm_stats(xt, LN_EPS)
        S.dve(lambda e: e.tensor_scalar(xn[:], xt[:], mv[:, 0:1], rstd[:, 0:1], ALU.subtract, ALU.mult),
              r=[xt, mv, rstd], w=[xn])
        if not full and next_src is not None:
            S.dma("sp", xt[:], next_src, w=[xt])
            xstate["loaded"] = True
        for b0, pt, ptv in transposes8(xn, mT, 8):
            S.dve(lambda e, ptv=ptv: e.tensor_tensor(mT[:], ptv, sc1_pk[:].unsqueeze(2).to_broadcast([128, 8, 128]),
                                                    ALU.mult), r=[pt, sc1_pk], w=[mT])
            S.dve(lambda e: e.tensor_tensor(mT[:], mT[:], sh1_pk[:].unsqueeze(2).to_broadcast([128, 8, 128]),
                                            ALU.add), r=[mT, sh1_pk], w=[mT])
        for i in range(nxb):
            rt, wv_ = wget("xbc%d" % i)
            pb_ = ps()
            for sub in range(4):
                S.mm(pb_[:, sub * 128:(sub + 1) * 128],
                     [(wv_[:, k, sub * 128:(sub + 1) * 128], mT[:, k, :]) for k in range(8)],
                     r=[rt, mT], w=[pb_])
            S.act(lambda e, i=i, pb_=pb_: e.copy(xBCT[:, 4 * i:4 * i + 4, 3:131],
                                                pb_[:].rearrange("p (j t) -> p j t", j=4)),
                  r=[pb_], w=[xBCT.s(i)])

    def chunk_rest(ci, mode, full, xt, nxb, hoist):
        if full and pending:
            pending.pop()()
        for i in range(nxb):
            pc = ps()
            for sub in range(4):
                j = 4 * i + sub
                S.mm(pc[:, sub * 128:(sub + 1) * 128],
                     [(Dg[:, j, k, :], xBCT[:, j, k:k + 128]) for k in range(4)] +
                     [(brow[0:1, j * 128:(j + 1) * 128], onesb[0:1, :])],
                     r=[Dg, xBCT.s(i), brow, onesb], w=[pc])
            th = tnh[i % 2]
            S.act(lambda e, th=th, pc=pc: e.activation(th[:], pc[:], AF.Tanh), r=[pc], w=[th])
            S.dve(lambda e, i=i, th=th, pc=pc: e.scalar_tensor_tensor(
                xc[:, 4 * i:4 * i + 4, :].rearrange("p j t -> p (j t)"), th[:], 1.0, pc[:], ALU.add, ALU.mult),
                r=[th, pc], w=[xc.s(i)])
        S.pool(lambda e: e.tensor_copy(xBCT[:, :, 0:3], xBCT[:, :, 128:131]),
               r=[xBCT] + xBCT.subs, w=[xBCT] + xBCT.subs)
        for i in range(6):
            pt = ps()
            ptv = psb(pt)[:, 0:512]
            for sub in range(4):
                j = 4 * i + sub
                S.tr(ptv[:, sub * 128:(sub + 1) * 128], xc[:, j, :], ident[:], r=[xc.s(i), ident], w=[pt])
            if i < 4:
                S.act(lambda e, i=i, ptv=ptv: e.copy(x_tm[:, i * 512:(i + 1) * 512], ptv), r=[pt], w=[x_tm])
            else:
                S.act(lambda e, i=i, ptv=ptv: e.copy(B_tm[:, (i - 4) * 512:(i - 3) * 512], ptv), r=[pt], w=[B_tm])
        def tap(name, src, n=D, rr=()):
            if debug == name and full:
                S.dve(lambda e: e.tensor_copy(h1[:, 0:n], src), r=list(rr), w=[h1])
                S.dma("sp", dbg[ci * 128:(ci + 1) * 128, :], h1[:], r=[h1])
        tap("x_tm", x_tm[:, 0:D], rr=[x_tm])
        tap("B_tm", B_tm[:, 0:D], rr=[B_tm])
        rt, wv_ = wget("dt")
        pd = ps()
        S.mm(pd[:, 0:32], [(mT[:, k, :], wv_[:, k, :]) for k in range(8)], r=[rt, mT], w=[pd])
        S.dve(lambda e: e.tensor_tensor(dtx[:], pd[:, 0:32], dtb_row[:], ALU.add), r=[pd, dtb_row], w=[dtx])
        S.act(lambda e: e.activation(dtx[:], dtx[:], AF.Exp), r=[dtx], w=[dtx])
        S.act(lambda e: e.activation(dtt[:], dtx[:], AF.Ln, bias=1.0), r=[dtx], w=[dtt])
        S.dve(lambda e: e.tensor_tensor(adt[:], dtt[:], a_row[:], ALU.mult), r=[dtt, a_row], w=[adt])
        S.dve(lambda e: e.tensor_copy(adt_hi[:], adt[:]), r=[adt], w=[adt_hi])
        S.dve(lambda e: e.tensor_tensor(adt_r[:], adt[:], adt_hi[:], ALU.subtract), r=[adt, adt_hi], w=[adt_r])
        S.dve(lambda e: e.tensor_copy(adt_lo[:], adt_r[:]), r=[adt_r], w=[adt_lo])
        pa_ = ps()
        S.mm(pa_[:, 0:32], [(Ub[:], adt_hi[:]), (Ub[:], adt_lo[:])], r=[Ub, adt_hi, adt_lo], w=[pa_])
        S.mm(pa_[:, 32:64], [(onesb[:], adt_hi[:]), (onesb[:], adt_lo[:])], r=[onesb, adt_hi, adt_lo], w=[pa_])
        S.act(lambda e: e.copy(acs[:], pa_[:, 0:32]), r=[pa_], w=[acs])
        S.act(lambda e: e.activation(ea[:], pa_[:, 0:32], AF.Exp), r=[pa_], w=[ea])
        S.act(lambda e: e.activation(dec[:], pa_[:, 32:64], AF.Exp), r=[pa_], w=[dec])
        S.dve(lambda e: e.tensor_tensor(eend[:], pa_[:, 32:64], acs[:], ALU.subtract), r=[pa_, acs], w=[eend])
        S.act(lambda e: e.activation(eend[:], eend[:], AF.Exp), r=[eend], w=[eend])
        x3 = x_tm[:].rearrange("p (h q) -> p h q", q=64)
        S.dve(lambda e: e.tensor_tensor(xdt[:].rearrange("p (h q) -> p h q", q=64), x3,
                                        dtt[:].unsqueeze(2).to_broadcast([128, 32, 64]), ALU.mult),
              r=[x_tm, dtt], w=[xdt])
        if full:
            S.pool(lambda e: e.tensor_tensor(xd_tm[:].rearrange("p (h q) -> p h q", q=64), x3,
                                             dsk32[:].unsqueeze(2).to_broadcast([128, 32, 64]), ALU.mult),
                   r=[x_tm, dsk32], w=[xd_tm])
        S.pool(lambda e: e.tensor_tensor(xdte[:].rearrange("p (h q) -> p h q", q=64),
                                        xdt[:].rearrange("p (h q) -> p h q", q=64),
                                        eend[:].unsqueeze(2).to_broadcast([128, 32, 64]), ALU.mult),
              r=[xdt, eend], w=[xdte])
        if full:
            def zblk(i):
                rt, wv_ = wget("z%d" % i)
                pz = ps()
                S.mm(pz[:], [(mT[:, k, :], wv_[:, k, :]) for k in range(8)], r=[rt, mT], w=[pz])
                th = tnh[i % 2]
                S.act(lambda e: e.activation(th[:], pz[:], AF.Tanh, scale=0.5), r=[pz], w=[th])
                S.dve(lambda e: e.scalar_tensor_tensor(zg[:, i * 512:(i + 1) * 512], th[:], 1.0, pz[:],
                                                       ALU.add, ALU.mult), r=[th, pz], w=[zg.s(i)])

            def uvblk(i):
                rt, wv_ = wget("uv%d" % i)
                pu = ps()
                S.mm(pu[:], [(mT[:, k, :], wv_[:, k, :]) for k in range(8)], r=[rt, mT], w=[pu])
                th = tnh[i % 2]
                mt = mtmp[i % 2]
                S.act(lambda e: e.activation(th[:], pu[:], AF.Square), r=[pu], w=[th])
                S.dve(lambda e: e.tensor_scalar(th[:], th[:], 0.044715 * GC, GC, ALU.mult, ALU.add), r=[th], w=[th])
                S.dve(lambda e: e.tensor_tensor(mt[:], th[:], pu[:], ALU.mult), r=[th, pu], w=[mt])
                S.act(lambda e: e.activation(mt[:], mt[:], AF.Tanh), r=[mt], w=[mt])
                if i < 2:
                    S.dve(lambda e: e.scalar_tensor_tensor(u_t[:, i * 512:(i + 1) * 512], mt[:], 1.0, pu[:],
                                                           ALU.add, ALU.mult), r=[mt, pu], w=[u_t.s(i)])
                else:
                    S.dve(lambda e: e.scalar_tensor_tensor(v_t[:, (i - 2) * 512:(i - 1) * 512], mt[:], 1.0, pu[:],
                                                           ALU.add, ALU.mult), r=[mt, pu], w=[v_t.s(i - 2)])

            def gblk(i):
                rt, wv_ = wget(("ga%d" % i) if i < 2 else ("gb%d" % (i - 2)))
                pg_ = ps()
                S.mm(pg_[:], [(mT[:, k, :], wv_[:, k, :]) for k in range(8)], r=[rt, mT], w=[pg_])
                dst = gA if i < 2 else gB
                hh_ = i % 2
                th = tnh[i % 2]
                S.act(lambda e: e.activation(th[:], pg_[:], AF.Tanh, scale=0.5), r=[pg_], w=[th])
                S.pool(lambda e: e.tensor_scalar(dst[:, hh_ * 512:(hh_ + 1) * 512], th[:], 1.0, 1.0,
                                                 ALU.add, ALU.mult), r=[th], w=[dst.s(hh_)])

            extra = {0: [lambda: zblk(1), lambda: uvblk(0)], 1: [lambda: uvblk(1)],
                     2: [lambda: zblk(2), lambda: uvblk(2)], 3: [lambda: uvblk(3)],
                     4: [lambda: zblk(3), lambda: gblk(0)], 5: [lambda: gblk(1)],
                     6: [lambda: gblk(2)], 7: [lambda: gblk(3)]}
            zblk(0)
            def s1(g):
                i2 = g % 2
                pcb = ps()
                S.mm(pcb[:, 0:128], [(xc[:, 16 + g, :], xc[:, 24 + g, :])], r=[xc.s(4 + g // 4), xc.s(6 + g // 4)], w=[pcb])
                S.dve(lambda e: e.tensor_tensor(cbm[i2][:], pcb[:, 0:128], Uf[:], ALU.mult), r=[pcb, Uf], w=[cbm[i2]])
                S.pool(lambda e: e.tensor_tensor(segr[i2][:], Ub[:].unsqueeze(1).to_broadcast([128, 4, 128]),
                                                 adt[:, 4 * g:4 * g + 4].unsqueeze(2).to_broadcast([128, 4, 128]),
                                                 ALU.mult), r=[Ub, adt], w=[segr[i2]])
                psg = ps()
                S.mm(psg[:], [(SLb[:], segr[i2][:].rearrange("p r l -> p (r l)"))], r=[SLb, segr[i2]], w=[psg])
                S.act(lambda e: e.activation(Eg[i2][:].rearrange("p r l -> p (r l)"), psg[:], AF.Exp), r=[psg], w=[Eg[i2]])
                S.dve(lambda e: e.tensor_tensor(WTg[i2][:], Eg[i2][:], cbm[i2][:].unsqueeze(1).to_broadcast([128, 4, 128]),
                                                ALU.mult), r=[Eg[i2], cbm[i2]], w=[WTg[i2]])

            def s2(g):
                i2 = g % 2
                py = ps()
                S.mm(py[:, 0:256], [(ident[:], xd_tm[:, g * 256:(g + 1) * 256])], r=[ident, xd_tm], w=[py], first=True, last=False)
                for r_ in range(4):
                    h = 4 * g + r_
                    S.mm(py[:, r_ * 64:(r_ + 1) * 64], [(WTg[i2][:, r_, :], xdt[:, h * 64:(h + 1) * 64])],
                         r=[WTg[i2], xdt], w=[py], first=False, last=True)
                S.mm(py[:, 256:512], [(xc[:, 24 + g, :], Sbf[:, g * 256:(g + 1) * 256])],
                     r=[xc.s(6 + g // 4), Sbf], w=[py])
                yt_ = ytmp[i2]
                S.dve(lambda e: e.tensor_tensor(
                    yt_[:].rearrange("p (r q) -> p r q", q=64), py[:, 256:512].rearrange("p (r q) -> p r q", q=64),
                    ea[:, 4 * g:4 * g + 4].unsqueeze(2).to_broadcast([128, 4, 64]), ALU.mult), r=[py, ea], w=[yt_])
                S.dve(lambda e: e.tensor_tensor(yt_[:], yt_[:], py[:, 0:256], ALU.add), r=[yt_, py], w=[yt_])
                S.dve(lambda e: e.tensor_tensor(yg[:, g * 256:(g + 1) * 256], yt_[:], zg[:, g * 256:(g + 1) * 256], ALU.mult),
                      r=[yt_, zg.s(g // 2)], w=[yg])
                S.act(lambda e: e.activation(sqj[:], yg[:, g * 256:(g + 1) * 256], AF.Square, accum_out=ssq[:, g:g + 1]),
                      r=[yg], w=[sqj, ssq])

            s1(0)
            for g in range(8):
                if g + 1 < 8:
                    s1(g + 1)
                s2(g)
                for f_ in extra[g]:
                    f_()
        if full:
            tap("yg", yg[:, 0:D], rr=[yg])
            tap("dt", dtt[:], n=32, rr=[dtt])
            tap("acs", acs[:], n=32, rr=[acs])
        for q4 in range(4):
            pst = ps()
            for gg in range(2):
                g = 2 * q4 + gg
                S.mm(pst[:, gg * 256:(gg + 1) * 256], [(B_tm[:, g * 128:(g + 1) * 128], xdte[:, g * 256:(g + 1) * 256])],
                     r=[B_tm, xdte], w=[pst])
            sl = slice(q4 * 512, (q4 + 1) * 512)
            S.dve(lambda e, sl=sl, q4=q4: e.tensor_tensor(Sst[:, sl].rearrange("p (h q) -> p h q", q=64),
                                                          Sst[:, sl].rearrange("p (h q) -> p h q", q=64),
                                                          dec[:, q4 * 8:(q4 + 1) * 8].unsqueeze(2).to_broadcast([128, 8, 64]),
                                                          ALU.mult), r=[Sst, dec], w=[Sst])
            S.dve(lambda e, sl=sl, pst=pst: e.tensor_tensor(Sst[:, sl], Sst[:, sl], pst[:], ALU.add), r=[Sst, pst], w=[Sst])
            S.act(lambda e, sl=sl: e.copy(Sbf[:, sl], Sst[:, sl]), r=[Sst], w=[Sbf])
        if not full:
            return
        S.dve(lambda e: e.tensor_reduce(rinv[:], ssq[:], AX.X, ALU.add), r=[ssq], w=[rinv])
        S.dve(lambda e: e.tensor_scalar(rinv[:], rinv[:], 1.0 / 2048.0, 4.0 * RMS_EPS, ALU.mult, ALU.add), r=[rinv], w=[rinv])
        S.pool(lambda e: e.tensor_tensor(rinv[:], rinv[:], mhalf[:], ALU.pow), r=[rinv, mhalf], w=[rinv])
        yTs = [xc.s(0), xc.s(1), xc.s(2), xc.s(3)]
        for b0, pt, ptv in transposes8(yg, None, 16):
            S.dve(lambda e, b0=b0, ptv=ptv: e.tensor_tensor(xc[:, b0:b0 + 8, :], ptv,
                                                           normw_pk[:, b0:b0 + 8].unsqueeze(2).to_broadcast([128, 8, 128]),
                                                           ALU.mult), r=[pt, normw_pk], w=yTs[b0 // 4:b0 // 4 + 2])
        S.dve(lambda e: e.bn_stats(st6[:, 0:6], v_t[:, 0:512]), r=[v_t.s(0)], w=[st6])
        S.dve(lambda e: e.bn_stats(st6[:, 6:12], v_t[:, 512:1024]), r=[v_t.s(1)], w=[st6])
        S.dve(lambda e: e.bn_aggr(mv[:], st6[:]), r=[st6], w=[mv])
        S.dve(lambda e: e.tensor_scalar_add(rstd[:], mv[:, 1:2], 4.0 * LN_EPS), r=[mv], w=[rstd])
        S.pool(lambda e: e.tensor_tensor(rstd[:], rstd[:], mhalf[:], ALU.pow), r=[rstd, mhalf], w=[rstd])
        S.dve(lambda e: e.tensor_scalar(v_t[:], v_t[:], mv[:, 0:1], rstd[:, 0:1], ALU.subtract, ALU.mult),
              r=[v_t.s(0), v_t.s(1), mv, rstd], w=[v_t.s(0), v_t.s(1)])
        S.dve(lambda e: e.tensor_tensor(v_t[:], v_t[:], lng_row[:], ALU.mult), r=[v_t.s(0), v_t.s(1), lng_row],
              w=[v_t.s(0), v_t.s(1)])
        S.dve(lambda e: e.tensor_tensor(vb[:], v_t[:], lnb_row[:], ALU.add), r=[v_t.s(0), v_t.s(1), lnb_row], w=[vb])
        for hf in range(2):
            pv = ps()
            for gg in range(4):
                g = 4 * hf + gg
                S.mm(pv[:, gg * 128:(gg + 1) * 128], [(WsT[:, g, :], vb[:, g * 128:(g + 1) * 128])], r=[WsT, vb], w=[pv])
            for gg in range(4):
                g = 4 * hf + gg
                S.dve(lambda e, g=g, gg=gg, pv=pv: e.scalar_tensor_tensor(
                    gm[:, g * 128:(g + 1) * 128], pv[:, gg * 128:(gg + 1) * 128], bsT[:, g:g + 1],
                    u_t[:, g * 128:(g + 1) * 128], ALU.add, ALU.mult), r=[pv, bsT, u_t.s(hf)], w=[gm])
        for b0, pt, ptv in transposes8(gm, gmT, 8):
            S.act(lambda e, ptv=ptv: e.copy(gmT[:], ptv), r=[pt], w=[gmT])
        pya = [ps(), ps()]
        for ch in range(2):
            for kh in range(2):
                rt, wv_ = wget("ps%d%d" % (ch, kh))
                S.mm(pya[ch][:], [(xc[:, kh * 8 + k, :], wv_[:, k, :]) for k in range(8)], r=[rt] + yTs, w=[pya[ch]],
                     first=(kh == 0), last=(kh == 1))
        pyb = [ps(), ps()]
        for ch in range(2):
            rt, wv_ = wget("pg%d" % ch)
            S.mm(pyb[ch][:], [(gmT[:, k, :], wv_[:, k, :]) for k in range(8)], r=[rt, gmT], w=[pyb[ch]])
        if debug in ("ya", "yb"):
            for ch in range(2):
                sl = slice(ch * 512, (ch + 1) * 512)
                if debug == "ya":
                    S.dve(lambda e, ch=ch, sl=sl: e.tensor_scalar(h1[:, sl], pya[ch][:], rinv[:, 0:1], None, ALU.mult),
                          r=[pya[ch], rinv], w=[h1])
                else:
                    S.dve(lambda e, ch=ch, sl=sl: e.tensor_scalar(h1[:, sl], pyb[ch][:], 0.5, None, ALU.mult),
                          r=[pyb[ch]], w=[h1])
            S.dma("sp", dbg[ci * 128:(ci + 1) * 128, :], h1[:], r=[h1])
        for ch in range(2):
            sl = slice(ch * 512, (ch + 1) * 512)
            S.dve(lambda e, ch=ch, sl=sl: e.scalar_tensor_tensor(mtmp[0][:], pya[ch][:], rinv[:, 0:1], gA[:, sl],
                                                                 ALU.mult, ALU.mult), r=[pya[ch], rinv, gA.s(ch)], w=[mtmp[0]])
            S.dve(lambda e, ch=ch, sl=sl: e.scalar_tensor_tensor(mtmp[1][:], pyb[ch][:], 0.5, gB[:, sl],
                                                                 ALU.mult, ALU.mult), r=[pyb[ch], gB.s(ch)], w=[mtmp[1]])
            S.dve(lambda e, sl=sl: e.tensor_tensor(mg[:, sl], mtmp[0][:], mtmp[1][:], ALU.add),
                  r=[mtmp[0], mtmp[1]], w=[mg])
        for b0, pt, ptv in transposes8(mg, mgT, 8):
            S.act(lambda e, ptv=ptv: e.copy(mgT[:], ptv), r=[pt], w=[mgT])
        pmx = [ps(), ps()]
        pin_ids = [banks.index(p_) for p_ in pmx]
        pinned.update(pin_ids)
        for ch in range(2):
            rt, wv_ = wget("wo%d" % ch)
            S.mm(pmx[ch][:], [(mgT[:, k, :], wv_[:, k, :]) for k in range(8)], r=[rt, mgT], w=[pmx[ch]])
        if hoist is not None:
            hoist()
        if debug == "mix":
            for ch in range(2):
                sl = slice(ch * 512, (ch + 1) * 512)
                S.dve(lambda e, ch=ch, sl=sl: e.tensor_scalar(h1[:, sl], pmx[ch][:], 0.5, None, ALU.mult),
                      r=[pmx[ch]], w=[h1])
            S.dma("sp", dbg[ci * 128:(ci + 1) * 128, :], h1[:], r=[h1])
        for ch in range(2):
            sl = slice(ch * 512, (ch + 1) * 512)
            S.dve(lambda e, ch=ch, sl=sl: e.tensor_tensor(mtmp[ch][:], pmx[ch][:], g1h_row[:, sl], ALU.mult),
                  r=[pmx[ch], g1h_row], w=[mtmp[ch]])
            S.dve(lambda e, ch=ch, sl=sl: e.scalar_tensor_tensor(xt[:, sl], xt[:, sl], ALPHA, mtmp[ch][:], ALU.mult, ALU.add),
                  r=[xt, mtmp[ch]], w=[xt])
        pinned.difference_update(pin_ids)
        layer_norm_stats(xt, LN_EPS)
        S.dve(lambda e: e.tensor_scalar(h1[:], xt[:], mv[:, 0:1], rstd[:, 0:1], ALU.subtract, ALU.mult),
              r=[xt, mv, rstd], w=[h1])
        S.dve(lambda e: e.tensor_tensor(h1[:], h1[:], ln1g_row[:], ALU.mult), r=[h1, ln1g_row], w=[h1])
        S.dve(lambda e: e.tensor_tensor(h1[:], h1[:], ln1b_row[:], ALU.add), r=[h1, ln1b_row], w=[h1])
        if debug == "h1":
            S.dma("sp", dbg[ci * 128:(ci + 1) * 128, :], h1[:], r=[h1])
        layer_norm_stats(h1, LN_EPS)
        S.dve(lambda e: e.tensor_scalar(xt[:], h1[:], mv[:, 0:1], rstd[:, 0:1], ALU.subtract, ALU.mult),
              r=[h1, mv, rstd], w=[xt])
        S.dve(lambda e: e.tensor_tensor(xt[:], xt[:], sc2_row[:], ALU.mult), r=[xt, sc2_row], w=[xt])
        S.dve(lambda e: e.tensor_tensor(m2[:], xt[:], sh2_row[:], ALU.add), r=[xt, sh2_row], w=[m2])
        def tail():
            for b0, pt, ptv in transposes8(m2, m2T, 8):
                S.act(lambda e, ptv=ptv: e.copy(m2T[:], ptv), r=[pt], w=[m2T])
            rt, wv_ = wget("rt")
            plg = ps()
            S.mm(plg[:, 0:256], [(m2T[:, k, :], wv_[:, k, :]) for k in range(8)], r=[rt, m2T], w=[plg])
            S.act(lambda e: e.activation(sco[:], plg[:, 0:256], AF.Tanh, scale=0.5), r=[plg], w=[sco])
            rt, wv_ = wget("sgu")
            phs = ps()
            for blk in range(4):
                S.mm(phs[:, blk * 128:(blk + 1) * 128], [(wv_[:, k, blk * 128:(blk + 1) * 128], m2T[:, k, :]) for k in range(8)],
                     r=[rt, m2T], w=[phs])
            S.act(lambda e: e.activation(hsa[:], phs[:, 0:256], AF.Tanh, scale=0.5), r=[phs], w=[hsa])
            S.dve(lambda e: e.scalar_tensor_tensor(hsa[:], hsa[:], 1.0, phs[:, 0:256], ALU.add, ALU.mult), r=[hsa, phs], w=[hsa])
            S.dve(lambda e: e.tensor_tensor(hs[:].rearrange("p b t -> p (b t)"), hsa[:], phs[:, 256:512], ALU.mult),
                  r=[hsa, phs], w=[hs])
            rt, wv_ = wget("sd")
            psd = [ps(), ps()]
            for ch in range(2):
                S.mm(psd[ch][:], [(hs[:, b, :], wv_[:, b, ch * 512:(ch + 1) * 512]) for b in range(2)], r=[rt, hs], w=[psd[ch]])
            for ch in range(2):
                sl = slice(ch * 512, (ch + 1) * 512)
                S.dve(lambda e, ch=ch, sl=sl: e.tensor_tensor(mtmp[ch][:], psd[ch][:], g2h_row[:, sl], ALU.mult),
                      r=[psd[ch], g2h_row], w=[mtmp[ch]])
                S.dve(lambda e, ch=ch, sl=sl: e.scalar_tensor_tensor(v_t[:, sl], h1[:, sl], ALPHA, mtmp[ch][:], ALU.mult, ALU.add),
                      r=[h1, mtmp[ch]], w=[v_t.s(ch)])
            S.dma(os.environ.get("K_STQ", "pool"), res2_d[ci * 128:(ci + 1) * 128, :], v_t[:], r=[v_t.s(0), v_t.s(1)], w=[res2_b[ci]])
            S.dve(lambda e: e.tensor_scalar(sco[:], sco[:], 0.5, 0.5, ALU.mult, ALU.add), r=[sco], w=[sco])
            S.dve(lambda e: e.tensor_tensor(cho[:], sco[:], rb_row[:], ALU.add), r=[sco, rb_row], w=[cho])
            for g in range(8):
                S.dve(lambda e, g=g: e.max(g8[:, g, :], cho[:, g * 32:(g + 1) * 32]), r=[cho], w=[g8])
            S.dve(lambda e: e.tensor_tensor(gs[:], g8[:, :, 0], g8[:, :, 1], ALU.add), r=[g8], w=[gs])
            S.dve(lambda e: e.max(gs8[:], gs[:]), r=[gs], w=[gs8])
            S.dve(lambda e: e.tensor_scalar(gpen[:], gs[:], gs8[:, 3:4], None, ALU.is_ge), r=[gs, gs8], w=[gpen])
            S.dve(lambda e: e.tensor_scalar(gpen[:], gpen[:], -1.0, BIG, ALU.add, ALU.mult), r=[gpen], w=[gpen])
            S.dve(lambda e: e.tensor_tensor(cho[:].rearrange("p (g q) -> p g q", q=32), cho[:].rearrange("p (g q) -> p g q", q=32),
                                            gpen[:].unsqueeze(2).to_broadcast([128, 8, 32]), ALU.add), r=[cho, gpen], w=[cho])
            S.dve(lambda e: e.max(top8[:], cho[:]), r=[cho], w=[top8])
            S.dve(lambda e: e.tensor_scalar(selb[:], cho[:], top8[:, 7:8], None, ALU.is_ge), r=[cho, top8], w=[selb])
            S.dve(lambda e: e.tensor_tensor(cho[:], sco[:], selb[:], ALU.mult), r=[sco, selb], w=[cho])
            S.dve(lambda e: e.max(sk8[:], cho[:]), r=[cho], w=[sk8])
            S.dve(lambda e: e.max_index(idx8[:], sk8[:], cho[:]), r=[sk8, cho], w=[idx8])
            S.dve(lambda e: e.tensor_copy(idxf[:], idx8[:]), r=[idx8], w=[idxf])
            ppos = ps()
            S.mm(ppos[:, 0:256], [(SUb[:], selb[:]), (onesb[:], Rcnt[:])], r=[SUb, selb, onesb, Rcnt], w=[ppos])
            S.act(lambda e: e.copy(posf[:], ppos[:, 0:256]), r=[ppos], w=[posf])
            S.pool(lambda e: e.tensor_tensor(Rcnt[:], Rcnt[:], selb[:], ALU.add), r=[Rcnt, selb], w=[Rcnt])
            for k in range(8):
                S.dve(lambda e, k=k: e.scalar_tensor_tensor(junk[:], iota_e[:], idxf[:, k:k + 1], posf[:], ALU.is_equal, ALU.mult,
                                                            accum_out=pk8[:, k:k + 1]), r=[iota_e, idxf, posf], w=[junk, pk8])
            S.dve(lambda e: e.tensor_copy(idx_all[:, ci, :], idxf[:]), r=[idxf], w=[idx_all])
            S.dve(lambda e: e.tensor_copy(pos_all[:, ci, :], pk8[:]), r=[pk8], w=[pos_all])
            S.dve(lambda e: e.tensor_reduce(ssum[:], sk8[:], AX.X, ALU.add), r=[sk8], w=[ssum])
            S.dve(lambda e: e.tensor_scalar_add(ssum[:], ssum[:], 1e-20), r=[ssum], w=[ssum])
            S.dve(lambda e: e.reciprocal(ssum[:], ssum[:]), r=[ssum], w=[ssum])
            S.dve(lambda e: e.tensor_scalar(wk_t[:, ci, :], sk8[:], ssum[:, 0:1], 1.25, ALU.mult, ALU.mult), r=[sk8, ssum], w=[wk_t])
            S.dma(os.environ.get("K_STQ", "pool"), m2_d[ci * 128:(ci + 1) * 128, :], m2[:], r=[m2], w=[m2_b[ci]])
        pending.append(tail)

    for i in range(NPV):
        nxt = x_prev[(i + 1) * 128:(i + 2) * 128, :] if i + 1 < NPV else None
        chunk(i, x_prev[i * 128:(i + 1) * 128, :], "A", last_prev=(i == NPV - 1), next_src=nxt)
    if NPV > 0:
        S.dve(lambda e: e.tensor_scalar_mul(Sst[:], Sst[:], flag_t[:, 0:1]), r=[Sst, flag_t], w=[Sst])
        S.dve(lambda e: e.tensor_scalar_mul(Sbf[:], Sbf[:], flag_t[:, 0:1]), r=[Sbf, flag_t], w=[Sbf])
        S.dve(lambda e: e.tensor_scalar_mul(xBCT[:, :, 0:3], xBCT[:, :, 0:3], flag_t[:, 0:1]),
              r=[xBCT, flag_t] + xBCT.subs, w=[xBCT] + xBCT.subs)
    chunk(0, x_cur[0:128, :], "M", part="head")
    for i in range(NCH):
        hz = None
        if i + 1 < NCH:
            hz = (lambda j=i + 1: chunk(j, x_cur[j * 128:(j + 1) * 128, :], "M", part="head"))
        chunk(i, x_cur[i * 128:(i + 1) * 128, :], "M", part="rest", hoist=hz)
    pending.pop()()

    S.barrier()
    ph1.close()
    ph2 = ExitStack()
    scope[0] = ph2
    I32 = mybir.dt.int32
    cntc = sb("cntc", [128, 2], F32)
    ci32 = sb("ci32", [128, 2], I32)
    padc = sb("padc", [128, 2], F32)
    padb = sb("padb", [128, 2], BF16)
    pendc = sb("pendc", [128, 2], F32)
    pstc = sb("pstc", [128, 2], F32)
    tot = sb("tot", [128, 1], F32)
    onesf = sb("onesf", [128, 128], F32)
    dgf = sb("dgf", [128, 128], F32)
    PSrow = sb("PSrow", [128, 256], F32)
    iob = sb("iob", [128, NEB], F32)
    iop = sb("iop", [128, 1], F32)
    cmpb = [sb("cmpb%d" % i, [128, NEB], BF16) for i in range(2)]
    BEf = sb("BEf", [128, NEB], F32)
    usedf = sb("usedf", [128, NEB], F32)
    IDXW = sb("IDXW", [128, NEB], U32)
    psk = sb("psk", [128, 8], F32)
    junk2 = sb("junk2", [128, 256], F32)
    m2r = [sb("m2r%d" % i, [128, D], BF16) for i in range(2)]
    S.pool(lambda e: e.memset(onesf[:], 1.0), w=[onesf])
    S.pool(lambda e: e.iota(iob[:], pattern=[[128, NEB]], base=0, channel_multiplier=0,
                            allow_small_or_imprecise_dtypes=True), w=[iob])
    S.pool(lambda e: e.iota(iop[:], pattern=[[0, 1]], base=0, channel_multiplier=1,
                            allow_small_or_imprecise_dtypes=True), w=[iop])
    pc_ = ps()
    for h in range(2):
        S.mm(pc_[:, h:h + 1], [(Rcnt[:, h * 128:(h + 1) * 128], onesb[:, 0:1])], r=[Rcnt, onesb], w=[pc_])
    S.dve(lambda e: e.tensor_copy(cntc[:], pc_[:, 0:2]), r=[pc_], w=[cntc])
    S.dve(lambda e: e.tensor_scalar_add(ci32[:], cntc[:], 127.0), r=[cntc], w=[ci32])
    S.dve(lambda e: e.tensor_scalar(ci32[:], ci32[:], 7, 7, ALU.arith_shift_right, ALU.logical_shift_left), r=[ci32], w=[ci32])
    S.dve(lambda e: e.tensor_copy(padc[:], ci32[:]), r=[ci32], w=[padc])
    S.dve(lambda e: e.tensor_copy(padb[:], padc[:]), r=[padc], w=[padb])
    pq = ps()
    S.mm(pq[:, 0:1], [(Ub[:], padb[:, 0:1])], r=[Ub, padb], w=[pq])
    S.mm(pq[:, 1:2], [(Ub[:], padb[:, 1:2]), (onesb[:], padb[:, 0:1])], r=[Ub, onesb, padb], w=[pq])
    S.mm(pq[:, 2:3], [(onesb[:], padb[:, 0:1]), (onesb[:], padb[:, 1:2])], r=[onesb, padb], w=[pq])
    S.dve(lambda e: e.tensor_copy(pendc[:], pq[:, 0:2]), r=[pq], w=[pendc])
    S.dve(lambda e: e.tensor_copy(tot[:], pq[:, 2:3]), r=[pq], w=[tot])
    S.dve(lambda e: e.tensor_tensor(pstc[:], pendc[:], padc[:], ALU.subtract), r=[pendc, padc], w=[pstc])
    pr_ = ps()
    for h in range(2):
        S.dve(lambda e, h=h: e.tensor_scalar(dgf[:], identf[:], pstc[:, h:h + 1], None, ALU.mult), r=[identf, pstc], w=[dgf])
        S.mm(pr_[:, h * 128:(h + 1) * 128], [(onesf[:], dgf[:])], r=[onesf, dgf], w=[pr_])
    S.dve(lambda e: e.tensor_copy(PSrow[:], pr_[:, 0:256]), r=[pr_], w=[PSrow])
    pbe = ps()
    for h in range(2):
        S.dve(lambda e, h=h: e.tensor_scalar(cmpb[h][:], iob[:], pendc[:, h:h + 1], None, ALU.is_ge), r=[iob, pendc], w=[cmpb[h]])
    S.mm(pbe[:, 0:NEB], [(onesb[:], cmpb[0][:]), (onesb[:], cmpb[1][:])], r=[onesb, cmpb[0], cmpb[1]], w=[pbe])
    S.dve(lambda e: e.tensor_scalar(BEf[:], pbe[:, 0:NEB], 255.0, 128.0, ALU.min, ALU.mult), r=[pbe], w=[BEf])
    S.dve(lambda e: e.tensor_scalar(BEf[:], BEf[:], iop[:, 0:1], None, ALU.add), r=[BEf, iop], w=[BEf])
    S.dve(lambda e: e.tensor_scalar(usedf[:], iob[:], tot[:, 0:1], 1.0e6, ALU.is_ge, ALU.mult), r=[iob, tot], w=[usedf])
    S.dve(lambda e: e.tensor_tensor(BEf[:], BEf[:], usedf[:], ALU.add), r=[BEf, usedf], w=[BEf])
    S.dve(lambda e: e.tensor_copy(IDXW[:], BEf[:]), r=[BEf], w=[IDXW])
    for ci in range(NCH):
        mr = m2r[ci % 2]
        S.dma("sp", mr[:], m2_d[ci * 128:(ci + 1) * 128, :], r=[m2_b[ci]], w=[mr])
        for k in range(8):
            S.dve(lambda e, k=k, ci=ci: e.scalar_tensor_tensor(junk2[:], iota_e[:], idx_all[:, ci, k:k + 1], PSrow[:],
                                                               ALU.is_equal, ALU.mult, accum_out=psk[:, k:k + 1]),
                  r=[iota_e, idx_all, PSrow], w=[junk2, psk])
        S.dve(lambda e, ci=ci: e.tensor_tensor(psk[:], psk[:], pos_all[:, ci, :], ALU.add), r=[psk, pos_all], w=[psk])
        S.dve(lambda e, ci=ci: e.tensor_copy(slot_u[:, ci, :], psk[:]), r=[psk], w=[slot_u])
        for k in range(8):
            S.dma("pool", None, None, r=[mr, slot_u], w=[],
                  fn=lambda e, k=k, ci=ci, mr=mr: e.indirect_dma_start(
                      out=xs_d[:, :], out_offset=bass.IndirectOffsetOnAxis(ap=slot_u[:, ci, k:k + 1], axis=0),
                      in_=mr[:, :], in_offset=None, bounds_check=reg_slot, oob_is_err=False))
    S.barrier()

    NWB = 3
    wg = [sb("wg%d" % i, [128, 8, 256], BF16) for i in range(NWB)]
    wu = [sb("wu%d" % i, [128, 8, 256], BF16) for i in range(NWB)]
    wd = [sb("wd%d" % i, [128, 2, D], BF16) for i in range(NWB)]
    xsb = [sb("xsb%d" % i, [128, D], BF16) for i in range(4)]
    xsT = [sb("xsT%d" % i, [128, 8, 128], BF16) for i in range(2)]
    hga = [sb("hga%d" % i, [128, 2, 128], F32) for i in range(2)]
    hh = [sb("hh%d" % i, [128, 2, 128], BF16) for i in range(2)]
    yo = [sb("yo%d" % i, [128, D], BF16) for i in range(3)]
    for t_ in wg + wu + wd:
        S.pool(lambda e, t_=t_: e.memset(t_[:], 0.0), w=[t_])
    weg = w_e_gate[:, :]
    weu = w_e_up[:, :]
    wed = w_e_down[:, :]
    def xs_load(b_):
        if b_ < NEB:
            S.dma("sp", xsb[b_ % 4][:], xs_d[b_ * 128:(b_ + 1) * 128, :], r=[xs_b], w=[xsb[b_ % 4]])

    xs_load(0)
    xs_load(1)
    for bk in range(NEB):
        i2 = bk % 2
        iw = bk % NWB
        xs_load(bk + 2)
        for (wt_, src) in ((wg[iw], weg), (wu[iw], weu), (wd[iw], wed)):
            S.dma("pool", None, None, r=[IDXW], w=[wt_],
                  fn=lambda e, wt_=wt_, src=src, bk=bk: e.indirect_dma_start(
                      out=wt_[:].rearrange("p a b -> p (a b)"), out_offset=None, in_=src,
                      in_offset=bass.IndirectOffsetOnAxis(ap=IDXW[:, bk:bk + 1], axis=0),
                      bounds_check=reg_w, oob_is_err=False))
        xb = xsb[bk % 4]
        sv = xb[:].rearrange("t (p k) -> t k p", k=8)
        pt = ps()
        ptv = psb(pt).rearrange("p (g t) -> p g t", g=8)
        for kk in range(8):
            S.tr(ptv[:, kk, :], sv[:, kk, :], ident[:], r=[xb, ident], w=[pt])
        S.act(lambda e, i2=i2, ptv=ptv: e.copy(xsT[i2][:], ptv), r=[pt], w=[xsT[i2]])
        pgu = ps()
        for fb in range(2):
            wgv = wg[iw][:].rearrange("p k (q b) -> p k b q", b=2)
            wuv = wu[iw][:].rearrange("p k (q b) -> p k b q", b=2)
            S.mm(pgu[:, fb * 128:(fb + 1) * 128], [(wgv[:, k, fb, :], xsT[i2][:, k, :]) for k in range(8)],
                 r=[wg[iw], xsT[i2]], w=[pgu])
            S.mm(pgu[:, 256 + fb * 128:256 + (fb + 1) * 128], [(wuv[:, k, fb, :], xsT[i2][:, k, :]) for k in range(8)],
                 r=[wu[iw], xsT[i2]], w=[pgu])
        hg_ = hga[i2][:].rearrange("p b t -> p (b t)")
        S.act(lambda e, hg_=hg_, pgu=pgu: e.activation(hg_, pgu[:, 0:256], AF.Tanh, scale=0.5), r=[pgu], w=[hga[i2]])
        S.dve(lambda e, hg_=hg_, pgu=pgu: e.scalar_tensor_tensor(hg_, hg_, 1.0, pgu[:, 0:256], ALU.add, ALU.mult),
              r=[hga[i2], pgu], w=[hga[i2]])
        S.dve(lambda e, i2=i2, hg_=hg_, pgu=pgu: e.tensor_tensor(hh[i2][:].rearrange("p b t -> p (b t)"), hg_, pgu[:, 256:512],
                                                                 ALU.mult), r=[hga[i2], pgu], w=[hh[i2]])
        yb_ = yo[bk % 3]
        for ch in range(2):
            pd_ = ps()
            S.mm(pd_[:], [(hh[i2][:, b_, :], wd[iw][:, b_, ch * 512:(ch + 1) * 512]) for b_ in range(2)],
                 r=[hh[i2], wd[iw]], w=[pd_])
            S.act(lambda e, yb_=yb_, ch=ch, pd_=pd_: e.copy(yb_[:, ch * 512:(ch + 1) * 512], pd_[:]), r=[pd_], w=[yb_])
        S.dma(os.environ.get("K_YSQ", "act"), ys_d[bk * 128:(bk + 1) * 128, :], yb_[:], r=[yb_], w=[])

    S.barrier()
    ph2.close()
    ph3 = ExitStack()
    scope[0] = ph3
    ln2g_row = sb("ln2g_row", [128, D], F32)
    ln2b_row = sb("ln2b_row", [128, D], F32)
    g2_row = sb("g2_row", [128, D], F32)
    S.dma("sp", ln2g_row[:], ln2_g.partition_broadcast(128), w=[ln2g_row])
    S.dma("sp", ln2b_row[:], ln2_b.partition_broadcast(128), w=[ln2b_row])
    S.dve(lambda e: e.tensor_scalar_mul(g2_row[:], g2h_row[:], 2.0), r=[g2h_row], w=[g2_row])
    yk = [sb("yk%d" % i, [128, D], BF16) for i in range(4)]
    facc = [sb("facc%d" % i, [128, D], F32) for i in range(2)]
    r2 = [sb("r2_%d" % i, [128, D], F32) for i in range(2)]
    for t_ in yk:
        S.pool(lambda e, t_=t_: e.memset(t_[:], 0.0), w=[t_])
    gi = 0
    for ci in range(NCH):
        i2 = ci % 2
        S.dma("sp", r2[i2][:], res2_d[ci * 128:(ci + 1) * 128, :], r=[res2_b[ci]], w=[r2[i2]])
        fa = facc[i2]
        for k in range(8):
            yt_ = yk[gi % 4]
            gi += 1
            S.dma("pool", None, None, r=[ys_b, slot_u], w=[yt_],
                  fn=lambda e, k=k, yt_=yt_: e.indirect_dma_start(
                      out=yt_[:, :], out_offset=None, in_=ys_d[:, :],
                      in_offset=bass.IndirectOffsetOnAxis(ap=slot_u[:, ci, k:k + 1], axis=0),
                      bounds_check=reg_slot, oob_is_err=False))
            if k == 0:
                S.dve(lambda e, yt_=yt_, fa=fa: e.tensor_scalar(fa[:], yt_[:], wk_t[:, ci, 0:1], None, ALU.mult),
                      r=[yt_, wk_t], w=[fa])
            else:
                S.dve(lambda e, yt_=yt_, fa=fa, k=k: e.scalar_tensor_tensor(fa[:], yt_[:], wk_t[:, ci, k:k + 1], fa[:],
                                                                          ALU.mult, ALU.add), r=[yt_, wk_t, fa], w=[fa])
        S.dve(lambda e, fa=fa: e.tensor_tensor(fa[:], fa[:], g2_row[:], ALU.mult), r=[fa, g2_row], w=[fa])
        S.dve(lambda e, fa=fa, i2=i2: e.tensor_tensor(fa[:], fa[:], r2[i2][:], ALU.add), r=[fa, r2[i2]], w=[fa])
        layer_norm_stats(fa, LN_EPS)
        S.dve(lambda e, fa=fa: e.tensor_scalar(fa[:], fa[:], mv[:, 0:1], rstd[:, 0:1], ALU.subtract, ALU.mult),
              r=[fa, mv, rstd], w=[fa])
        S.dve(lambda e, fa=fa: e.tensor_tensor(fa[:], fa[:], ln2g_row[:], ALU.mult), r=[fa, ln2g_row], w=[fa])
        S.dve(lambda e, fa=fa: e.tensor_tensor(fa[:], fa[:], ln2b_row[:], ALU.add), r=[fa, ln2b_row], w=[fa])
        S.dma("sp", out[ci * 128:(ci + 1) * 128, :], fa[:], r=[fa])
    S.finish()
    S.barrier()
    ph3.close()
    return nc, S


_NAMES = ["w_ada", "b_ada", "w_in", "conv_w", "conv_b", "dt_bias", "a_log", "d_skip", "ssd_norm_w",
          "gmlp_ln_g", "gmlp_ln_b", "gmlp_ws", "gmlp_bs", "w_proj_ssd", "w_proj_gmlp", "w_out",
          "ln1_g", "ln1_b", "w_router", "router_bias", "w_e_gate", "w_e_up", "w_e_down",
          "w_sh_gate", "w_sh_up", "w_sh_down", "ln2_g", "ln2_b"]


def make_in_maps(inputs, NCH, NPV, seq_per_core=None):
    x = np.asarray(inputs["x"], dtype=np.float32)
    c = np.asarray(inputs["c"], dtype=np.float32)
    shared = {n: np.ascontiguousarray(np.asarray(inputs[n], dtype=np.float32)[0]) for n in _NAMES}
    for n in ("w_e_gate", "w_e_up", "w_e_down"):
        shared[n] = shared[n].reshape(256 * 128, 2048)
    maps = []
    ncores = 2 * x.shape[0]
    for core in range(ncores):
        b, half = core // 2, core % 2
        cur0 = half * NPV * 128 if NPV > 0 else 0
        m = dict(shared)
        m["x_cur"] = np.ascontiguousarray(x[b, cur0:cur0 + NCH * 128, :])
        m["x_prev"] = np.ascontiguousarray(x[b, 0:max(NPV, 1) * 128, :])
        m["flag"] = np.full((128, 1), float(half), dtype=np.float32)
        m["c_b"] = np.ascontiguousarray(c[b])
        maps.append(m)
    return maps


def kernel(**inputs):
    NCH, NPV, C = 32, 32, 256
    nc, _ = build(NCH, NPV, C)
    maps = make_in_maps(inputs, NCH, NPV)
    res = run_bass_kernel_spmd(nc, maps, core_ids=list(range(8)))
    x = np.asarray(inputs["x"])
    out = np.empty(x.shape, dtype=np.float32)
    for core in range(8):
        b, half = core // 2, core % 2
        out[b, half * NCH * 128:(half + 1) * NCH * 128, :] = res.results[core]["out"]
    return out
```
